# Optimizing a Trainium2 kernel written in Bass

```python
import math
import jax, jax.numpy as jnp
from jax import lax
import numpy as np

D_MODEL = 1024
BATCH = 16
SEQ = 2048
DEPTH = 4

N_MIXERS = 2
N_NSA_LAYERS = (DEPTH + N_MIXERS - 1) // N_MIXERS
N_SSD_LAYERS = DEPTH // N_MIXERS

NSA_HEADS = 16
NSA_HEAD_DIM = 64
NSA_KV_GROUPS = 4
NSA_HEADS_PER_GROUP = NSA_HEADS // NSA_KV_GROUPS
COMP_BLOCK = 32
COMP_STRIDE = 16
COMP_HIDDEN = 256
SEL_BLOCK = 64
N_SELECT = 16
N_LOCAL_BLOCKS = 2
WINDOW = 512
N_BRANCHES = 3
WIN_Q_BLOCK = 128
SEL_Q_BLOCK = 16
NSA_Q_DIM = NSA_HEADS * NSA_HEAD_DIM
NSA_KV_DIM = NSA_KV_GROUPS * NSA_HEAD_DIM
NSA_PROJ_DIM = NSA_Q_DIM + 2 * N_BRANCHES * NSA_KV_DIM + NSA_HEADS * N_BRANCHES

SSD_EXPAND = 2
SSD_D_INNER = SSD_EXPAND * D_MODEL
SSD_HEAD_DIM = 64
SSD_HEADS = SSD_D_INNER // SSD_HEAD_DIM
SSD_GROUPS = 4
SSD_STATE = 128
SSD_CONV = 4
SSD_CHUNK = 128
SSD_CONV_DIM = SSD_D_INNER + 2 * SSD_GROUPS * SSD_STATE
SSD_PROJ_DIM = SSD_D_INNER + SSD_CONV_DIM + SSD_HEADS

FFN_HIDDEN = 2816
FFN_CONV = 3

RMS_EPS = 1e-6
NEG_INF = -1e30
SEL_FORCE = 1e9

kernel_name = "hybrid_nsa_ssd_convffn_trunk"


def rms_norm(x, gain):
    xf = x.astype(jnp.float32)
    y = xf * lax.rsqrt(jnp.mean(xf * xf, axis=-1, keepdims=True) + RMS_EPS)
    return (y * gain.astype(jnp.float32)).astype(x.dtype)


def causal_dwconv(x, w, b):
    width, ch = w.shape
    y = lax.conv_general_dilated(
        x, w.astype(x.dtype)[:, None, :], window_strides=(1,),
        padding=[(width - 1, 0)], dimension_numbers=("NWC", "WIO", "NWC"),
        feature_group_count=ch)
    return y + b.astype(x.dtype)


def masked_softmax(scores, mask):
    return jax.nn.softmax(jnp.where(mask, scores, NEG_INF), axis=-1)


def compress_tokens(t, pos, w1, w2):
    b, s, g, d = t.shape
    nc = (s - COMP_BLOCK) // COMP_STRIDE + 1
    idx = np.arange(nc)[:, None] * COMP_STRIDE + np.arange(COMP_BLOCK)[None, :]
    blocks = t[:, idx] + pos.astype(t.dtype)[None, None, :, None, :]
    flat = blocks.transpose(0, 1, 3, 2, 4).reshape(b, nc, g, COMP_BLOCK * d)
    return jax.nn.gelu(flat @ w1) @ w2


def cmp_to_sel_overlap(nc, nsb):
    cs = np.arange(nc) * COMP_STRIDE
    ss = np.arange(nsb) * SEL_BLOCK
    ov = (np.minimum(cs[:, None] + COMP_BLOCK, ss[None, :] + SEL_BLOCK)
          - np.maximum(cs[:, None], ss[None, :]))
    return np.clip(ov, 0, None).astype(np.float32) / COMP_BLOCK


def nsa_mixer(h, w_in, cmp_pos, cmp_w1, cmp_w2, w_out):
    b, s, _ = h.shape
    g, hpg, hd = NSA_KV_GROUPS, NSA_HEADS_PER_GROUP, NSA_HEAD_DIM
    scale = hd ** -0.5
    kv_end = NSA_Q_DIM + 2 * N_BRANCHES * NSA_KV_DIM
    proj = h @ w_in
    q = proj[..., :NSA_Q_DIM].reshape(b, s, g, hpg, hd)
    kv = proj[..., NSA_Q_DIM:kv_end].reshape(b, s, 2 * N_BRANCHES, g, hd)
    gates = jax.nn.sigmoid(proj[..., kv_end:].astype(jnp.float32)).reshape(b, s, g, hpg, N_BRANCHES)
    k_cmp, v_cmp = kv[:, :, 0], kv[:, :, 1]
    k_sel, v_sel = kv[:, :, 2], kv[:, :, 3]
    k_win, v_win = kv[:, :, 4], kv[:, :, 5]
    pos = np.arange(s)

    kc = compress_tokens(k_cmp, cmp_pos[0], cmp_w1[0], cmp_w2[0])
    vc = compress_tokens(v_cmp, cmp_pos[1], cmp_w1[1], cmp_w2[1])
    nc = kc.shape[1]
    cmp_mask = (np.arange(nc)[None, :] * COMP_STRIDE + COMP_BLOCK - 1) <= pos[:, None]
    sc = jnp.einsum("bsghd,bcgd->bghsc", q, kc).astype(jnp.float32) * scale
    p_cmp = jnp.where(cmp_mask, masked_softmax(sc, cmp_mask), 0.0)
    o_cmp = jnp.einsum("bghsc,bcgd->bsghd", p_cmp.astype(vc.dtype), vc)

    nsb = s // SEL_BLOCK
    p_slc = jnp.einsum("bgsc,cj->bgsj", p_cmp.sum(axis=2),
                       jnp.asarray(cmp_to_sel_overlap(nc, nsb)))
    blk = np.arange(nsb)[None, :]
    cur = (pos // SEL_BLOCK)[:, None]
    allowed = blk * SEL_BLOCK <= pos[:, None]
    forced = (blk == 0) | ((blk <= cur) & (blk > cur - N_LOCAL_BLOCKS))
    sel_score = jnp.where(forced, SEL_FORCE, jnp.where(allowed, p_slc, NEG_INF))
    n_sel = min(N_SELECT, nsb)
    _, sel_idx = lax.top_k(sel_score, n_sel)
    ks_blocks = k_sel.reshape(b, nsb, SEL_BLOCK, g, hd).transpose(0, 3, 1, 2, 4).reshape(b, g, nsb, SEL_BLOCK * hd)
    vs_blocks = v_sel.reshape(b, nsb, SEL_BLOCK, g, hd).transpose(0, 3, 1, 2, 4).reshape(b, g, nsb, SEL_BLOCK * hd)
    n_qc = s // SEL_Q_BLOCK
    q_ch = q.reshape(b, n_qc, SEL_Q_BLOCK, g, hpg, hd).transpose(1, 0, 3, 4, 2, 5)
    idx_ch = sel_idx.reshape(b, g, n_qc, SEL_Q_BLOCK, n_sel).transpose(2, 0, 1, 3, 4)
    pos_ch = jnp.arange(s).reshape(n_qc, SEL_Q_BLOCK)
    n_keys = n_sel * SEL_BLOCK

    def sel_chunk(args):
        qc, ic, pc = args
        flat_idx = ic.reshape(b, g, SEL_Q_BLOCK * n_sel)[..., None]
        kg = jnp.take_along_axis(ks_blocks, flat_idx, axis=2).reshape(b, g, SEL_Q_BLOCK, n_keys, hd)
        vg = jnp.take_along_axis(vs_blocks, flat_idx, axis=2).reshape(b, g, SEL_Q_BLOCK, n_keys, hd)
        kpos = (ic[..., None] * SEL_BLOCK + jnp.arange(SEL_BLOCK)).reshape(b, g, SEL_Q_BLOCK, n_keys)
        mask = (kpos <= pc[None, None, :, None])[:, :, None]
        scs = jnp.einsum("bghqd,bgqkd->bghqk", qc, kg).astype(jnp.float32) * scale
        p = masked_softmax(scs, mask)
        return jnp.einsum("bghqk,bgqkd->bghqd", p.astype(vg.dtype), vg)

    o_sel = lax.map(sel_chunk, (q_ch, idx_ch, pos_ch)).transpose(1, 0, 4, 2, 3, 5).reshape(b, s, g, hpg, hd)

    n_wb = s // WIN_Q_BLOCK
    span = WIN_Q_BLOCK + WINDOW
    kp = jnp.pad(k_win, ((0, 0), (WINDOW, 0), (0, 0), (0, 0)))
    vp = jnp.pad(v_win, ((0, 0), (WINDOW, 0), (0, 0), (0, 0)))
    q_wb = q.reshape(b, n_wb, WIN_Q_BLOCK, g, hpg, hd).transpose(1, 0, 3, 4, 2, 5)

    def win_block(args):
        qb, i = args
        start = i * WIN_Q_BLOCK
        kb = lax.dynamic_slice_in_dim(kp, start, span, axis=1)
        vb = lax.dynamic_slice_in_dim(vp, start, span, axis=1)
        qpos = start + jnp.arange(WIN_Q_BLOCK)
        kpos = start - WINDOW + jnp.arange(span)
        mask = ((kpos[None, :] >= 0) & (kpos[None, :] <= qpos[:, None])
                & (kpos[None, :] > qpos[:, None] - WINDOW))
        scs = jnp.einsum("bghqd,bkgd->bghqk", qb, kb).astype(jnp.float32) * scale
        p = masked_softmax(scs, mask)
        return jnp.einsum("bghqk,bkgd->bghqd", p.astype(vb.dtype), vb)

    o_win = lax.map(win_block, (q_wb, jnp.arange(n_wb))).transpose(1, 0, 4, 2, 3, 5).reshape(b, s, g, hpg, hd)

    o = (gates[..., 0:1] * o_cmp + gates[..., 1:2] * o_sel + gates[..., 2:3] * o_win).astype(h.dtype)
    return o.reshape(b, s, NSA_Q_DIM) @ w_out


def ssd_scan(x, dt, a, bm, cm):
    b, s, nh, p = x.shape
    g, n = bm.shape[2], bm.shape[3]
    hg = nh // g
    L = SSD_CHUNK
    nc = s // L
    xc = x.reshape(b, nc, L, g, hg, p).transpose(1, 0, 2, 3, 4, 5)
    dtc = dt.reshape(b, nc, L, g, hg).transpose(1, 0, 2, 3, 4)
    bc = bm.reshape(b, nc, L, g, n).transpose(1, 0, 2, 3, 4)
    cc = cm.reshape(b, nc, L, g, n).transpose(1, 0, 2, 3, 4)
    a_g = a.reshape(g, hg)
    causal = np.tril(np.ones((L, L), dtype=bool))[None, :, :, None, None]

    def step(state, inp):
        x_k, dt_k, b_k, c_k = inp
        acs = jnp.cumsum(dt_k * a_g, axis=1)
        seg = acs[:, :, None] - acs[:, None, :]
        lmat = jnp.exp(jnp.where(causal, seg, -jnp.inf))
        cb = jnp.einsum("blgn,bsgn->blsg", c_k, b_k)
        w = cb[..., None] * lmat * dt_k[:, None]
        y = jnp.einsum("blsgh,bsghp->blghp", w, x_k)
        y = y + jnp.einsum("blgn,bghpn->blghp", c_k, state) * jnp.exp(acs)[..., None]
        decay = jnp.exp(acs[:, -1:] - acs) * dt_k
        state = (state * jnp.exp(acs[:, -1])[..., None, None]
                 + jnp.einsum("bsgn,bsgh,bsghp->bghpn", b_k, decay, x_k))
        return state, y

    state0 = jnp.zeros((b, g, hg, p, n), jnp.float32)
    _, ys = lax.scan(step, state0, (xc, dtc, bc, cc))
    return ys.transpose(1, 0, 2, 3, 4, 5).reshape(b, s, nh, p)


def ssd_mixer(h, w_in, conv_w, conv_b, dt_bias, a_log, d_skip, norm_w, w_out):
    b, s, _ = h.shape
    f32 = jnp.float32
    gn = SSD_GROUPS * SSD_STATE
    proj = h @ w_in
    z = proj[..., :SSD_D_INNER]
    xbc = jax.nn.silu(causal_dwconv(proj[..., SSD_D_INNER:SSD_D_INNER + SSD_CONV_DIM], conv_w, conv_b))
    dt_raw = proj[..., SSD_D_INNER + SSD_CONV_DIM:]
    x_ssm = xbc[..., :SSD_D_INNER].astype(f32).reshape(b, s, SSD_HEADS, SSD_HEAD_DIM)
    bm = xbc[..., SSD_D_INNER:SSD_D_INNER + gn].astype(f32).reshape(b, s, SSD_GROUPS, SSD_STATE)
    cm = xbc[..., SSD_D_INNER + gn:].astype(f32).reshape(b, s, SSD_GROUPS, SSD_STATE)
    dt = jax.nn.softplus(dt_raw.astype(f32) + dt_bias.astype(f32))
    a = -jnp.exp(a_log.astype(f32))
    y = ssd_scan(x_ssm, dt, a, bm, cm) + d_skip.astype(f32)[:, None] * x_ssm
    y = y.reshape(b, s, SSD_D_INNER) * jax.nn.silu(z.astype(f32))
    yg = y.reshape(b, s, SSD_GROUPS, SSD_D_INNER // SSD_GROUPS)
    yg = yg * lax.rsqrt(jnp.mean(yg * yg, axis=-1, keepdims=True) + RMS_EPS)
    y = (yg.reshape(b, s, SSD_D_INNER) * norm_w.astype(f32)).astype(h.dtype)
    return y @ w_out


def conv_ffn(h, w_up, conv_w, conv_b, w_down):
    u = causal_dwconv(h @ w_up, conv_w, conv_b)
    val, gate = u[..., :FFN_HIDDEN], u[..., FFN_HIDDEN:]
    return (jax.nn.silu(gate) * val) @ w_down


def setup_inputs(seed: int = 0) -> dict:
    key = jax.random.key(seed)
    ks = jax.random.split(key, 19)
    f32 = jnp.float32

    def dense(k, shape, fan_in):
        return jax.random.normal(k, shape, f32) * fan_in ** -0.5

    x = jax.random.normal(ks[0], (BATCH, SEQ, D_MODEL), f32)
    norm_gains = 1.0 + 0.05 * jax.random.normal(ks[1], (DEPTH, 4, D_MODEL), f32)
    nsa_w_in = dense(ks[2], (N_NSA_LAYERS, D_MODEL, NSA_PROJ_DIM), D_MODEL)
    nsa_cmp_pos = 0.1 * jax.random.normal(ks[3], (N_NSA_LAYERS, 2, COMP_BLOCK, NSA_HEAD_DIM), f32)
    nsa_cmp_w1 = dense(ks[4], (N_NSA_LAYERS, 2, COMP_BLOCK * NSA_HEAD_DIM, COMP_HIDDEN), COMP_BLOCK * NSA_HEAD_DIM)
    nsa_cmp_w2 = dense(ks[5], (N_NSA_LAYERS, 2, COMP_HIDDEN, NSA_HEAD_DIM), COMP_HIDDEN)
    nsa_w_out = dense(ks[6], (N_NSA_LAYERS, NSA_Q_DIM, D_MODEL), NSA_Q_DIM)
    ssd_w_in = dense(ks[7], (N_SSD_LAYERS, D_MODEL, SSD_PROJ_DIM), D_MODEL)
    ssd_conv_w = dense(ks[8], (N_SSD_LAYERS, SSD_CONV, SSD_CONV_DIM), SSD_CONV)
    ssd_conv_b = 0.02 * jax.random.normal(ks[9], (N_SSD_LAYERS, SSD_CONV_DIM), f32)
    dt0 = jnp.exp(jax.random.uniform(ks[10], (N_SSD_LAYERS, SSD_HEADS), f32,
                                     minval=math.log(1e-3), maxval=math.log(1e-1)))
    ssd_dt_bias = dt0 + jnp.log(-jnp.expm1(-dt0))
    ssd_a_log = jnp.log(jax.random.uniform(ks[11], (N_SSD_LAYERS, SSD_HEADS), f32, minval=1.0, maxval=16.0))
    ssd_d = 1.0 + 0.05 * jax.random.normal(ks[12], (N_SSD_LAYERS, SSD_HEADS), f32)
    ssd_norm_w = 1.0 + 0.05 * jax.random.normal(ks[13], (N_SSD_LAYERS, SSD_D_INNER), f32)
    ssd_w_out = dense(ks[14], (N_SSD_LAYERS, SSD_D_INNER, D_MODEL), SSD_D_INNER)
    ffn_w_up = dense(ks[15], (DEPTH, D_MODEL, 2 * FFN_HIDDEN), D_MODEL)
    ffn_conv_w = dense(ks[16], (DEPTH, FFN_CONV, 2 * FFN_HIDDEN), FFN_CONV)
    ffn_conv_b = 0.02 * jax.random.normal(ks[17], (DEPTH, 2 * FFN_HIDDEN), f32)
    ffn_w_down = dense(ks[18], (DEPTH, FFN_HIDDEN, D_MODEL), FFN_HIDDEN)
    return {
        "x": x, "norm_gains": norm_gains,
        "nsa_w_in": nsa_w_in, "nsa_cmp_pos": nsa_cmp_pos, "nsa_cmp_w1": nsa_cmp_w1,
        "nsa_cmp_w2": nsa_cmp_w2, "nsa_w_out": nsa_w_out,
        "ssd_w_in": ssd_w_in, "ssd_conv_w": ssd_conv_w, "ssd_conv_b": ssd_conv_b,
        "ssd_dt_bias": ssd_dt_bias, "ssd_a_log": ssd_a_log, "ssd_d": ssd_d,
        "ssd_norm_w": ssd_norm_w, "ssd_w_out": ssd_w_out,
        "ffn_w_up": ffn_w_up, "ffn_conv_w": ffn_conv_w, "ffn_conv_b": ffn_conv_b,
        "ffn_w_down": ffn_w_down,
    }


def reference(x, norm_gains, nsa_w_in, nsa_cmp_pos, nsa_cmp_w1, nsa_cmp_w2, nsa_w_out,
              ssd_w_in, ssd_conv_w, ssd_conv_b, ssd_dt_bias, ssd_a_log, ssd_d, ssd_norm_w,
              ssd_w_out, ffn_w_up, ffn_conv_w, ffn_conv_b, ffn_w_down):
    h = x
    for i in range(DEPTH):
        gains = norm_gains[i]
        slot = i // N_MIXERS
        hn = rms_norm(h, gains[0])
        if i % N_MIXERS == 0:
            m = nsa_mixer(hn, nsa_w_in[slot], nsa_cmp_pos[slot], nsa_cmp_w1[slot],
                          nsa_cmp_w2[slot], nsa_w_out[slot])
        else:
            m = ssd_mixer(hn, ssd_w_in[slot], ssd_conv_w[slot], ssd_conv_b[slot],
                          ssd_dt_bias[slot], ssd_a_log[slot], ssd_d[slot],
                          ssd_norm_w[slot], ssd_w_out[slot])
        h = h + rms_norm(m, gains[1])
        f = conv_ffn(rms_norm(h, gains[2]), ffn_w_up[i], ffn_conv_w[i], ffn_conv_b[i], ffn_w_down[i])
        h = h + rms_norm(f, gains[3])
    return h
```

```python
import math
from contextlib import ExitStack

import numpy as np
import concourse.bass as bass
import concourse.mybir as mybir
from concourse.bass_utils import run_bass_kernel_spmd

F32 = mybir.dt.float32
BF16 = mybir.dt.bfloat16
AF = mybir.ActivationFunctionType
ALU = mybir.AluOpType
AX = mybir.AxisListType

D_MODEL = 1024
SEQ = 2048
N_CORES = 8
FFN_HIDDEN = 2816
RMS_EPS = 1e-6

WRITE_KEYS = ("out", "accum_out", "ap")
DT_SIZE = {F32: 4, BF16: 2}


def _box(ap):
    t = ap.tensor
    tn = type(t).__name__
    off = ap.offset
    dims = list(ap.ap)
    if tn.startswith("SB") or tn.startswith("PSum"):
        pstep = 1
        for s in list(t.shape)[1:]:
            pstep *= int(s)
        p0 = off // pstep
        f0 = off % pstep
        st0, cn0 = dims[0]
        pe = (cn0 - 1) * (st0 // pstep) if cn0 > 1 else 0
        fe = 0
        for st, cn in dims[1:]:
            if cn > 1:
                assert st >= 0
                fe += (cn - 1) * st
        f1 = f0 + fe + 1
        if tn.startswith("PSum"):
            be = 2048 // DT_SIZE[t.dtype]
            f0 = (f0 // be) * be
            f1 = -(-f1 // be) * be
        return (p0, p0 + pe + 1, f0, f1)
    lo = hi = off
    for st, cn in dims:
        if cn > 1:
            if st >= 0:
                hi += (cn - 1) * st
            else:
                lo += (cn - 1) * st
    return (0, 1, lo, hi + 1)


class Prog:
    def __init__(self, nc, n_dma_sems=10, same_engine_sync=True):
        self.nc = nc
        self.e = dict(pe=nc.tensor, act=nc.scalar, dve=nc.vector, pool=nc.gpsimd, sp=nc.sync)
        self.sem = {}
        self.cnt = {}
        self.allsems = []
        for k in self.e:
            self._new_sem(k)
        self.seen = {k: {} for k in self.e}
        self.acc = {}
        self.nodep = set()
        self.dsems = {}
        self.dnext = {}
        self.dval = {}
        self.semh = {}
        for q in ("sp", "act", "pool"):
            self.dsems[q] = []
            for i in range(n_dma_sems):
                s = nc.alloc_semaphore(name=f"d_{q}_{i}")
                self.dsems[q].append(s)
                self.dval[s.num] = 0
                self.semh[s.num] = s
            self.dnext[q] = 0
        self.same = same_engine_sync
        self.ninst = 0
        self.nwait = 0

    def _new_sem(self, k):
        s = self.nc.alloc_semaphore(name=f"s_{k}_{len(self.allsems)}")
        self.sem[k] = s
        self.cnt[k] = 0
        self.allsems.append(s)

    def _wait(self, ek, sem, val):
        sn = self.seen[ek]
        if sn.get(sem.num, 0) >= val:
            return
        self.e[ek].wait_ge(sem, val)
        sn[sem.num] = val
        self.nwait += 1

    def _sync(self, ek, reads, writes):
        need = {}
        for is_w, aps in ((False, reads), (True, writes)):
            for ap in aps:
                name = ap.tensor.name
                if name in self.nodep:
                    continue
                b = _box(ap)
                is_psum = type(ap.tensor).__name__.startswith("PSum")
                for r in self.acc.get(name, ()):
                    if not (is_w or r[0]):
                        if not (is_psum and r[7] != ek):
                            continue
                    if r[1] < b[1] and b[0] < r[2] and r[3] < b[3] and b[2] < r[4]:
                        if r[7] == ek and (ek == "pe" or not self.same) and r[7] != "dma":
                            continue
                        s, v = r[5], r[6]
                        if need.get(s.num, (None, 0))[1] < v:
                            need[s.num] = (s, v)
        for s, v in need.values():
            self._wait(ek, s, v)

    def _record(self, ek, reads, writes, sem, val, is_dma=False):
        tag = "dma" if is_dma else ek
        for is_w, aps in ((False, reads), (True, writes)):
            for ap in aps:
                name = ap.tensor.name
                if name in self.nodep:
                    continue
                b = _box(ap)
                lst = self.acc.setdefault(name, [])
                keep = []
                for r in lst:
                    inside = r[1] >= b[0] and r[2] <= b[1] and r[3] >= b[2] and r[4] <= b[3]
                    if inside and (is_w or ((not r[0]) and r[5].num == sem.num)):
                        continue
                    keep.append(r)
                keep.append((is_w, b[0], b[1], b[2], b[3], sem, val, tag))
                self.acc[name] = keep

    def I(self, ek, name, **kw):
        reads, writes = [], []
        for k, v in kw.items():
            if isinstance(v, bass.AP):
                (writes if k in WRITE_KEYS else reads).append(v)
        self._sync(ek, reads, writes)
        ins = getattr(self.e[ek], name)(**kw)
        self.cnt[ek] += 1
        ins.then_inc(self.sem[ek], 1)
        self._record(ek, reads, writes, self.sem[ek], self.cnt[ek])
        self.ninst += 1
        if self.cnt[ek] >= 30000:
            self._new_sem(ek)
        return ins

    def dma(self, q, out, in_, **kw):
        reads, writes = [in_], [out]
        self._sync(q, reads, writes)
        lst = self.dsems[q]
        s = lst[self.dnext[q] % len(lst)]
        self.dnext[q] += 1
        prev = self.dval[s.num]
        if prev:
            self._wait(q, s, prev)
        ins = self.e[q].dma_start(out=out, in_=in_, **kw)
        ins.then_inc(s, 16)
        self.dval[s.num] = prev + 16
        self._record(q, reads, writes, s, prev + 16, is_dma=True)
        self.ninst += 1
        return ins

    def barrier(self, engines=None):
        for x in (engines or self.e):
            for k in self.e:
                if self.cnt[k] and not (k == x):
                    self._wait(x, self.sem[k], self.cnt[k])
            for n, v in self.dval.items():
                if v:
                    self._wait(x, self.semh[n], v)
        self.acc = {}

    def mm(self, out, lhsT, rhs, start=True, stop=True, **kw):
        return self.I("pe", "matmul", out=out, lhsT=lhsT, rhs=rhs, start=start, stop=stop, **kw)

    def tr(self, out, in_, identity):
        return self.I("pe", "transpose", out=out, in_=in_, identity=identity)

    def act(self, out, in_, func, **kw):
        return self.I("act", "activation", out=out, in_=in_, func=func, **kw)

    def ts(self, ek, out, in0, s1, op0, s2=None, op1=None, **kw):
        if op1 is None:
            s2, op1 = 0.0, ALU.add
        return self.I(ek, "tensor_scalar", out=out, in0=in0, scalar1=s1, scalar2=s2, op0=op0, op1=op1, **kw)

    def tt(self, ek, out, in0, in1, op):
        return self.I(ek, "tensor_tensor", out=out, in0=in0, in1=in1, op=op)

    def stt(self, ek, out, in0, scalar, in1, op0, op1):
        return self.I(ek, "scalar_tensor_tensor", out=out, in0=in0, scalar=scalar, in1=in1, op0=op0, op1=op1)

    def copy(self, ek, out, in_):
        if ek == "act":
            return self.I("act", "activation", out=out, in_=in_, func=AF.Copy)
        return self.I(ek, "tensor_copy", out=out, in_=in_)

    def memset(self, ek, ap, val):
        return self.I(ek, "memset", ap=ap, constant=val)


class Ctx:
    def __init__(self, nc, P):
        self.nc = nc
        self.P = P
        self.uid = 0

    def name(self, base):
        self.uid += 1
        return f"{base}_{self.uid}"


def sb(C, es, base, shape, dtype):
    return es.enter_context(C.nc.sbuf_tensor(C.name(base), shape, dtype))


def ps(C, es, base, shape, dtype=F32):
    return es.enter_context(C.nc.psum_tensor(C.name(base), shape, dtype))


def bcast_rows(ap_row, nparts):
    return ap_row.partition_broadcast(nparts)


def emit_consts(C, es):
    P = C.P
    ident_f = sb(C, es, "identf", [128, 128], F32)
    ident = sb(C, es, "ident", [128, 128], BF16)
    P.memset("pool", ident_f[:], 0.0)
    P.I("pool", "affine_select", out=ident_f[:], in_=ident_f[:], pattern=[[-1, 128]],
        compare_op=ALU.not_equal, fill=1.0, base=0, channel_multiplier=1)
    P.copy("dve", ident[:], ident_f[:])
    C.ident = ident
    C.ident_f = ident_f


def rstd_from_ss(C, rstd, ss, width):
    C.P.act(rstd, ss, AF.Sqrt, scale=1.0 / width, bias=RMS_EPS)
    C.P.I("dve", "reciprocal", out=rstd, in_=rstd)


def rmsnorm_rows(C, x_ap, gain_bc, out_ap, ss, rstd, junk, width=D_MODEL, eng="dve"):
    P = C.P
    P.act(junk, x_ap, AF.Square, accum_out=ss)
    rstd_from_ss(C, rstd, ss, width)
    P.stt(eng, out_ap, x_ap, rstd, gain_bc, ALU.mult, ALU.mult)


def transpose_rows(C, hb, pt, hT, c0, kc, gain=None, ev="dve"):
    P = C.P
    for k0 in range(0, kc, 8):
        kn = min(8, kc - k0)
        for k in range(kn):
            P.tr(pt[:, k, :], hb[:, (k0 + k) * 128:(k0 + k + 1) * 128], C.ident[:])
        dst = hT[:, k0:k0 + kn, c0:c0 + 128]
        if gain is None:
            P.copy(ev, dst, pt[:, 0:kn, :])
        elif ev == "dve":
            P.tt("dve", dst, pt[:, 0:kn, :], gain[:, 0, k0:k0 + kn].unsqueeze(2).broadcast_to([128, kn, 128]), ALU.mult)
        else:
            for k in range(kn):
                P.act(hT[:, k0 + k, c0:c0 + 128], pt[:, k, :], AF.Identity, scale=gain[:, 0, k0 + k:k0 + k + 1])


def load_cast(C, dst, w_dram_rows):
    C.P.dma("pool", dst, w_dram_rows)


def load_featmajor_params(C, es, rows, nchunk, name):
    P = C.P
    nr = len(rows)
    out = sb(C, es, name, [128, nr, nchunk], F32)
    with ExitStack() as es2:
        raw = sb(C, es2, name + "_raw", [nchunk, nr, 128], F32)
        pp = ps(C, es2, name + "_ps", [128, nchunk], F32)
        for j, r in enumerate(rows):
            P.dma("sp", raw[:, j, :], r.rearrange("(m p) -> m p", p=128))
        for j in range(nr):
            P.tr(pp[:, :], raw[:, j, :], C.ident_f[0:nchunk, 0:nchunk])
            P.copy("dve", out[:, j, :], pp[:, :])
        P.barrier()
    return out


def emit_ffn(C, h_in, h_out, fpart, g_pre, g_post, w_up, conv_w, conv_b, w_down, ntok, seq=SEQ):
    P = C.P
    F = FFN_HIDDEN
    NM = F // 128
    NS = 2
    MH = NM // NS
    KC = D_MODEL // 128
    TG = 512
    with ExitStack() as es:
        cwb = load_featmajor_params(C, es, [conv_w[0], conv_w[1], conv_w[2], conv_b], 2 * NM, "cwb")
        gpre = load_featmajor_params(C, es, [g_pre], KC, "gpre")
        wu = sb(C, es, "wu", [128, KC, 2 * MH * 128], BF16)
        wd = sb(C, es, "wd", [128, MH, D_MODEL], BF16)
        gpost = sb(C, es, "gpost", [128, D_MODEL], F32)
        xt = [sb(C, es, "xt", [128, D_MODEL], F32) for _ in range(6)]
        hn = [sb(C, es, "hn", [128, D_MODEL], BF16) for _ in range(2)]
        hnT = [sb(C, es, "hnT", [128, KC, TG], BF16) for _ in range(2)]
        gT = [sb(C, es, "gT", [128, MH, TG], BF16) for _ in range(2)]
        uv = [sb(C, es, "uv", [128, TG], F32) for _ in range(2)]
        ug = [sb(C, es, "ug", [128, TG], F32) for _ in range(2)]
        halo = [sb(C, es, "halo", [128, 2 * NM, 2], F32) for _ in range(2)]
        junk = sb(C, es, "junk", [128, D_MODEL], BF16)
        ss = [sb(C, es, "ss", [128, 1], F32) for _ in range(4)]
        rstd = [sb(C, es, "rstd", [128, 1], F32) for _ in range(4)]
        ft = [sb(C, es, "ft", [128, D_MODEL], F32) for _ in range(3)]
        pT = [ps(C, es, "pT", [128, 8, 128], BF16) for _ in range(2)]
        pY = [ps(C, es, "pY", [128, TG], F32) for _ in range(4)]
        pO = ps(C, es, "pO", [128, D_MODEL], F32)

        P.dma("sp", gpost[:], g_post.partition_broadcast(128))
        ngroups = ntok // TG
        xi = 0
        fi = 0
        for s in range(NS):
            for k in range(KC):
                rows = w_up[k * 128:(k + 1) * 128, :]
                load_cast(C, wu[:, k, 0:MH * 128], rows[:, s * MH * 128:(s + 1) * MH * 128])
                load_cast(C, wu[:, k, MH * 128:2 * MH * 128], rows[:, F + s * MH * 128:F + (s + 1) * MH * 128])
            for m in range(MH):
                r0 = (s * MH + m) * 128
                load_cast(C, wd[:, m, :], w_down[r0:r0 + 128, :])
            for g in range(ngroups):
                seq_start = (g * TG) % seq == 0
                hT = hnT[g % 2]
                xts = []
                for t in range(TG // 128):
                    x = xt[xi % len(xt)]
                    xi += 1
                    xts.append(x)
                    r0 = g * TG + t * 128
                    P.dma("sp", x[:], h_in[r0:r0 + 128, :])
                    hb = hn[t % 2]
                    sq, rs = ss[t % 4], rstd[t % 4]
                    P.act(junk[:], x[:], AF.Square, accum_out=sq[:])
                    rstd_from_ss(C, rs[:], sq[:], D_MODEL)
                    P.ts("dve", hb[:], x[:], rs[:], ALU.mult)
                    transpose_rows(C, hb, pT[t % 2], hT, t * 128, KC, gpre, ev=("dve" if t % 2 else "act"))
                G = gT[g % 2]
                for m in range(MH):
                    yv = pY[(2 * m) % 4]
                    yg = pY[(2 * m + 1) % 4]
                    cv = s * MH + m
                    for (y, c0) in ((yv, m * 128), (yg, (MH + m) * 128)):
                        for k in range(KC):
                            P.mm(y[:], wu[:, k, c0:c0 + 128], hT[:, k, :], start=(k == 0), stop=(k == KC - 1))
                    for (y, u, ch) in ((yv, uv[m % 2], cv), (yg, ug[m % 2], NM + cv)):
                        P.act(u[:], y[:], AF.Identity, scale=cwb[:, 2, ch:ch + 1], bias=cwb[:, 3, ch:ch + 1])
                        P.copy("act", halo[g % 2][:, ch, :], y[:, TG - 2:TG])
                        P.stt("dve", u[:, 1:TG], y[:, 0:TG - 1], cwb[:, 1, ch:ch + 1], u[:, 1:TG], ALU.mult, ALU.add)
                        P.stt("dve", u[:, 2:TG], y[:, 0:TG - 2], cwb[:, 0, ch:ch + 1], u[:, 2:TG], ALU.mult, ALU.add)
                        if not seq_start:
                            hp = halo[(g + 1) % 2]
                            P.stt("dve", u[:, 0:2], hp[:, ch, :], cwb[:, 0, ch:ch + 1], u[:, 0:2], ALU.mult, ALU.add)
                            P.stt("dve", u[:, 0:1], hp[:, ch, 1:2], cwb[:, 1, ch:ch + 1], u[:, 0:1], ALU.mult, ALU.add)
                    P.act(ug[m % 2][:], ug[m % 2][:], AF.Silu)
                    P.tt("pool", G[:, m, :], ug[m % 2][:], uv[m % 2][:], ALU.mult)
                for t in range(TG // 128):
                    for half in range(2):
                        for m in range(MH):
                            P.mm(pO[:, half * 512:(half + 1) * 512], G[:, m, t * 128:(t + 1) * 128],
                                 wd[:, m, half * 512:(half + 1) * 512], start=(m == 0), stop=(m == MH - 1))
                    r0 = g * TG + t * 128
                    f = ft[fi % len(ft)]
                    fi += 1
                    if s == 0:
                        P.copy("act", f[:], pO[:])
                        P.dma("sp", fpart[r0:r0 + 128, :], f[:])
                    else:
                        P.dma("sp", f[:], fpart[r0:r0 + 128, :])
                        P.tt("dve", f[:], pO[:], f[:], ALU.add)
                        sq, rs = ss[t % 4], rstd[t % 4]
                        P.act(junk[:], f[:], AF.Square, accum_out=sq[:])
                        rstd_from_ss(C, rs[:], sq[:], D_MODEL)
                        P.stt("dve", f[:], f[:], rs[:], gpost[:], ALU.mult, ALU.mult)
                        P.tt("pool", f[:], f[:], xts[t][:], ALU.add)
                        P.dma("sp", h_out[r0:r0 + 128, :], f[:])
        P.barrier()


SSD_DI = 2048
SSD_H = 32
SSD_P = 64
SSD_G = 4
SSD_N = 128
SSD_CONVD = SSD_DI + 2 * SSD_G * SSD_N


def emit_ssd_a(C, h_in, y_pre, g_pre, w_in, conv_w, conv_b, dt_bias, a_log, d_skip, ntok, seq=SEQ):
    P = C.P
    KC = D_MODEL // 128
    TG = 512
    NT = TG // 128
    NFC = SSD_CONVD // 128
    XO = SSD_DI
    NW = SSD_CONVD + SSD_H
    with ExitStack() as es:
        cwb = load_featmajor_params(C, es, [conv_w[0], conv_w[1], conv_w[2], conv_w[3], conv_b], NFC, "scwb")
        gpre = load_featmajor_params(C, es, [g_pre], KC, "sgpre")
        wx = sb(C, es, "wx", [128, KC, NW], BF16)
        for k in range(KC):
            load_cast(C, wx[:, k, :], w_in[k * 128:(k + 1) * 128, XO:XO + NW])
        dtb = sb(C, es, "dtb", [128, SSD_H], F32)
        a_bc = sb(C, es, "a_bc", [128, SSD_H], F32)
        d_bc = sb(C, es, "d_bc", [128, SSD_H], F32)
        P.dma("sp", dtb[:], dt_bias.partition_broadcast(128))
        P.dma("sp", a_bc[:], a_log.partition_broadcast(128))
        P.dma("sp", d_bc[:], d_skip.partition_broadcast(128))
        P.act(a_bc[:], a_bc[:], AF.Exp)
        P.ts("dve", a_bc[:], a_bc[:], -1.0, ALU.mult)
        Mf = sb(C, es, "Mf", [128, 128], F32)
        Uf = sb(C, es, "Uf", [128, 128], F32)
        onesf = sb(C, es, "onesf", [128, 128], F32)
        P.memset("pool", onesf[:], 1.0)
        P.I("pool", "affine_select", out=Mf[:], in_=onesf[:], pattern=[[1, 128]], compare_op=ALU.is_ge, fill=0.0,
            base=0, channel_multiplier=-1)
        P.I("pool", "affine_select", out=Uf[:], in_=onesf[:], pattern=[[-1, 128]], compare_op=ALU.is_gt, fill=0.0,
            base=0, channel_multiplier=1)

        xt = [sb(C, es, "sxt", [128, D_MODEL], F32) for _ in range(2)]
        hn = [sb(C, es, "shn", [128, D_MODEL], BF16) for _ in range(2)]
        junk = sb(C, es, "sjunk", [128, D_MODEL], BF16)
        ss = [sb(C, es, "sss", [128, 1], F32) for _ in range(2)]
        rstd = [sb(C, es, "srs", [128, 1], F32) for _ in range(2)]
        hnT = sb(C, es, "shnT", [128, KC, TG], BF16)
        dt_all = sb(C, es, "dt_all", [128, NT, SSD_H], F32)
        dtA_all = sb(C, es, "dtA_all", [128, NT, SSD_H], F32)
        dtmp = sb(C, es, "dtmp", [128, SSD_H], F32)
        u = [sb(C, es, "su", [128, TG], F32) for _ in range(2)]
        xs = [sb(C, es, "sxs", [128, TG], BF16) for _ in range(2)]
        halo = [sb(C, es, "shalo", [128, NFC, 3], F32) for _ in range(2)]
        x_tok = sb(C, es, "x_tok", [128, NT, SSD_DI], BF16)
        B_tok = sb(C, es, "B_tok", [128, NT, SSD_G * SSD_N], BF16)
        BT = sb(C, es, "BT", [128, SSD_G, TG], BF16)
        CT = sb(C, es, "CT", [128, SSD_G, TG], BF16)
        state = sb(C, es, "state", [128, SSD_DI], F32)
        state_bf = sb(C, es, "state_bf", [128, SSD_DI], BF16)
        Rg = [sb(C, es, "Rg", [128, 8, 128], F32) for _ in range(2)]
        lmT = [sb(C, es, "lmT", [128, 8, 128], F32) for _ in range(2)]
        wT = [sb(C, es, "wT", [128, 8, 128], BF16) for _ in range(2)]
        cbm = [sb(C, es, "cbm", [128, 128], F32) for _ in range(2)]
        xd = sb(C, es, "xd", [128, SSD_DI], BF16)
        xdsk = sb(C, es, "xdsk", [128, SSD_DI], F32)
        xdec = sb(C, es, "xdec", [128, SSD_DI], BF16)
        tmpg = [sb(C, es, "tmpg", [128, 512], F32) for _ in range(2)]
        y_sb = sb(C, es, "y_sb", [128, SSD_DI], F32)
        acs_sb = sb(C, es, "acs_sb", [128, SSD_H], F32)
        E_sb = sb(C, es, "E_sb", [128, SSD_H], F32)
        dec = sb(C, es, "dec", [128, SSD_H], F32)
        Etot = sb(C, es, "Etot", [128, SSD_H], F32)

        pT = ps(C, es, "spT", [128, 8, 128], BF16)
        pY = [ps(C, es, "spY", [128, TG], F32) for _ in range(2)]
        pS = ps(C, es, "spS", [128, 8, 128], F32)
        pC = ps(C, es, "spC", [128, 512], F32)
        pD = ps(C, es, "spD", [128, 512], F32)
        pD2 = ps(C, es, "spD2", [128, 512], F32)

        def bc_h(ap_h, n):
            return ap_h.unsqueeze(2).broadcast_to([128, ap_h.shape[1], n])

        ngroups = ntok // TG
        for g in range(ngroups):
            seq_start = (g * TG) % seq == 0
            for t in range(NT):
                x = xt[t % 2]
                r0 = g * TG + t * 128
                P.dma("sp", x[:], h_in[r0:r0 + 128, :])
                hb = hn[t % 2]
                P.act(junk[:], x[:], AF.Square, accum_out=ss[t % 2][:])
                rstd_from_ss(C, rstd[t % 2][:], ss[t % 2][:], D_MODEL)
                P.ts("pool", hb[:], x[:], rstd[t % 2][:], ALU.mult)
                transpose_rows(C, hb, pT, hnT, t * 128, KC, gpre, ev=("dve" if t % 2 else "act"))
            for t in range(NT):
                for k in range(KC):
                    P.mm(pD[:, 0:SSD_H], hnT[:, k, t * 128:(t + 1) * 128], wx[:, k, SSD_CONVD:NW],
                         start=(k == 0), stop=(k == KC - 1))
                P.tt("dve", dtmp[:], pD[:, 0:SSD_H], dtb[:], ALU.add)
                P.act(dtmp[:], dtmp[:], AF.Exp)
                P.act(dt_all[:, t, :], dtmp[:], AF.Ln, bias=1.0)
                P.tt("dve", dtA_all[:, t, :], dt_all[:, t, :], a_bc[:], ALU.mult)
            for fc in range(NFC):
                y = pY[fc % 2]
                for k in range(KC):
                    P.mm(y[:], wx[:, k, fc * 128:(fc + 1) * 128], hnT[:, k, :], start=(k == 0), stop=(k == KC - 1))
                uu = u[fc % 2]
                P.act(uu[:], y[:], AF.Identity, scale=cwb[:, 3, fc:fc + 1], bias=cwb[:, 4, fc:fc + 1])
                P.copy("act", halo[g % 2][:, fc, :], y[:, TG - 3:TG])
                for j in range(1, 4):
                    P.stt("dve", uu[:, j:TG], y[:, 0:TG - j], cwb[:, 3 - j, fc:fc + 1], uu[:, j:TG], ALU.mult, ALU.add)
                if not seq_start:
                    hp = halo[(g + 1) % 2]
                    for j in range(1, 4):
                        P.stt("dve", uu[:, 0:j], hp[:, fc, 3 - j:3], cwb[:, 3 - j, fc:fc + 1], uu[:, 0:j], ALU.mult, ALU.add)
                if fc < 16:
                    xx = xs[fc % 2]
                    P.act(xx[:], uu[:], AF.Silu)
                    for t in range(NT):
                        P.tr(pT[:, t, :], xx[:, t * 128:(t + 1) * 128], C.ident[:])
                    P.copy("dve" if fc % 2 else "act", x_tok[:, :, fc * 128:(fc + 1) * 128], pT[:, 0:NT, :])
                elif fc < 20:
                    P.act(BT[:, fc - 16, :], uu[:], AF.Silu)
                    for t in range(NT):
                        P.tr(pT[:, t, :], BT[:, fc - 16, t * 128:(t + 1) * 128], C.ident[:])
                    P.copy("dve" if fc % 2 else "act", B_tok[:, :, (fc - 16) * 128:(fc - 15) * 128], pT[:, 0:NT, :])
                else:
                    P.act(CT[:, fc - 20, :], uu[:], AF.Silu)
            for t in range(NT):
                first = seq_start and t == 0
                r0 = g * TG + t * 128
                c0 = t * 128
                dtA = dtA_all[:, t, :]
                dt = dt_all[:, t, :]
                xv = x_tok[:, t, :].rearrange("p (h d) -> p h d", d=SSD_P)
                if first:
                    P.memset("pool", state[:], 0.0)
                    P.memset("pool", state_bf[:], 0.0)
                P.mm(pD[:, 0:SSD_H], Mf[:], dtA, start=True, stop=True)
                P.mm(pD2[:, 0:SSD_H], onesf[:], dtA, start=True, stop=True)
                P.copy("act", acs_sb[:], pD[:, 0:SSD_H])
                P.act(E_sb[:], pD[:, 0:SSD_H], AF.Exp)
                P.tt("dve", dec[:], pD2[:, 0:SSD_H], acs_sb[:], ALU.subtract)
                P.act(dec[:], dec[:], AF.Exp)
                P.tt("dve", dec[:], dec[:], dt, ALU.mult)
                P.act(Etot[:], pD2[:, 0:SSD_H], AF.Exp)
                P.tt("pool", xd[:].rearrange("p (h d) -> p h d", d=SSD_P), xv, bc_h(dt, SSD_P), ALU.mult)
                P.tt("pool", xdsk[:].rearrange("p (h d) -> p h d", d=SSD_P), xv, bc_h(d_bc[:], SSD_P), ALU.mult)
                P.tt("pool", xdec[:].rearrange("p (h d) -> p h d", d=SSD_P), xv, bc_h(dec[:], SSD_P), ALU.mult)
                for gg in range(SSD_G):
                    P.mm(pC[:, gg * 128:(gg + 1) * 128], BT[:, gg, c0:c0 + 128], CT[:, gg, c0:c0 + 128], start=True, stop=True)
                for gg in range(SSD_G):
                    R = Rg[gg % 2]
                    hs = slice(gg * 8, (gg + 1) * 8)
                    P.tt("pool", R[:], Mf[:].unsqueeze(1).broadcast_to([128, 8, 128]), bc_h(dtA_all[:, t, hs], 128), ALU.mult)
                    for half in range(2):
                        P.mm(pS[:, half * 4:(half + 1) * 4, :], Uf[:], R[:, half * 4:(half + 1) * 4, :], start=True, stop=True)
                    lm = lmT[gg % 2]
                    P.act(lm[:], pS[:], AF.Exp)
                    cm = cbm[gg % 2]
                    P.tt("dve", cm[:], pC[:, gg * 128:(gg + 1) * 128], Mf[:], ALU.mult)
                    w = wT[gg % 2]
                    P.tt("dve", w[:], lm[:], cm[:].unsqueeze(1).broadcast_to([128, 8, 128]), ALU.mult)
                    yi = pY[0]
                    for hg in range(8):
                        hh = gg * 8 + hg
                        P.mm(yi[:, hg * 64:(hg + 1) * 64], w[:, hg, :], xd[:, hh * 64:(hh + 1) * 64], start=True, stop=True)
                    cs = slice(gg * 512, (gg + 1) * 512)
                    if first:
                        P.tt("dve", y_sb[:, cs], yi[:], xdsk[:, cs], ALU.add)
                    else:
                        yo = pY[1]
                        P.mm(yo[:], CT[:, gg, c0:c0 + 128], state_bf[:, cs], start=True, stop=True)
                        tg_ = tmpg[gg % 2]
                        P.tt("dve", tg_[:].rearrange("p (h d) -> p h d", d=SSD_P), yo[:].rearrange("p (h d) -> p h d", d=SSD_P),
                             bc_h(E_sb[:, hs], SSD_P), ALU.mult)
                        P.tt("pool", tg_[:], tg_[:], xdsk[:, cs], ALU.add)
                        P.tt("dve", y_sb[:, cs], yi[:], tg_[:], ALU.add)
                    P.mm(pD[:], B_tok[:, t, gg * 128:(gg + 1) * 128], xdec[:, cs], start=True, stop=True)
                    if first:
                        P.copy("act", state[:, cs], pD[:])
                    else:
                        P.tt("pool", state[:, cs].rearrange("p (h d) -> p h d", d=SSD_P),
                             state[:, cs].rearrange("p (h d) -> p h d", d=SSD_P), bc_h(Etot[:, hs], SSD_P), ALU.mult)
                        P.tt("dve", state[:, cs], pD[:], state[:, cs], ALU.add)
                    P.copy("act", state_bf[:, cs], state[:, cs])
                P.dma("sp", y_pre[r0:r0 + 128, :], y_sb[:])
        P.barrier()


def emit_out(C, mode, h_in, h_out, src, w_out, g_post, ntok, g_pre=None, w_in=None, norm_w=None):
    P = C.P
    KC = D_MODEL // 128
    K = SSD_DI if mode == "ssd" else D_MODEL
    KO = K // 128
    with ExitStack() as es:
        wo = sb(C, es, "wo", [128, KO, D_MODEL], BF16)
        for k in range(KO):
            load_cast(C, wo[:, k, :], w_out[k * 128:(k + 1) * 128, :])
        gpost = sb(C, es, "ogpost", [128, D_MODEL], F32)
        P.dma("sp", gpost[:], g_post.partition_broadcast(128))
        if mode == "ssd":
            gpre = load_featmajor_params(C, es, [g_pre], KC, "ogpre")
            wz = sb(C, es, "wz", [128, KC, SSD_DI], BF16)
            for k in range(KC):
                load_cast(C, wz[:, k, :], w_in[k * 128:(k + 1) * 128, 0:SSD_DI])
            nw = sb(C, es, "nw", [128, SSD_DI], F32)
            P.dma("sp", nw[:], norm_w.partition_broadcast(128))
            hn = [sb(C, es, "ohn", [128, D_MODEL], BF16) for _ in range(2)]
            hnT = [sb(C, es, "ohnT", [128, KC, 128], BF16) for _ in range(2)]
            zs = [sb(C, es, "zs", [128, SSD_DI], F32) for _ in range(2)]
            ssg = [sb(C, es, "ssg", [128, 4], F32) for _ in range(2)]
            rsg = [sb(C, es, "rsg", [128, 4], F32) for _ in range(2)]
            pZ = ps(C, es, "pZ", [128, SSD_DI], F32)
        xt = [sb(C, es, "oxt", [128, D_MODEL], F32) for _ in range(3)]
        yt = [sb(C, es, "oyt", [128, K], F32) for _ in range(2)]
        yb = [sb(C, es, "oyb", [128, K], BF16) for _ in range(2)]
        yT = [sb(C, es, "oyT", [128, KO, 128], BF16) for _ in range(2)]
        junk = sb(C, es, "ojunk", [128, D_MODEL], BF16)
        ss = [sb(C, es, "oss", [128, 1], F32) for _ in range(2)]
        rstd = [sb(C, es, "ors", [128, 1], F32) for _ in range(2)]
        ft = [sb(C, es, "oft", [128, D_MODEL], F32) for _ in range(2)]
        pT = ps(C, es, "opT", [128, 8, 128], BF16)
        pO = ps(C, es, "opO", [128, D_MODEL], F32)
        for t in range(ntok // 128):
            r0 = t * 128
            x = xt[t % 3]
            y = yt[t % 2]
            ybf = yb[t % 2]
            P.dma("sp", x[:], h_in[r0:r0 + 128, :])
            P.dma("sp", y[:], src[r0:r0 + 128, :])
            if mode == "ssd":
                hb = hn[t % 2]
                P.act(junk[:], x[:], AF.Square, accum_out=ss[t % 2][:])
                rstd_from_ss(C, rstd[t % 2][:], ss[t % 2][:], D_MODEL)
                P.ts("pool", hb[:], x[:], rstd[t % 2][:], ALU.mult)
                hT = hnT[t % 2]
                transpose_rows(C, hb, pT, hT, 0, KC, gpre, ev="dve")
                for nch in range(4):
                    for k in range(KC):
                        P.mm(pZ[:, nch * 512:(nch + 1) * 512], hT[:, k, :], wz[:, k, nch * 512:(nch + 1) * 512],
                             start=(k == 0), stop=(k == KC - 1))
                z = zs[t % 2]
                sg, rg = ssg[t % 2], rsg[t % 2]
                for nch in range(4):
                    cs = slice(nch * 512, (nch + 1) * 512)
                    P.act(z[:, cs], pZ[:, cs], AF.Silu)
                    P.tt("pool", y[:, cs], y[:, cs], z[:, cs], ALU.mult)
                    P.act(junk[:, 0:512], y[:, cs], AF.Square, accum_out=sg[:, nch:nch + 1])
                P.act(rg[:], sg[:], AF.Sqrt, scale=1.0 / 512, bias=RMS_EPS)
                P.I("dve", "reciprocal", out=rg[:], in_=rg[:])
                y3 = y[:].rearrange("p (g d) -> p g d", g=4)
                P.tt("dve", y3, y3, rg[:].unsqueeze(2).broadcast_to([128, 4, 512]), ALU.mult)
                P.tt("pool", ybf[:], y[:], nw[:], ALU.mult)
            else:
                P.copy("pool", ybf[:], y[:])
            transpose_rows(C, ybf, pT, yT[t % 2], 0, KO, None, ev="act")
            for half in range(2):
                for k in range(KO):
                    P.mm(pO[:, half * 512:(half + 1) * 512], yT[t % 2][:, k, :], wo[:, k, half * 512:(half + 1) * 512],
                         start=(k == 0), stop=(k == KO - 1))
            f = ft[t % 2]
            P.act(junk[:], pO[:], AF.Square, accum_out=ss[t % 2][:])
            rstd_from_ss(C, rstd[t % 2][:], ss[t % 2][:], D_MODEL)
            P.stt("dve", f[:], pO[:], rstd[t % 2][:], gpost[:], ALU.mult, ALU.mult)
            P.tt("pool", f[:], f[:], x[:], ALU.add)
            P.dma("sp", h_out[r0:r0 + 128, :], f[:])
        P.barrier()


NSA_H = 16
NSA_HD = 64
NSA_G = 4
NSA_PROJ = 2608
NEG = -30000.0


def nsa_consts(S):
    nsb = S // 64
    ncmp = (S - 32) // 16 + 1
    cs = np.arange(ncmp) * 16
    sblk = np.arange(nsb) * 64
    ov = np.minimum(cs[:, None] + 32, sblk[None, :] + 64) - np.maximum(cs[:, None], sblk[None, :])
    ov = np.clip(ov, 0, None).astype(np.float32) / 32
    ov_aug = np.zeros((128, 1 + nsb), np.float32)
    ov_aug[:, 0] = 1.0
    ov_aug[:ncmp, 1:] = ov
    pos = np.arange(S)
    blk = np.arange(nsb)[None, :]
    cur = (pos // 64)[:, None]
    forced = (blk == 0) | ((blk <= cur) & (blk > cur - 2))
    forced = np.where(forced, 1e9, 0.0).astype(np.float32)
    E = (np.arange(S)[None, :] // 64 == np.arange(nsb)[:, None]).astype(np.float32)
    return dict(c_ov=ov_aug, c_forced=forced, c_E=E)


def emit_nsa_a(C, h_in, g_pre, w_in, qT_d, kT_d, v_d, gates_d, ntok, seq=SEQ):
    P = C.P
    KC = D_MODEL // 128
    TG = 512
    NT = TG // 128
    with ExitStack() as es:
        gpre = load_featmajor_params(C, es, [g_pre], KC, "ngpre")
        wq = sb(C, es, "wq", [128, KC, NSA_PROJ], BF16)
        for k in range(KC):
            load_cast(C, wq[:, k, :], w_in[k * 128:(k + 1) * 128, :])
        xt = [sb(C, es, "nxt", [128, D_MODEL], F32) for _ in range(2)]
        hn = [sb(C, es, "nhn", [128, D_MODEL], BF16) for _ in range(2)]
        junk = sb(C, es, "njunk", [128, D_MODEL], BF16)
        ss = [sb(C, es, "nss", [128, 1], F32) for _ in range(2)]
        rstd = [sb(C, es, "nrs", [128, 1], F32) for _ in range(2)]
        hnT = sb(C, es, "nhnT", [128, KC, TG], BF16)
        fm = [sb(C, es, "nfm", [64, TG], BF16) for _ in range(4)]
        vt = [sb(C, es, "nvt", [128, 512], BF16) for _ in range(2)]
        gt = [sb(C, es, "ngt", [128, 48], F32) for _ in range(2)]
        pT = ps(C, es, "npT", [128, 8, 128], BF16)
        pY = [ps(C, es, "npY", [128, TG], F32) for _ in range(2)]
        pV = ps(C, es, "npV", [128, 512], F32)
        pG = ps(C, es, "npG", [128, 512], F32)
        for g in range(ntok // TG):
            sidx = (g * TG) // seq
            s0 = (g * TG) % seq
            for t in range(NT):
                x = xt[t % 2]
                r0 = g * TG + t * 128
                P.dma("sp", x[:], h_in[r0:r0 + 128, :])
                hb = hn[t % 2]
                P.act(junk[:], x[:], AF.Square, accum_out=ss[t % 2][:])
                rstd_from_ss(C, rstd[t % 2][:], ss[t % 2][:], D_MODEL)
                P.ts("pool", hb[:], x[:], rstd[t % 2][:], ALU.mult)
                transpose_rows(C, hb, pT, hnT, t * 128, KC, gpre, ev=("dve" if t % 2 else "act"))
            for blk in range(32):
                if blk < 16:
                    col = blk * 64
                    dst = qT_d[sidx, blk, :, s0:s0 + TG]
                else:
                    b2 = blk - 16
                    typ, gg = b2 // 4, b2 % 4
                    col = 1024 + (0, 1, 2, 4)[typ] * 256 + gg * 64
                    dst = kT_d[sidx, typ, gg, :, s0:s0 + TG]
                y = pY[blk % 2]
                for k in range(KC):
                    P.mm(y[0:64, :], wq[:, k, col:col + 64], hnT[:, k, :], start=(k == 0), stop=(k == KC - 1))
                f = fm[blk % 4]
                P.copy("act" if blk % 2 == 0 else "dve", f[:], y[0:64, :])
                P.dma("sp", dst, f[:])
            for t in range(NT):
                hsl = slice(t * 128, (t + 1) * 128)
                for vi, c0 in enumerate((1792, 2304)):
                    for k in range(KC):
                        P.mm(pV[:, vi * 256:(vi + 1) * 256], hnT[:, k, hsl], wq[:, k, c0:c0 + 256], start=(k == 0), stop=(k == KC - 1))
                for k in range(KC):
                    P.mm(pG[:, 0:48], hnT[:, k, hsl], wq[:, k, 2560:2608], start=(k == 0), stop=(k == KC - 1))
                v = vt[t % 2]
                P.copy("dve", v[:], pV[:])
                P.dma("sp", v_d[sidx, 0, s0 + t * 128:s0 + (t + 1) * 128, :], v[:, 0:256])
                P.dma("sp", v_d[sidx, 1, s0 + t * 128:s0 + (t + 1) * 128, :], v[:, 256:512])
                gg_ = gt[t % 2]
                P.act(gg_[:], pG[:, 0:48], AF.Sigmoid)
                r0 = g * TG + t * 128
                P.dma("sp", gates_d[r0:r0 + 128, :], gg_[:])
        P.barrier()


def emit_nsa_c(C, qT_d, kT_d, v_d, gates_d, o_d, cmp_pos, cmp_w1, cmp_w2, c_ov, c_forced, c_E, nseq, seq=SEQ):
    P = C.P
    S = seq
    NQ = S // 128
    NSB = S // 64
    NCMP = (S - 32) // 16 + 1
    assert NCMP <= 127 and NSB <= 32 and NSB >= 8
    NSEL = min(16, NSB)
    WT = 4
    VW = 65 + NSB
    with ExitStack() as es:
        E = sb(C, es, "cE", [NSB, S], BF16)
        P.dma("pool", E[:], c_E[:, :])
        forced = sb(C, es, "cforced", [128, NQ, NSB], F32)
        P.dma("sp", forced[:], c_forced.rearrange("(t p) j -> p t j", p=128))
        onesf = sb(C, es, "aones", [128, 512], F32)
        P.memset("pool", onesf[:], 0.0)
        negU = sb(C, es, "negU", [128, 4, 128], BF16)
        negL = sb(C, es, "negL", [128, 4, 128], BF16)
        zf = onesf[:].rearrange("p (a b) -> p a b", a=4)
        P.I("pool", "affine_select", out=negU[:], in_=zf, pattern=[[0, 4], [1, 128]], compare_op=ALU.is_ge, fill=NEG,
            base=0, channel_multiplier=-1)
        P.I("pool", "affine_select", out=negL[:], in_=zf, pattern=[[0, 4], [-1, 128]], compare_op=ALU.is_gt, fill=NEG,
            base=0, channel_multiplier=1)
        negC = sb(C, es, "negC", [128, NQ, 128], BF16)
        for i in range(NQ):
            P.I("pool", "affine_select", out=negC[:, i, :], in_=onesf[:, 0:128], pattern=[[1, 128]], compare_op=ALU.is_ge,
                fill=NEG, base=128 * i - 31, channel_multiplier=-16)
        w1s = [sb(C, es, "w1s", [64, 32, 256], BF16) for _ in range(2)]
        w2s = [sb(C, es, "w2s", [128, 2, 64], BF16) for _ in range(2)]
        posT = [sb(C, es, "posT", [64, 32], F32) for _ in range(2)]
        with ExitStack() as es2:
            praw = sb(C, es2, "praw", [32, 64], F32)
            pp = ps(C, es2, "ppos", [64, 32], F32)
            for kv in range(2):
                P.dma("pool", w1s[kv][:], cmp_w1[kv].rearrange("(l d) j -> d l j", d=64))
                P.dma("pool", w2s[kv][:], cmp_w2[kv].rearrange("(a p) d -> p a d", p=128))
                P.dma("sp", praw[:], cmp_pos[kv])
                P.tr(pp[:, :], praw[:, :], C.ident_f[0:32, 0:32])
                P.copy("dve", posT[kv][:], pp[:, :])
            P.barrier()
        qT = sb(C, es, "qT", [64, 4, S], BF16)
        kTs = sb(C, es, "kTs", [64, S], BF16)
        kTw = sb(C, es, "kTw", [64, S], BF16)
        tT = [sb(C, es, "tT", [64, S], BF16) for _ in range(2)]
        tl = sb(C, es, "tl", [64, 32, NCMP], BF16)
        hgT = sb(C, es, "hgT", [128, 2, 128], BF16)
        g_sq = sb(C, es, "g_sq", [128, NCMP], F32)
        g_in = sb(C, es, "g_in", [128, NCMP], F32)
        kcT = sb(C, es, "kcT", [64, 128], BF16)
        Vc = sb(C, es, "Vc", [128, VW], BF16)
        Vs = sb(C, es, "Vs", [128, NQ, 65], BF16)
        Vw = sb(C, es, "Vw", [128, NQ, 65], BF16)
        ovf = sb(C, es, "ovf", [128, 1 + NSB], F32)
        gts = sb(C, es, "gts", [128, NQ, 12], F32)
        PTc = sb(C, es, "PTc", [128, 512], BF16)
        PTs = sb(C, es, "PTs", [128, NQ, 512], BF16)
        PTw = sb(C, es, "PTw", [128, WT + 1, 512], BF16)
        rden = [sb(C, es, "rden", [128, 4], F32) for _ in range(3)]
        coef = [sb(C, es, "coef", [128, 4], F32) for _ in range(3)]
        o_acc = [sb(C, es, "o_acc", [128, 4, 64], F32) for _ in range(2)]
        tmpo = [sb(C, es, "tmpo", [128, 4, 64], F32) for _ in range(2)]
        tmp4 = sb(C, es, "tmp4", [128, 4, NSB], F32)
        score = sb(C, es, "score", [128, NSB], F32)
        score2 = sb(C, es, "score2", [128, NSB], F32)
        m8a = sb(C, es, "m8a", [128, 8], F32)
        m8b = sb(C, es, "m8b", [128, 8], F32)
        negq = sb(C, es, "negq", [128, NSB], F32)
        negT = [sb(C, es, "negT", [NSB, 128], BF16) for _ in range(2)]
        pS = [ps(C, es, "apS", [128, 512], F32) for _ in range(2)]
        pOc = ps(C, es, "apOc", [128, 4, 128], F32)
        pOs = ps(C, es, "apOs", [128, 4, 128], F32)
        pOw = ps(C, es, "apOw", [128, 4, 128], F32)
        pN = ps(C, es, "apN", [128, 512], F32)
        P.dma("sp", ovf[:], c_ov[:, :])
        P.memset("pool", hgT[:], 0.0)
        P.memset("pool", Vs[:], 1.0)
        P.memset("pool", Vw[:], 1.0)

        def bc4(ap4, n):
            return ap4.unsqueeze(2).broadcast_to([128, 4, n])

        for sq_ in range(nseq):
            for g in range(NSA_G):
                P.dma("sp", qT[:], qT_d[sq_, 4 * g:4 * g + 4].rearrange("h d s -> d h s"))
                P.dma("sp", tT[0][:], kT_d[sq_, 0, g])
                P.dma("sp", tT[1][:], kT_d[sq_, 1, g])
                P.dma("sp", kTs[:], kT_d[sq_, 2, g])
                P.dma("sp", kTw[:], kT_d[sq_, 3, g])
                P.dma("sp", Vs[:, :, 0:64], v_d[sq_, 0, :, g * 64:(g + 1) * 64].rearrange("(t p) d -> p t d", p=128))
                P.dma("sp", Vw[:, :, 0:64], v_d[sq_, 1, :, g * 64:(g + 1) * 64].rearrange("(t p) d -> p t d", p=128))
                P.dma("sp", gts[:], gates_d[sq_ * S:(sq_ + 1) * S, g * 12:(g + 1) * 12].rearrange("(t p) c -> p t c", p=128))
                for kv in range(2):
                    for l in range(32):
                        b0 = (l // 16) * 16
                        src = tT[kv][:, b0:b0 + NCMP * 16].rearrange("p (c r) -> p c r", r=16)[:, :, l % 16]
                        P.ts("dve" if l % 2 else "pool", tl[:, l, :], src, posT[kv][:, l:l + 1], ALU.add)
                    for jh in range(2):
                        for l in range(32):
                            P.mm(pS[jh][:, 0:NCMP], w1s[kv][:, l, jh * 128:(jh + 1) * 128], tl[:, l, :], start=(l == 0), stop=(l == 31))
                    for jh in range(2):
                        xg = pS[jh][:, 0:NCMP]
                        P.act(g_sq[:], xg, AF.Square)
                        P.ts("dve", g_sq[:], g_sq[:], 0.044715, ALU.mult, 1.0, ALU.add)
                        P.tt("dve", g_in[:], g_sq[:], xg, ALU.mult)
                        P.act(g_in[:], g_in[:], AF.Sigmoid, scale=1.5957691216057308)
                        P.tt("dve", hgT[:, jh, 0:NCMP], g_in[:], xg, ALU.mult)
                    if kv == 0:
                        for jh in range(2):
                            P.mm(pN[0:64, 0:128], w2s[0][:, jh, :], hgT[:, jh, :], start=(jh == 0), stop=(jh == 1))
                        P.copy("act", kcT[:], pN[0:64, 0:128])
                    else:
                        for jh in range(2):
                            P.mm(pN[:, 0:64], hgT[:, jh, :], w2s[1][:, jh, :], start=(jh == 0), stop=(jh == 1))
                        P.copy("act", Vc[:, 0:64], pN[:, 0:64])
                        P.copy("pool", Vc[:, 64:VW], ovf[:])
                for i in range(NQ):
                    qv = qT[:, :, i * 128:(i + 1) * 128]
                    oa = o_acc[i % 2]
                    tm = tmpo[i % 2]
                    sc = pS[0]
                    P.mm(sc[:], kcT[:], qv, start=True, stop=False)
                    P.mm(sc[:], C.ident[:], negC[:, i, :].unsqueeze(1).broadcast_to([128, 4, 128]), start=False, stop=True)
                    P.act(PTc[:], sc[:], AF.Exp, scale=0.125)
                    for h in range(4):
                        P.mm(pOc[:, h, 0:VW], PTc[:, h * 128:(h + 1) * 128], Vc[:], start=True, stop=True)
                    P.ts("dve", rden[0][:], pOc[:, :, 64], 1e-30, ALU.add)
                    P.I("dve", "reciprocal", out=rden[0][:], in_=rden[0][:])
                    P.tt("dve", coef[0][:], rden[0][:], gts[:, i, 0:12:3], ALU.mult)
                    P.tt("dve", oa[:], pOc[:, :, 0:64], bc4(coef[0][:], 64), ALU.mult)
                    P.tt("dve", tmp4[:], pOc[:, :, 65:VW], bc4(rden[0][:], NSB), ALU.mult)
                    P.I("dve", "tensor_reduce", out=score[:], in_=tmp4[:].rearrange("p h j -> p j h"), axis=AX.X, op=ALU.add)
                    P.tt("dve", score[:], score[:], forced[:, i, :], ALU.add)
                    P.I("dve", "max", out=m8a[:], in_=score[:])
                    thr = m8a
                    if NSEL > 8:
                        P.I("dve", "match_replace", out=score2[:], in_to_replace=m8a[:], in_values=score[:], imm_value=-1.0)
                        P.I("dve", "max", out=m8b[:], in_=score2[:])
                        thr = m8b
                    P.ts("dve", negq[:], score[:], thr[:, 7:8], ALU.is_lt, NEG, ALU.mult)
                    P.tr(pN[0:NSB, 0:128], negq[:, :], C.ident_f[:, :])
                    nT = negT[i % 2]
                    P.copy("act", nT[:], pN[0:NSB, 0:128])
                    for j in range(i + 1):
                        sc = pS[(j + 1) % 2]
                        ks = slice(j * 128, (j + 1) * 128)
                        P.mm(sc[:], kTs[:, ks], qv, start=True, stop=False)
                        P.mm(sc[:], E[:, ks], nT[:].unsqueeze(1).broadcast_to([NSB, 4, 128]), start=False, stop=(j != i))
                        if j == i:
                            P.mm(sc[:], C.ident[:], negU[:], start=False, stop=True)
                        P.act(PTs[:, j, :], sc[:], AF.Exp, scale=0.125)
                    for h in range(4):
                        for j in range(i + 1):
                            P.mm(pOs[:, h, 0:65], PTs[:, j, h * 128:(h + 1) * 128], Vs[:, j, :], start=(j == 0), stop=(j == i))
                    P.I("dve", "reciprocal", out=rden[1][:], in_=pOs[:, :, 64])
                    P.tt("dve", coef[1][:], rden[1][:], gts[:, i, 1:12:3], ALU.mult)
                    P.tt("dve", tm[:], pOs[:, :, 0:64], bc4(coef[1][:], 64), ALU.mult)
                    P.tt("pool", oa[:], oa[:], tm[:], ALU.add)
                    j0 = max(0, i - WT)
                    for j in range(j0, i + 1):
                        sc = pS[(j + i) % 2]
                        ks = slice(j * 128, (j + 1) * 128)
                        last_plain = not (j == i or j == i - WT)
                        P.mm(sc[:], kTw[:, ks], qv, start=True, stop=last_plain)
                        if j == i:
                            P.mm(sc[:], C.ident[:], negU[:], start=False, stop=True)
                        elif j == i - WT:
                            P.mm(sc[:], C.ident[:], negL[:], start=False, stop=True)
                        P.act(PTw[:, j - j0, :], sc[:], AF.Exp, scale=0.125)
                    for h in range(4):
                        for j in range(j0, i + 1):
                            P.mm(pOw[:, h, 0:65], PTw[:, j - j0, h * 128:(h + 1) * 128], Vw[:, j, :], start=(j == j0), stop=(j == i))
                    P.I("dve", "reciprocal", out=rden[2][:], in_=pOw[:, :, 64])
                    P.tt("dve", coef[2][:], rden[2][:], gts[:, i, 2:12:3], ALU.mult)
                    P.tt("dve", tm[:], pOw[:, :, 0:64], bc4(coef[2][:], 64), ALU.mult)
                    P.tt("pool", oa[:], oa[:], tm[:], ALU.add)
                    r0 = sq_ * S + i * 128
                    P.dma("sp", o_d[r0:r0 + 128, g * 256:(g + 1) * 256], oa[:].rearrange("p h d -> p (h d)"))
        P.barrier()


DEPTH = 4
NSEQ_CORE = 2
W_SHAPES = {
    "norm_gains": [4, 4, 1024],
    "nsa_w_in": [2, 1024, 2608], "nsa_cmp_pos": [2, 2, 32, 64], "nsa_cmp_w1": [2, 2, 2048, 256],
    "nsa_cmp_w2": [2, 2, 256, 64], "nsa_w_out": [2, 1024, 1024],
    "ssd_w_in": [2, 1024, 5152], "ssd_conv_w": [2, 4, 3072], "ssd_conv_b": [2, 3072], "ssd_dt_bias": [2, 32],
    "ssd_a_log": [2, 32], "ssd_d": [2, 32], "ssd_norm_w": [2, 2048], "ssd_w_out": [2, 2048, 1024],
    "ffn_w_up": [4, 1024, 5632], "ffn_conv_w": [4, 3, 5632], "ffn_conv_b": [4, 5632], "ffn_w_down": [4, 2816, 1024],
}


def build_program(nseq=NSEQ_CORE, seq=SEQ, depth=DEPTH):
    nc = bass.Bass("TRN2", target_bir_lowering=False)
    ntok = nseq * seq
    nsb = seq // 64
    W = {k: nc.dram_tensor(k, v, F32, kind="ExternalInput").ap() for k, v in W_SHAPES.items()}
    x = nc.dram_tensor("x", [ntok, D_MODEL], F32, kind="ExternalInput").ap()
    c_ov = nc.dram_tensor("c_ov", [128, 1 + nsb], F32, kind="ExternalInput").ap()
    c_forced = nc.dram_tensor("c_forced", [seq, nsb], F32, kind="ExternalInput").ap()
    c_E = nc.dram_tensor("c_E", [nsb, seq], F32, kind="ExternalInput").ap()
    out = nc.dram_tensor("out", [ntok, D_MODEL], F32, kind="ExternalOutput").ap()

    def scratch(name, shape, dt=F32):
        return nc.dram_tensor(name, shape, dt, kind="Internal").ap()
    hA = scratch("hA", [ntok, D_MODEL])
    hB = scratch("hB", [ntok, D_MODEL])
    fpart = scratch("fpart", [ntok, D_MODEL])
    y_pre = scratch("y_pre", [ntok, SSD_DI])
    o_d = scratch("o_d", [ntok, D_MODEL])
    gates_d = scratch("gates_d", [ntok, 48])
    qT_d = scratch("qT_d", [nseq, NSA_H, 64, seq], BF16)
    kT_d = scratch("kT_d", [nseq, 4, NSA_G, 64, seq], BF16)
    v_d = scratch("v_d", [nseq, 2, seq, 256], BF16)

    P = Prog(nc)
    P.nodep |= set(W_SHAPES) | {"x", "c_ov", "c_forced", "c_E"}
    C = Ctx(nc, P)
    with ExitStack() as es:
        emit_consts(C, es)
        P.barrier()
        cur = x
        for i in range(depth):
            g = W["norm_gains"][i]
            slot = i // 2
            if i % 2 == 0:
                emit_nsa_a(C, cur, g[0], W["nsa_w_in"][slot], qT_d, kT_d, v_d, gates_d, ntok, seq=seq)
                emit_nsa_c(C, qT_d, kT_d, v_d, gates_d, o_d, W["nsa_cmp_pos"][slot], W["nsa_cmp_w1"][slot],
                           W["nsa_cmp_w2"][slot], c_ov, c_forced, c_E, nseq, seq=seq)
                emit_out(C, "nsa", cur, hA, o_d, W["nsa_w_out"][slot], g[1], ntok)
            else:
                emit_ssd_a(C, cur, y_pre, g[0], W["ssd_w_in"][slot], W["ssd_conv_w"][slot], W["ssd_conv_b"][slot],
                           W["ssd_dt_bias"][slot], W["ssd_a_log"][slot], W["ssd_d"][slot], ntok, seq=seq)
                emit_out(C, "ssd", cur, hA, y_pre, W["ssd_w_out"][slot], g[1], ntok,
                         g_pre=g[0], w_in=W["ssd_w_in"][slot], norm_w=W["ssd_norm_w"][slot])
            dst = out if i == depth - 1 else hB
            emit_ffn(C, hA, dst, fpart, g[2], g[3], W["ffn_w_up"][i], W["ffn_conv_w"][i], W["ffn_conv_b"][i],
                     W["ffn_w_down"][i], ntok, seq=seq)
            cur = hB
        P.barrier()
    return nc, P


def kernel(**inputs):
    x = np.ascontiguousarray(inputs["x"], dtype=np.float32)
    B, S, D = x.shape
    assert (B, S, D) == (N_CORES * NSEQ_CORE, SEQ, D_MODEL)
    nc, _ = build_program()
    consts = nsa_consts(SEQ)
    shared = {k: np.ascontiguousarray(inputs[k], dtype=np.float32) for k in W_SHAPES}
    shared.update(consts)
    in_maps = []
    for c in range(N_CORES):
        m = dict(shared)
        m["x"] = x[c * NSEQ_CORE:(c + 1) * NSEQ_CORE].reshape(NSEQ_CORE * SEQ, D_MODEL)
        in_maps.append(m)
    res = run_bass_kernel_spmd(nc, in_maps, core_ids=list(range(N_CORES)))
    outs = [r["out"].reshape(NSEQ_CORE, SEQ, D_MODEL) for r in res.results]
    return np.concatenate(outs, axis=0).astype(np.float32)
```

```python
import math
from contextlib import ExitStack

import numpy as np
import concourse.bass as bass
import concourse.mybir as mybir
from concourse.bass_utils import run_bass_kernel_spmd

F32 = mybir.dt.float32
BF16 = mybir.dt.bfloat16
AF = mybir.ActivationFunctionType
ALU = mybir.AluOpType
AX = mybir.AxisListType

D_MODEL = 1024
SEQ = 2048
N_CORES = 8
FFN_HIDDEN = 2816
RMS_EPS = 1e-6

WRITE_KEYS = ("out", "accum_out", "ap")
DT_SIZE = {F32: 4, BF16: 2}


def _box(ap):
    t = ap.tensor
    tn = type(t).__name__
    off = ap.offset
    dims = list(ap.ap)
    if tn.startswith("SB") or tn.startswith("PSum"):
        pstep = 1
        for s in list(t.shape)[1:]:
            pstep *= int(s)
        p0 = off // pstep
        f0 = off % pstep
        st0, cn0 = dims[0]
        pe = (cn0 - 1) * (st0 // pstep) if cn0 > 1 else 0
        fe = 0
        for st, cn in dims[1:]:
            if cn > 1:
                assert st >= 0
                fe += (cn - 1) * st
        f1 = f0 + fe + 1
        if tn.startswith("PSum"):
            be = 2048 // DT_SIZE[t.dtype]
            f0 = (f0 // be) * be
            f1 = -(-f1 // be) * be
        return (p0, p0 + pe + 1, f0, f1)
    lo = hi = off
    for st, cn in dims:
        if cn > 1:
            if st >= 0:
                hi += (cn - 1) * st
            else:
                lo += (cn - 1) * st
    return (0, 1, lo, hi + 1)


class Prog:
    def __init__(self, nc, n_dma_sems=10, same_engine_sync=True):
        self.nc = nc
        self.e = dict(pe=nc.tensor, act=nc.scalar, dve=nc.vector, pool=nc.gpsimd, sp=nc.sync)
        self.sem = {}
        self.cnt = {}
        self.allsems = []
        for k in self.e:
            self._new_sem(k)
        self.seen = {k: {} for k in self.e}
        self.acc = {}
        self.nodep = set()
        self.dsems = {}
        self.dnext = {}
        self.dval = {}
        self.semh = {}
        for q in ("sp", "act", "pool"):
            self.dsems[q] = []
            for i in range(n_dma_sems):
                s = nc.alloc_semaphore(name=f"d_{q}_{i}")
                self.dsems[q].append(s)
                self.dval[s.num] = 0
                self.semh[s.num] = s
            self.dnext[q] = 0
        self.same = same_engine_sync
        self.ninst = 0
        self.nwait = 0

    def _new_sem(self, k):
        s = self.nc.alloc_semaphore(name=f"s_{k}_{len(self.allsems)}")
        self.sem[k] = s
        self.cnt[k] = 0
        self.allsems.append(s)

    def _wait(self, ek, sem, val):
        sn = self.seen[ek]
        if sn.get(sem.num, 0) >= val:
            return
        self.e[ek].wait_ge(sem, val)
        sn[sem.num] = val
        self.nwait += 1

    def _sync(self, ek, reads, writes):
        need = {}
        for is_w, aps in ((False, reads), (True, writes)):
            for ap in aps:
                name = ap.tensor.name
                if name in self.nodep:
                    continue
                b = _box(ap)
                is_psum = type(ap.tensor).__name__.startswith("PSum")
                for r in self.acc.get(name, ()):
                    if not (is_w or r[0]):
                        if not (is_psum and r[7] != ek):
                            continue
                    if r[1] < b[1] and b[0] < r[2] and r[3] < b[3] and b[2] < r[4]:
                        if r[7] == ek and (ek == "pe" or not self.same) and r[7] != "dma":
                            continue
                        s, v = r[5], r[6]
                        if need.get(s.num, (None, 0))[1] < v:
                            need[s.num] = (s, v)
        for s, v in need.values():
            self._wait(ek, s, v)

    def _record(self, ek, reads, writes, sem, val, is_dma=False):
        tag = "dma" if is_dma else ek
        for is_w, aps in ((False, reads), (True, writes)):
            for ap in aps:
                name = ap.tensor.name
                if name in self.nodep:
                    continue
                b = _box(ap)
                lst = self.acc.setdefault(name, [])
                keep = []
                for r in lst:
                    inside = r[1] >= b[0] and r[2] <= b[1] and r[3] >= b[2] and r[4] <= b[3]
                    if inside and (is_w or ((not r[0]) and r[5].num == sem.num)):
                        continue
                    keep.append(r)
                keep.append((is_w, b[0], b[1], b[2], b[3], sem, val, tag))
                self.acc[name] = keep

    def I(self, ek, name, **kw):
        reads, writes = [], []
        for k, v in kw.items():
            if isinstance(v, bass.AP):
                (writes if k in WRITE_KEYS else reads).append(v)
        self._sync(ek, reads, writes)
        ins = getattr(self.e[ek], name)(**kw)
        self.cnt[ek] += 1
        ins.then_inc(self.sem[ek], 1)
        self._record(ek, reads, writes, self.sem[ek], self.cnt[ek])
        self.ninst += 1
        if self.cnt[ek] >= 30000:
            self._new_sem(ek)
        return ins

    def dma(self, q, out, in_, **kw):
        reads, writes = [in_], [out]
        self._sync(q, reads, writes)
        lst = self.dsems[q]
        s = lst[self.dnext[q] % len(lst)]
        self.dnext[q] += 1
        prev = self.dval[s.num]
        if prev:
            self._wait(q, s, prev)
        ins = self.e[q].dma_start(out=out, in_=in_, **kw)
        ins.then_inc(s, 16)
        self.dval[s.num] = prev + 16
        self._record(q, reads, writes, s, prev + 16, is_dma=True)
        self.ninst += 1
        return ins

    def barrier(self, engines=None):
        for x in (engines or self.e):
            for k in self.e:
                if self.cnt[k] and not (k == x):
                    self._wait(x, self.sem[k], self.cnt[k])
            for n, v in self.dval.items():
                if v:
                    self._wait(x, self.semh[n], v)
        self.acc = {}

    def mm(self, out, lhsT, rhs, start=True, stop=True, **kw):
        return self.I("pe", "matmul", out=out, lhsT=lhsT, rhs=rhs, start=start, stop=stop, **kw)

    def tr(self, out, in_, identity):
        return self.I("pe", "transpose", out=out, in_=in_, identity=identity)

    def act(self, out, in_, func, **kw):
        return self.I("act", "activation", out=out, in_=in_, func=func, **kw)

    def ts(self, ek, out, in0, s1, op0, s2=None, op1=None, **kw):
        if op1 is None:
            s2, op1 = 0.0, ALU.add
        return self.I(ek, "tensor_scalar", out=out, in0=in0, scalar1=s1, scalar2=s2, op0=op0, op1=op1, **kw)

    def tt(self, ek, out, in0, in1, op):
        return self.I(ek, "tensor_tensor", out=out, in0=in0, in1=in1, op=op)

    def stt(self, ek, out, in0, scalar, in1, op0, op1):
        return self.I(ek, "scalar_tensor_tensor", out=out, in0=in0, scalar=scalar, in1=in1, op0=op0, op1=op1)

    def copy(self, ek, out, in_):
        if ek == "act":
            return self.I("act", "activation", out=out, in_=in_, func=AF.Copy)
        return self.I(ek, "tensor_copy", out=out, in_=in_)

    def memset(self, ek, ap, val):
        return self.I(ek, "memset", ap=ap, constant=val)


class Ctx:
    def __init__(self, nc, P):
        self.nc = nc
        self.P = P
        self.uid = 0

    def name(self, base):
        self.uid += 1
        return f"{base}_{self.uid}"


def sb(C, es, base, shape, dtype):
    return es.enter_context(C.nc.sbuf_tensor(C.name(base), shape, dtype))


def ps(C, es, base, shape, dtype=F32):
    return es.enter_context(C.nc.psum_tensor(C.name(base), shape, dtype))


def bcast_rows(ap_row, nparts):
    return ap_row.partition_broadcast(nparts)


def emit_consts(C, es):
    P = C.P
    ident_f = sb(C, es, "identf", [128, 128], F32)
    ident = sb(C, es, "ident", [128, 128], BF16)
    P.memset("pool", ident_f[:], 0.0)
    P.I("pool", "affine_select", out=ident_f[:], in_=ident_f[:], pattern=[[-1, 128]],
        compare_op=ALU.not_equal, fill=1.0, base=0, channel_multiplier=1)
    P.copy("dve", ident[:], ident_f[:])
    C.ident = ident
    C.ident_f = ident_f


def rstd_from_ss(C, rstd, ss, width):
    C.P.act(rstd, ss, AF.Sqrt, scale=1.0 / width, bias=RMS_EPS)
    C.P.I("dve", "reciprocal", out=rstd, in_=rstd)


def rmsnorm_rows(C, x_ap, gain_bc, out_ap, ss, rstd, junk, width=D_MODEL, eng="dve"):
    P = C.P
    P.act(junk, x_ap, AF.Square, accum_out=ss)
    rstd_from_ss(C, rstd, ss, width)
    P.stt(eng, out_ap, x_ap, rstd, gain_bc, ALU.mult, ALU.mult)


def transpose_rows(C, hb, pt, hT, c0, kc, gain=None, ev="dve"):
    P = C.P
    for k0 in range(0, kc, 8):
        kn = min(8, kc - k0)
        for k in range(kn):
            P.tr(pt[:, k, :], hb[:, (k0 + k) * 128:(k0 + k + 1) * 128], C.ident[:])
        dst = hT[:, k0:k0 + kn, c0:c0 + 128]
        if gain is None:
            P.copy(ev, dst, pt[:, 0:kn, :])
        elif ev == "dve":
            P.tt("dve", dst, pt[:, 0:kn, :], gain[:, 0, k0:k0 + kn].unsqueeze(2).broadcast_to([128, kn, 128]), ALU.mult)
        else:
            for k in range(kn):
                P.act(hT[:, k0 + k, c0:c0 + 128], pt[:, k, :], AF.Identity, scale=gain[:, 0, k0 + k:k0 + k + 1])


def load_cast(C, dst, w_dram_rows):
    C.P.dma("pool", dst, w_dram_rows)


def load_featmajor_params(C, es, rows, nchunk, name):
    P = C.P
    nr = len(rows)
    out = sb(C, es, name, [128, nr, nchunk], F32)
    with ExitStack() as es2:
        raw = sb(C, es2, name + "_raw", [nchunk, nr, 128], F32)
        pp = ps(C, es2, name + "_ps", [128, nchunk], F32)
        for j, r in enumerate(rows):
            P.dma("sp", raw[:, j, :], r.rearrange("(m p) -> m p", p=128))
        for j in range(nr):
            P.tr(pp[:, :], raw[:, j, :], C.ident_f[0:nchunk, 0:nchunk])
            P.copy("dve", out[:, j, :], pp[:, :])
        P.barrier()
    return out


def emit_ffn(C, h_in, h_out, fpart, g_pre, g_post, w_up, conv_w, conv_b, w_down, ntok, seq=SEQ):
    P = C.P
    F = FFN_HIDDEN
    NM = F // 128
    NS = 2
    MH = NM // NS
    KC = D_MODEL // 128
    TG = 512
    with ExitStack() as es:
        cwb = load_featmajor_params(C, es, [conv_w[0], conv_w[1], conv_w[2], conv_b], 2 * NM, "cwb")
        gpre = load_featmajor_params(C, es, [g_pre], KC, "gpre")
        wu = sb(C, es, "wu", [128, KC, 2 * MH * 128], BF16)
        wd = sb(C, es, "wd", [128, MH, D_MODEL], BF16)
        gpost = sb(C, es, "gpost", [128, D_MODEL], F32)
        xt = [sb(C, es, "xt", [128, D_MODEL], F32) for _ in range(6)]
        hn = [sb(C, es, "hn", [128, D_MODEL], BF16) for _ in range(2)]
        hnT = [sb(C, es, "hnT", [128, KC, TG], BF16) for _ in range(2)]
        gT = [sb(C, es, "gT", [128, MH, TG], BF16) for _ in range(2)]
        uv = [sb(C, es, "uv", [128, TG], F32) for _ in range(2)]
        ug = [sb(C, es, "ug", [128, TG], F32) for _ in range(2)]
        halo = [sb(C, es, "halo", [128, 2 * NM, 2], F32) for _ in range(2)]
        junk = sb(C, es, "junk", [128, D_MODEL], BF16)
        ss = [sb(C, es, "ss", [128, 1], F32) for _ in range(4)]
        rstd = [sb(C, es, "rstd", [128, 1], F32) for _ in range(4)]
        ft = [sb(C, es, "ft", [128, D_MODEL], F32) for _ in range(3)]
        pT = [ps(C, es, "pT", [128, 8, 128], BF16) for _ in range(2)]
        pY = [ps(C, es, "pY", [128, TG], F32) for _ in range(4)]
        pO = ps(C, es, "pO", [128, D_MODEL], F32)

        P.dma("sp", gpost[:], g_post.partition_broadcast(128))
        ngroups = ntok // TG
        xi = 0
        fi = 0
        for s in range(NS):
            for k in range(KC):
                rows = w_up[k * 128:(k + 1) * 128, :]
                load_cast(C, wu[:, k, 0:MH * 128], rows[:, s * MH * 128:(s + 1) * MH * 128])
                load_cast(C, wu[:, k, MH * 128:2 * MH * 128], rows[:, F + s * MH * 128:F + (s + 1) * MH * 128])
            for m in range(MH):
                r0 = (s * MH + m) * 128
                load_cast(C, wd[:, m, :], w_down[r0:r0 + 128, :])
            for g in range(ngroups):
                seq_start = (g * TG) % seq == 0
                hT = hnT[g % 2]
                xts = []
                for t in range(TG // 128):
                    x = xt[xi % len(xt)]
                    xi += 1
                    xts.append(x)
                    r0 = g * TG + t * 128
                    P.dma("sp", x[:], h_in[r0:r0 + 128, :])
                    hb = hn[t % 2]
                    sq, rs = ss[t % 4], rstd[t % 4]
                    P.act(junk[:], x[:], AF.Square, accum_out=sq[:])
                    rstd_from_ss(C, rs[:], sq[:], D_MODEL)
                    P.ts("dve", hb[:], x[:], rs[:], ALU.mult)
                    transpose_rows(C, hb, pT[t % 2], hT, t * 128, KC, gpre, ev=("dve" if t % 2 else "act"))
                G = gT[g % 2]
                for m in range(MH):
                    yv = pY[(2 * m) % 4]
                    yg = pY[(2 * m + 1) % 4]
                    cv = s * MH + m
                    for (y, c0) in ((yv, m * 128), (yg, (MH + m) * 128)):
                        for k in range(KC):
                            P.mm(y[:], wu[:, k, c0:c0 + 128], hT[:, k, :], start=(k == 0), stop=(k == KC - 1))
                    for (y, u, ch) in ((yv, uv[m % 2], cv), (yg, ug[m % 2], NM + cv)):
                        P.act(u[:], y[:], AF.Identity, scale=cwb[:, 2, ch:ch + 1], bias=cwb[:, 3, ch:ch + 1])
                        P.copy("act", halo[g % 2][:, ch, :], y[:, TG - 2:TG])
                        P.stt("dve", u[:, 1:TG], y[:, 0:TG - 1], cwb[:, 1, ch:ch + 1], u[:, 1:TG], ALU.mult, ALU.add)
                        P.stt("dve", u[:, 2:TG], y[:, 0:TG - 2], cwb[:, 0, ch:ch + 1], u[:, 2:TG], ALU.mult, ALU.add)
                        if not seq_start:
                            hp = halo[(g + 1) % 2]
                            P.stt("dve", u[:, 0:2], hp[:, ch, :], cwb[:, 0, ch:ch + 1], u[:, 0:2], ALU.mult, ALU.add)
                            P.stt("dve", u[:, 0:1], hp[:, ch, 1:2], cwb[:, 1, ch:ch + 1], u[:, 0:1], ALU.mult, ALU.add)
                    P.act(ug[m % 2][:], ug[m % 2][:], AF.Silu)
                    P.tt("pool", G[:, m, :], ug[m % 2][:], uv[m % 2][:], ALU.mult)
                for t in range(TG // 128):
                    for half in range(2):
                        for m in range(MH):
                            P.mm(pO[:, half * 512:(half + 1) * 512], G[:, m, t * 128:(t + 1) * 128],
                                 wd[:, m, half * 512:(half + 1) * 512], start=(m == 0), stop=(m == MH - 1))
                    r0 = g * TG + t * 128
                    f = ft[fi % len(ft)]
                    fi += 1
                    if s == 0:
                        P.copy("act", f[:], pO[:])
                        P.dma("sp", fpart[r0:r0 + 128, :], f[:])
                    else:
                        P.dma("sp", f[:], fpart[r0:r0 + 128, :])
                        P.tt("dve", f[:], pO[:], f[:], ALU.add)
                        sq, rs = ss[t % 4], rstd[t % 4]
                        P.act(junk[:], f[:], AF.Square, accum_out=sq[:])
                        rstd_from_ss(C, rs[:], sq[:], D_MODEL)
                        P.stt("dve", f[:], f[:], rs[:], gpost[:], ALU.mult, ALU.mult)
                        P.tt("pool", f[:], f[:], xts[t][:], ALU.add)
                        P.dma("sp", h_out[r0:r0 + 128, :], f[:])
        P.barrier()


SSD_DI = 2048
SSD_H = 32
SSD_P = 64
SSD_G = 4
SSD_N = 128
SSD_CONVD = SSD_DI + 2 * SSD_G * SSD_N


def emit_ssd_a(C, h_in, y_pre, g_pre, w_in, conv_w, conv_b, dt_bias, a_log, d_skip, ntok, seq=SEQ):
    P = C.P
    KC = D_MODEL // 128
    TG = 512
    NT = TG // 128
    NFC = SSD_CONVD // 128
    XO = SSD_DI
    NW = SSD_CONVD + SSD_H
    with ExitStack() as es:
        cwb = load_featmajor_params(C, es, [conv_w[0], conv_w[1], conv_w[2], conv_w[3], conv_b], NFC, "scwb")
        gpre = load_featmajor_params(C, es, [g_pre], KC, "sgpre")
        wx = sb(C, es, "wx", [128, KC, NW], BF16)
        for k in range(KC):
            load_cast(C, wx[:, k, :], w_in[k * 128:(k + 1) * 128, XO:XO + NW])
        dtb = sb(C, es, "dtb", [128, SSD_H], F32)
        a_bc = sb(C, es, "a_bc", [128, SSD_H], F32)
        d_bc = sb(C, es, "d_bc", [128, SSD_H], F32)
        P.dma("sp", dtb[:], dt_bias.partition_broadcast(128))
        P.dma("sp", a_bc[:], a_log.partition_broadcast(128))
        P.dma("sp", d_bc[:], d_skip.partition_broadcast(128))
        P.act(a_bc[:], a_bc[:], AF.Exp)
        P.ts("dve", a_bc[:], a_bc[:], -1.0, ALU.mult)
        Mf = sb(C, es, "Mf", [128, 128], F32)
        Uf = sb(C, es, "Uf", [128, 128], F32)
        onesf = sb(C, es, "onesf", [128, 128], F32)
        P.memset("pool", onesf[:], 1.0)
        P.I("pool", "affine_select", out=Mf[:], in_=onesf[:], pattern=[[1, 128]], compare_op=ALU.is_ge, fill=0.0,
            base=0, channel_multiplier=-1)
        P.I("pool", "affine_select", out=Uf[:], in_=onesf[:], pattern=[[-1, 128]], compare_op=ALU.is_gt, fill=0.0,
            base=0, channel_multiplier=1)

        xt = [sb(C, es, "sxt", [128, D_MODEL], F32) for _ in range(2)]
        hn = [sb(C, es, "shn", [128, D_MODEL], BF16) for _ in range(2)]
        junk = sb(C, es, "sjunk", [128, D_MODEL], BF16)
        ss = [sb(C, es, "sss", [128, 1], F32) for _ in range(2)]
        rstd = [sb(C, es, "srs", [128, 1], F32) for _ in range(2)]
        hnT = sb(C, es, "shnT", [128, KC, TG], BF16)
        dt_all = sb(C, es, "dt_all", [128, NT, SSD_H], F32)
        dtA_all = sb(C, es, "dtA_all", [128, NT, SSD_H], F32)
        dtmp = sb(C, es, "dtmp", [128, SSD_H], F32)
        u = [sb(C, es, "su", [128, TG], F32) for _ in range(2)]
        xs = [sb(C, es, "sxs", [128, TG], BF16) for _ in range(2)]
        halo = [sb(C, es, "shalo", [128, NFC, 3], F32) for _ in range(2)]
        x_tok = sb(C, es, "x_tok", [128, NT, SSD_DI], BF16)
        B_tok = sb(C, es, "B_tok", [128, NT, SSD_G * SSD_N], BF16)
        BT = sb(C, es, "BT", [128, SSD_G, TG], BF16)
        CT = sb(C, es, "CT", [128, SSD_G, TG], BF16)
        state = sb(C, es, "state", [128, SSD_DI], F32)
        state_bf = sb(C, es, "state_bf", [128, SSD_DI], BF16)
        Rg = [sb(C, es, "Rg", [128, 8, 128], F32) for _ in range(2)]
        lmT = [sb(C, es, "lmT", [128, 8, 128], F32) for _ in range(2)]
        wT = [sb(C, es, "wT", [128, 8, 128], BF16) for _ in range(2)]
        cbm = [sb(C, es, "cbm", [128, 128], F32) for _ in range(2)]
        xd = sb(C, es, "xd", [128, SSD_DI], BF16)
        xdsk = sb(C, es, "xdsk", [128, SSD_DI], F32)
        xdec = sb(C, es, "xdec", [128, SSD_DI], BF16)
        tmpg = [sb(C, es, "tmpg", [128, 512], F32) for _ in range(2)]
        y_sb = sb(C, es, "y_sb", [128, SSD_DI], F32)
        acs_sb = sb(C, es, "acs_sb", [128, SSD_H], F32)
        E_sb = sb(C, es, "E_sb", [128, SSD_H], F32)
        dec = sb(C, es, "dec", [128, SSD_H], F32)
        Etot = sb(C, es, "Etot", [128, SSD_H], F32)

        pT = ps(C, es, "spT", [128, 8, 128], BF16)
        pY = [ps(C, es, "spY", [128, TG], F32) for _ in range(2)]
        pS = ps(C, es, "spS", [128, 8, 128], F32)
        pC = ps(C, es, "spC", [128, 512], F32)
        pD = ps(C, es, "spD", [128, 512], F32)
        pD2 = ps(C, es, "spD2", [128, 512], F32)

        def bc_h(ap_h, n):
            return ap_h.unsqueeze(2).broadcast_to([128, ap_h.shape[1], n])

        ngroups = ntok // TG
        for g in range(ngroups):
            seq_start = (g * TG) % seq == 0
            for t in range(NT):
                x = xt[t % 2]
                r0 = g * TG + t * 128
                P.dma("sp", x[:], h_in[r0:r0 + 128, :])
                hb = hn[t % 2]
                P.act(junk[:], x[:], AF.Square, accum_out=ss[t % 2][:])
                rstd_from_ss(C, rstd[t % 2][:], ss[t % 2][:], D_MODEL)
                P.ts("pool", hb[:], x[:], rstd[t % 2][:], ALU.mult)
                transpose_rows(C, hb, pT, hnT, t * 128, KC, gpre, ev=("dve" if t % 2 else "act"))
            for t in range(NT):
                for k in range(KC):
                    P.mm(pD[:, 0:SSD_H], hnT[:, k, t * 128:(t + 1) * 128], wx[:, k, SSD_CONVD:NW],
                         start=(k == 0), stop=(k == KC - 1))
                P.tt("dve", dtmp[:], pD[:, 0:SSD_H], dtb[:], ALU.add)
                P.act(dtmp[:], dtmp[:], AF.Exp)
                P.act(dt_all[:, t, :], dtmp[:], AF.Ln, bias=1.0)
                P.tt("dve", dtA_all[:, t, :], dt_all[:, t, :], a_bc[:], ALU.mult)
            for fc in range(NFC):
                y = pY[fc % 2]
                for k in range(KC):
                    P.mm(y[:], wx[:, k, fc * 128:(fc + 1) * 128], hnT[:, k, :], start=(k == 0), stop=(k == KC - 1))
                uu = u[fc % 2]
                P.act(uu[:], y[:], AF.Identity, scale=cwb[:, 3, fc:fc + 1], bias=cwb[:, 4, fc:fc + 1])
                P.copy("act", halo[g % 2][:, fc, :], y[:, TG - 3:TG])
                for j in range(1, 4):
                    P.stt("dve", uu[:, j:TG], y[:, 0:TG - j], cwb[:, 3 - j, fc:fc + 1], uu[:, j:TG], ALU.mult, ALU.add)
                if not seq_start:
                    hp = halo[(g + 1) % 2]
                    for j in range(1, 4):
                        P.stt("dve", uu[:, 0:j], hp[:, fc, 3 - j:3], cwb[:, 3 - j, fc:fc + 1], uu[:, 0:j], ALU.mult, ALU.add)
                if fc < 16:
                    xx = xs[fc % 2]
                    P.act(xx[:], uu[:], AF.Silu)
                    for t in range(NT):
                        P.tr(pT[:, t, :], xx[:, t * 128:(t + 1) * 128], C.ident[:])
                    P.copy("dve" if fc % 2 else "act", x_tok[:, :, fc * 128:(fc + 1) * 128], pT[:, 0:NT, :])
                elif fc < 20:
                    P.act(BT[:, fc - 16, :], uu[:], AF.Silu)
                    for t in range(NT):
                        P.tr(pT[:, t, :], BT[:, fc - 16, t * 128:(t + 1) * 128], C.ident[:])
                    P.copy("dve" if fc % 2 else "act", B_tok[:, :, (fc - 16) * 128:(fc - 15) * 128], pT[:, 0:NT, :])
                else:
                    P.act(CT[:, fc - 20, :], uu[:], AF.Silu)
            for t in range(NT):
                first = seq_start and t == 0
                r0 = g * TG + t * 128
                c0 = t * 128
                dtA = dtA_all[:, t, :]
                dt = dt_all[:, t, :]
                xv = x_tok[:, t, :].rearrange("p (h d) -> p h d", d=SSD_P)
                if first:
                    P.memset("pool", state[:], 0.0)
                    P.memset("pool", state_bf[:], 0.0)
                P.mm(pD[:, 0:SSD_H], Mf[:], dtA, start=True, stop=True)
                P.mm(pD2[:, 0:SSD_H], onesf[:], dtA, start=True, stop=True)
                P.copy("act", acs_sb[:], pD[:, 0:SSD_H])
                P.act(E_sb[:], pD[:, 0:SSD_H], AF.Exp)
                P.tt("dve", dec[:], pD2[:, 0:SSD_H], acs_sb[:], ALU.subtract)
                P.act(dec[:], dec[:], AF.Exp)
                P.tt("dve", dec[:], dec[:], dt, ALU.mult)
                P.act(Etot[:], pD2[:, 0:SSD_H], AF.Exp)
                P.tt("pool", xd[:].rearrange("p (h d) -> p h d", d=SSD_P), xv, bc_h(dt, SSD_P), ALU.mult)
                P.tt("pool", xdsk[:].rearrange("p (h d) -> p h d", d=SSD_P), xv, bc_h(d_bc[:], SSD_P), ALU.mult)
                P.tt("pool", xdec[:].rearrange("p (h d) -> p h d", d=SSD_P), xv, bc_h(dec[:], SSD_P), ALU.mult)
                for gg in range(SSD_G):
                    P.mm(pC[:, gg * 128:(gg + 1) * 128], BT[:, gg, c0:c0 + 128], CT[:, gg, c0:c0 + 128], start=True, stop=True)
                for gg in range(SSD_G):
                    R = Rg[gg % 2]
                    hs = slice(gg * 8, (gg + 1) * 8)
                    P.tt("pool", R[:], Mf[:].unsqueeze(1).broadcast_to([128, 8, 128]), bc_h(dtA_all[:, t, hs], 128), ALU.mult)
                    for half in range(2):
                        P.mm(pS[:, half * 4:(half + 1) * 4, :], Uf[:], R[:, half * 4:(half + 1) * 4, :], start=True, stop=True)
                    lm = lmT[gg % 2]
                    P.act(lm[:], pS[:], AF.Exp)
                    cm = cbm[gg % 2]
                    P.tt("dve", cm[:], pC[:, gg * 128:(gg + 1) * 128], Mf[:], ALU.mult)
                    w = wT[gg % 2]
                    P.tt("dve", w[:], lm[:], cm[:].unsqueeze(1).broadcast_to([128, 8, 128]), ALU.mult)
                    yi = pY[0]
                    for hg in range(8):
                        hh = gg * 8 + hg
                        P.mm(yi[:, hg * 64:(hg + 1) * 64], w[:, hg, :], xd[:, hh * 64:(hh + 1) * 64], start=True, stop=True)
                    cs = slice(gg * 512, (gg + 1) * 512)
                    if first:
                        P.tt("dve", y_sb[:, cs], yi[:], xdsk[:, cs], ALU.add)
                    else:
                        yo = pY[1]
                        P.mm(yo[:], CT[:, gg, c0:c0 + 128], state_bf[:, cs], start=True, stop=True)
                        tg_ = tmpg[gg % 2]
                        P.tt("dve", tg_[:].rearrange("p (h d) -> p h d", d=SSD_P), yo[:].rearrange("p (h d) -> p h d", d=SSD_P),
                             bc_h(E_sb[:, hs], SSD_P), ALU.mult)
                        P.tt("pool", tg_[:], tg_[:], xdsk[:, cs], ALU.add)
                        P.tt("dve", y_sb[:, cs], yi[:], tg_[:], ALU.add)
                    P.mm(pD[:], B_tok[:, t, gg * 128:(gg + 1) * 128], xdec[:, cs], start=True, stop=True)
                    if first:
                        P.copy("act", state[:, cs], pD[:])
                    else:
                        P.tt("pool", state[:, cs].rearrange("p (h d) -> p h d", d=SSD_P),
                             state[:, cs].rearrange("p (h d) -> p h d", d=SSD_P), bc_h(Etot[:, hs], SSD_P), ALU.mult)
                        P.tt("dve", state[:, cs], pD[:], state[:, cs], ALU.add)
                    P.copy("act", state_bf[:, cs], state[:, cs])
                P.dma("sp", y_pre[r0:r0 + 128, :], y_sb[:])
        P.barrier()


def emit_out(C, mode, h_in, h_out, src, w_out, g_post, ntok, g_pre=None, w_in=None, norm_w=None):
    P = C.P
    KC = D_MODEL // 128
    K = SSD_DI if mode == "ssd" else D_MODEL
    KO = K // 128
    with ExitStack() as es:
        wo = sb(C, es, "wo", [128, KO, D_MODEL], BF16)
        for k in range(KO):
            load_cast(C, wo[:, k, :], w_out[k * 128:(k + 1) * 128, :])
        gpost = sb(C, es, "ogpost", [128, D_MODEL], F32)
        P.dma("sp", gpost[:], g_post.partition_broadcast(128))
        if mode == "ssd":
            gpre = load_featmajor_params(C, es, [g_pre], KC, "ogpre")
            wz = sb(C, es, "wz", [128, KC, SSD_DI], BF16)
            for k in range(KC):
                load_cast(C, wz[:, k, :], w_in[k * 128:(k + 1) * 128, 0:SSD_DI])
            nw = sb(C, es, "nw", [128, SSD_DI], F32)
            P.dma("sp", nw[:], norm_w.partition_broadcast(128))
            hn = [sb(C, es, "ohn", [128, D_MODEL], BF16) for _ in range(2)]
            hnT = [sb(C, es, "ohnT", [128, KC, 128], BF16) for _ in range(2)]
            zs = [sb(C, es, "zs", [128, SSD_DI], F32) for _ in range(2)]
            ssg = [sb(C, es, "ssg", [128, 4], F32) for _ in range(2)]
            rsg = [sb(C, es, "rsg", [128, 4], F32) for _ in range(2)]
            pZ = ps(C, es, "pZ", [128, SSD_DI], F32)
        xt = [sb(C, es, "oxt", [128, D_MODEL], F32) for _ in range(3)]
        yt = [sb(C, es, "oyt", [128, K], F32) for _ in range(2)]
        yb = [sb(C, es, "oyb", [128, K], BF16) for _ in range(2)]
        yT = [sb(C, es, "oyT", [128, KO, 128], BF16) for _ in range(2)]
        junk = sb(C, es, "ojunk", [128, D_MODEL], BF16)
        ss = [sb(C, es, "oss", [128, 1], F32) for _ in range(2)]
        rstd = [sb(C, es, "ors", [128, 1], F32) for _ in range(2)]
        ft = [sb(C, es, "oft", [128, D_MODEL], F32) for _ in range(2)]
        pT = [ps(C, es, "opT", [128, 8, 128], BF16) for _ in range(2)]
        pO = ps(C, es, "opO", [128, D_MODEL], F32)
        junk2 = sb(C, es, "ojunk2", [128, 512], BF16)
        junk3 = sb(C, es, "ojunk3", [128, D_MODEL], BF16)
        ss2 = [sb(C, es, "oss2", [128, 1], F32) for _ in range(2)]
        rstd2 = [sb(C, es, "ors2", [128, 1], F32) for _ in range(2)]
        NTILE = ntok // 128

        def front(t):
            r0 = t * 128
            x = xt[t % 3]
            y = yt[t % 2]
            ybf = yb[t % 2]
            P.dma("sp", x[:], h_in[r0:r0 + 128, :])
            P.dma("sp", y[:], src[r0:r0 + 128, :])
            if mode == "ssd":
                hb = hn[t % 2]
                P.act(junk[:], x[:], AF.Square, accum_out=ss[t % 2][:])
                rstd_from_ss(C, rstd[t % 2][:], ss[t % 2][:], D_MODEL)
                P.ts("pool", hb[:], x[:], rstd[t % 2][:], ALU.mult)
                hT = hnT[t % 2]
                transpose_rows(C, hb, pT[0], hT, 0, KC, gpre, ev="dve")
                for nch in range(4):
                    for k in range(KC):
                        P.mm(pZ[:, nch * 512:(nch + 1) * 512], hT[:, k, :], wz[:, k, nch * 512:(nch + 1) * 512],
                             start=(k == 0), stop=(k == KC - 1))
                z = zs[t % 2]
                sg, rg = ssg[t % 2], rsg[t % 2]
                for nch in range(4):
                    cs = slice(nch * 512, (nch + 1) * 512)
                    P.act(z[:, cs], pZ[:, cs], AF.Silu)
                    P.tt("pool", y[:, cs], y[:, cs], z[:, cs], ALU.mult)
                    P.act(junk2[:, 0:512], y[:, cs], AF.Square, accum_out=sg[:, nch:nch + 1])
                P.act(rg[:], sg[:], AF.Sqrt, scale=1.0 / 512, bias=RMS_EPS)
                P.I("dve", "reciprocal", out=rg[:], in_=rg[:])
                y3 = y[:].rearrange("p (g d) -> p g d", g=4)
                P.tt("dve", y3, y3, rg[:].unsqueeze(2).broadcast_to([128, 4, 512]), ALU.mult)
                P.tt("pool", ybf[:], y[:], nw[:], ALU.mult)
            else:
                P.copy("pool", ybf[:], y[:])

        def back(t):
            r0 = t * 128
            x = xt[t % 3]
            transpose_rows(C, yb[t % 2], pT[1], yT[t % 2], 0, KO, None, ev="act")
            for half in range(2):
                for k in range(KO):
                    P.mm(pO[:, half * 512:(half + 1) * 512], yT[t % 2][:, k, :], wo[:, k, half * 512:(half + 1) * 512],
                         start=(k == 0), stop=(k == KO - 1))
            f = ft[t % 2]
            P.act(junk3[:], pO[:], AF.Square, accum_out=ss2[t % 2][:])
            rstd_from_ss(C, rstd2[t % 2][:], ss2[t % 2][:], D_MODEL)
            P.stt("dve", f[:], pO[:], rstd2[t % 2][:], gpost[:], ALU.mult, ALU.mult)
            P.tt("pool", f[:], f[:], x[:], ALU.add)
            P.dma("sp", h_out[r0:r0 + 128, :], f[:])

        front(0)
        for t in range(NTILE):
            if t + 1 < NTILE:
                front(t + 1)
            back(t)
        P.barrier()


NSA_H = 16
NSA_HD = 64
NSA_G = 4
NSA_PROJ = 2608
NEG = -30000.0


def nsa_consts(S):
    nsb = S // 64
    ncmp = (S - 32) // 16 + 1
    cs = np.arange(ncmp) * 16
    sblk = np.arange(nsb) * 64
    ov = np.minimum(cs[:, None] + 32, sblk[None, :] + 64) - np.maximum(cs[:, None], sblk[None, :])
    ov = np.clip(ov, 0, None).astype(np.float32) / 32
    ov_aug = np.zeros((128, 1 + nsb), np.float32)
    ov_aug[:, 0] = 1.0
    ov_aug[:ncmp, 1:] = ov
    pos = np.arange(S)
    blk = np.arange(nsb)[None, :]
    cur = (pos // 64)[:, None]
    forced = (blk == 0) | ((blk <= cur) & (blk > cur - 2))
    forced = np.where(forced, 1e9, 0.0).astype(np.float32)
    E = (np.arange(S)[None, :] // 64 == np.arange(nsb)[:, None]).astype(np.float32)
    return dict(c_ov=ov_aug, c_forced=forced, c_E=E)


def emit_nsa_a(C, h_in, g_pre, w_in, qT_d, kT_d, v_d, gates_d, ntok, seq=SEQ):
    P = C.P
    KC = D_MODEL // 128
    TG = 512
    NT = TG // 128
    with ExitStack() as es:
        gpre = load_featmajor_params(C, es, [g_pre], KC, "ngpre")
        wq = sb(C, es, "wq", [128, KC, NSA_PROJ], BF16)
        for k in range(KC):
            load_cast(C, wq[:, k, :], w_in[k * 128:(k + 1) * 128, :])
        xt = [sb(C, es, "nxt", [128, D_MODEL], F32) for _ in range(2)]
        hn = [sb(C, es, "nhn", [128, D_MODEL], BF16) for _ in range(2)]
        junk = sb(C, es, "njunk", [128, D_MODEL], BF16)
        ss = [sb(C, es, "nss", [128, 1], F32) for _ in range(2)]
        rstd = [sb(C, es, "nrs", [128, 1], F32) for _ in range(2)]
        hnT = sb(C, es, "nhnT", [128, KC, TG], BF16)
        fm = [sb(C, es, "nfm", [64, TG], BF16) for _ in range(4)]
        vt = [sb(C, es, "nvt", [128, 512], BF16) for _ in range(2)]
        gt = [sb(C, es, "ngt", [128, 48], F32) for _ in range(2)]
        pT = ps(C, es, "npT", [128, 8, 128], BF16)
        pY = [ps(C, es, "npY", [128, TG], F32) for _ in range(2)]
        pV = ps(C, es, "npV", [128, 512], F32)
        pG = ps(C, es, "npG", [128, 512], F32)
        for g in range(ntok // TG):
            sidx = (g * TG) // seq
            s0 = (g * TG) % seq
            for t in range(NT):
                x = xt[t % 2]
                r0 = g * TG + t * 128
                P.dma("sp", x[:], h_in[r0:r0 + 128, :])
                hb = hn[t % 2]
                P.act(junk[:], x[:], AF.Square, accum_out=ss[t % 2][:])
                rstd_from_ss(C, rstd[t % 2][:], ss[t % 2][:], D_MODEL)
                P.ts("pool", hb[:], x[:], rstd[t % 2][:], ALU.mult)
                transpose_rows(C, hb, pT, hnT, t * 128, KC, gpre, ev=("dve" if t % 2 else "act"))
            for blk in range(32):
                if blk < 16:
                    col = blk * 64
                    dst = qT_d[sidx, blk, :, s0:s0 + TG]
                else:
                    b2 = blk - 16
                    typ, gg = b2 // 4, b2 % 4
                    col = 1024 + (0, 1, 2, 4)[typ] * 256 + gg * 64
                    dst = kT_d[sidx, typ, gg, :, s0:s0 + TG]
                y = pY[blk % 2]
                for k in range(KC):
                    P.mm(y[0:64, :], wq[:, k, col:col + 64], hnT[:, k, :], start=(k == 0), stop=(k == KC - 1))
                f = fm[blk % 4]
                P.copy("act" if blk % 2 == 0 else "dve", f[:], y[0:64, :])
                P.dma("sp", dst, f[:])
            for t in range(NT):
                hsl = slice(t * 128, (t + 1) * 128)
                for vi, c0 in enumerate((1792, 2304)):
                    for k in range(KC):
                        P.mm(pV[:, vi * 256:(vi + 1) * 256], hnT[:, k, hsl], wq[:, k, c0:c0 + 256], start=(k == 0), stop=(k == KC - 1))
                for k in range(KC):
                    P.mm(pG[:, 0:48], hnT[:, k, hsl], wq[:, k, 2560:2608], start=(k == 0), stop=(k == KC - 1))
                v = vt[t % 2]
                P.copy("dve", v[:], pV[:])
                P.dma("sp", v_d[sidx, 0, s0 + t * 128:s0 + (t + 1) * 128, :], v[:, 0:256])
                P.dma("sp", v_d[sidx, 1, s0 + t * 128:s0 + (t + 1) * 128, :], v[:, 256:512])
                gg_ = gt[t % 2]
                P.act(gg_[:], pG[:, 0:48], AF.Sigmoid)
                r0 = g * TG + t * 128
                P.dma("sp", gates_d[r0:r0 + 128, :], gg_[:])
        P.barrier()


def emit_nsa_c(C, qT_d, kT_d, v_d, gates_d, o_d, cmp_pos, cmp_w1, cmp_w2, c_ov, c_forced, c_E, nseq, seq=SEQ):
    P = C.P
    S = seq
    NQ = S // 128
    NSB = S // 64
    NCMP = (S - 32) // 16 + 1
    assert NCMP <= 127 and NSB <= 32 and NSB >= 8
    NSEL = min(16, NSB)
    WT = 4
    VW = 65 + NSB
    with ExitStack() as es:
        E = sb(C, es, "cE", [NSB, S], BF16)
        P.dma("pool", E[:], c_E[:, :])
        forced = sb(C, es, "cforced", [128, NQ, NSB], F32)
        P.dma("sp", forced[:], c_forced.rearrange("(t p) j -> p t j", p=128))
        onesf = sb(C, es, "aones", [128, 512], F32)
        P.memset("pool", onesf[:], 0.0)
        negU = sb(C, es, "negU", [128, 4, 128], BF16)
        negL = sb(C, es, "negL", [128, 4, 128], BF16)
        zf = onesf[:].rearrange("p (a b) -> p a b", a=4)
        P.I("pool", "affine_select", out=negU[:], in_=zf, pattern=[[0, 4], [1, 128]], compare_op=ALU.is_ge, fill=NEG,
            base=0, channel_multiplier=-1)
        P.I("pool", "affine_select", out=negL[:], in_=zf, pattern=[[0, 4], [-1, 128]], compare_op=ALU.is_gt, fill=NEG,
            base=0, channel_multiplier=1)
        negC = sb(C, es, "negC", [128, NQ, 128], BF16)
        for i in range(NQ):
            P.I("pool", "affine_select", out=negC[:, i, :], in_=onesf[:, 0:128], pattern=[[1, 128]], compare_op=ALU.is_ge,
                fill=NEG, base=128 * i - 31, channel_multiplier=-16)
        w1s = [sb(C, es, "w1s", [64, 32, 256], BF16) for _ in range(2)]
        w2s = [sb(C, es, "w2s", [128, 2, 64], BF16) for _ in range(2)]
        posT = [sb(C, es, "posT", [64, 32], F32) for _ in range(2)]
        with ExitStack() as es2:
            praw = sb(C, es2, "praw", [32, 64], F32)
            pp = ps(C, es2, "ppos", [64, 32], F32)
            for kv in range(2):
                P.dma("pool", w1s[kv][:], cmp_w1[kv].rearrange("(l d) j -> d l j", d=64))
                P.dma("pool", w2s[kv][:], cmp_w2[kv].rearrange("(a p) d -> p a d", p=128))
                P.dma("sp", praw[:], cmp_pos[kv])
                P.tr(pp[:, :], praw[:, :], C.ident_f[0:32, 0:32])
                P.copy("dve", posT[kv][:], pp[:, :])
            P.barrier()
        qT = sb(C, es, "qT", [64, 4, S], BF16)
        kTs = sb(C, es, "kTs", [64, S], BF16)
        kTw = sb(C, es, "kTw", [64, S], BF16)
        tT = [sb(C, es, "tT", [64, S], BF16) for _ in range(2)]
        tl = sb(C, es, "tl", [64, 32, NCMP], BF16)
        hgT = sb(C, es, "hgT", [128, 2, 128], BF16)
        g_sq = sb(C, es, "g_sq", [128, NCMP], F32)
        g_in = sb(C, es, "g_in", [128, NCMP], F32)
        kcT = sb(C, es, "kcT", [64, 128], BF16)
        Vc = sb(C, es, "Vc", [128, VW], BF16)
        Vs = sb(C, es, "Vs", [128, NQ, 65], BF16)
        Vw = sb(C, es, "Vw", [128, NQ, 65], BF16)
        ovf = sb(C, es, "ovf", [128, 1 + NSB], F32)
        gts = sb(C, es, "gts", [128, NQ, 12], F32)
        PTc = sb(C, es, "PTc", [128, 512], BF16)
        PTs = sb(C, es, "PTs", [128, NQ, 512], BF16)
        PTw = sb(C, es, "PTw", [128, WT + 1, 512], BF16)
        rden = [sb(C, es, "rden", [128, 4], F32) for _ in range(3)]
        coef = [sb(C, es, "coef", [128, 4], F32) for _ in range(3)]
        o_acc = [sb(C, es, "o_acc", [128, 4, 64], F32) for _ in range(2)]
        tmpo = [sb(C, es, "tmpo", [128, 4, 64], F32) for _ in range(2)]
        tmp4 = sb(C, es, "tmp4", [128, 4, NSB], F32)
        score = sb(C, es, "score", [128, NSB], F32)
        score2 = sb(C, es, "score2", [128, NSB], F32)
        m8a = sb(C, es, "m8a", [128, 8], F32)
        m8b = sb(C, es, "m8b", [128, 8], F32)
        negq = sb(C, es, "negq", [128, NSB], F32)
        negT = [sb(C, es, "negT", [NSB, 128], BF16) for _ in range(2)]
        pS = [ps(C, es, "apS", [128, 512], F32) for _ in range(2)]
        pOc = ps(C, es, "apOc", [128, 4, 128], F32)
        pOs = ps(C, es, "apOs", [128, 4, 128], F32)
        pOw = ps(C, es, "apOw", [128, 4, 128], F32)
        pN = ps(C, es, "apN", [128, 512], F32)
        P.dma("sp", ovf[:], c_ov[:, :])
        P.memset("pool", hgT[:], 0.0)
        P.memset("pool", Vs[:], 1.0)
        P.memset("pool", Vw[:], 1.0)

        def bc4(ap4, n):
            return ap4.unsqueeze(2).broadcast_to([128, 4, n])

        for sq_ in range(nseq):
            for g in range(NSA_G):
                P.dma("sp", qT[:], qT_d[sq_, 4 * g:4 * g + 4].rearrange("h d s -> d h s"))
                P.dma("sp", tT[0][:], kT_d[sq_, 0, g])
                P.dma("sp", tT[1][:], kT_d[sq_, 1, g])
                P.dma("sp", kTs[:], kT_d[sq_, 2, g])
                P.dma("sp", kTw[:], kT_d[sq_, 3, g])
                P.dma("sp", Vs[:, :, 0:64], v_d[sq_, 0, :, g * 64:(g + 1) * 64].rearrange("(t p) d -> p t d", p=128))
                P.dma("sp", Vw[:, :, 0:64], v_d[sq_, 1, :, g * 64:(g + 1) * 64].rearrange("(t p) d -> p t d", p=128))
                P.dma("sp", gts[:], gates_d[sq_ * S:(sq_ + 1) * S, g * 12:(g + 1) * 12].rearrange("(t p) c -> p t c", p=128))
                for kv in range(2):
                    for l in range(32):
                        b0 = (l // 16) * 16
                        src = tT[kv][:, b0:b0 + NCMP * 16].rearrange("p (c r) -> p c r", r=16)[:, :, l % 16]
                        P.ts("dve" if l % 2 else "pool", tl[:, l, :], src, posT[kv][:, l:l + 1], ALU.add)
                    for jh in range(2):
                        for l in range(32):
                            P.mm(pS[jh][:, 0:NCMP], w1s[kv][:, l, jh * 128:(jh + 1) * 128], tl[:, l, :], start=(l == 0), stop=(l == 31))
                    for jh in range(2):
                        xg = pS[jh][:, 0:NCMP]
                        P.act(g_sq[:], xg, AF.Square)
                        P.ts("dve", g_sq[:], g_sq[:], 0.044715, ALU.mult, 1.0, ALU.add)
                        P.tt("dve", g_in[:], g_sq[:], xg, ALU.mult)
                        P.act(g_in[:], g_in[:], AF.Sigmoid, scale=1.5957691216057308)
                        P.tt("dve", hgT[:, jh, 0:NCMP], g_in[:], xg, ALU.mult)
                    if kv == 0:
                        for jh in range(2):
                            P.mm(pN[0:64, 0:128], w2s[0][:, jh, :], hgT[:, jh, :], start=(jh == 0), stop=(jh == 1))
                        P.copy("act", kcT[:], pN[0:64, 0:128])
                    else:
                        for jh in range(2):
                            P.mm(pN[:, 0:64], hgT[:, jh, :], w2s[1][:, jh, :], start=(jh == 0), stop=(jh == 1))
                        P.copy("act", Vc[:, 0:64], pN[:, 0:64])
                        P.copy("pool", Vc[:, 64:VW], ovf[:])
                def cmp_topk(i):
                    qv = qT[:, :, i * 128:(i + 1) * 128]
                    oa = o_acc[i % 2]
                    sc = pS[0]
                    P.mm(sc[:], kcT[:], qv, start=True, stop=False)
                    P.mm(sc[:], C.ident[:], negC[:, i, :].unsqueeze(1).broadcast_to([128, 4, 128]), start=False, stop=True)
                    P.act(PTc[:], sc[:], AF.Exp, scale=0.125)
                    for h in range(4):
                        P.mm(pOc[:, h, 0:VW], PTc[:, h * 128:(h + 1) * 128], Vc[:], start=True, stop=True)
                    P.ts("dve", rden[0][:], pOc[:, :, 64], 1e-30, ALU.add)
                    P.I("dve", "reciprocal", out=rden[0][:], in_=rden[0][:])
                    P.tt("dve", coef[0][:], rden[0][:], gts[:, i, 0:12:3], ALU.mult)
                    P.tt("dve", oa[:], pOc[:, :, 0:64], bc4(coef[0][:], 64), ALU.mult)
                    P.tt("dve", tmp4[:], pOc[:, :, 65:VW], bc4(rden[0][:], NSB), ALU.mult)
                    P.I("dve", "tensor_reduce", out=score[:], in_=tmp4[:].rearrange("p h j -> p j h"), axis=AX.X, op=ALU.add)
                    P.tt("dve", score[:], score[:], forced[:, i, :], ALU.add)
                    P.I("dve", "max", out=m8a[:], in_=score[:])
                    thr = m8a
                    if NSEL > 8:
                        P.I("dve", "match_replace", out=score2[:], in_to_replace=m8a[:], in_values=score[:], imm_value=-1.0)
                        P.I("dve", "max", out=m8b[:], in_=score2[:])
                        thr = m8b
                    P.ts("dve", negq[:], score[:], thr[:, 7:8], ALU.is_lt, NEG, ALU.mult)
                    P.tr(pN[0:NSB, 0:128], negq[:, :], C.ident_f[:, :])
                    P.copy("act", negT[i % 2][:], pN[0:NSB, 0:128])

                def win(i):
                    qv = qT[:, :, i * 128:(i + 1) * 128]
                    j0 = max(0, i - WT)
                    for j in range(j0, i + 1):
                        sc = pS[j % 2]
                        ks = slice(j * 128, (j + 1) * 128)
                        last_plain = not (j == i or j == i - WT)
                        P.mm(sc[:], kTw[:, ks], qv, start=True, stop=last_plain)
                        if j == i:
                            P.mm(sc[:], C.ident[:], negU[:], start=False, stop=True)
                        elif j == i - WT:
                            P.mm(sc[:], C.ident[:], negL[:], start=False, stop=True)
                        P.act(PTw[:, j - j0, :], sc[:], AF.Exp, scale=0.125)
                    for h in range(4):
                        for j in range(j0, i + 1):
                            P.mm(pOw[:, h, 0:65], PTw[:, j - j0, h * 128:(h + 1) * 128], Vw[:, j, :], start=(j == j0), stop=(j == i))
                    P.I("dve", "reciprocal", out=rden[2][:], in_=pOw[:, :, 64])
                    P.tt("dve", coef[2][:], rden[2][:], gts[:, i, 2:12:3], ALU.mult)
                    P.tt("dve", tmpo[0][:], pOw[:, :, 0:64], bc4(coef[2][:], 64), ALU.mult)
                    P.tt("pool", o_acc[i % 2][:], o_acc[i % 2][:], tmpo[0][:], ALU.add)

                def sel(i):
                    qv = qT[:, :, i * 128:(i + 1) * 128]
                    nT = negT[i % 2]
                    oa = o_acc[i % 2]
                    for j in range(i + 1):
                        sc = pS[(j + 1) % 2]
                        ks = slice(j * 128, (j + 1) * 128)
                        P.mm(sc[:], kTs[:, ks], qv, start=True, stop=False)
                        P.mm(sc[:], E[:, ks], nT[:].unsqueeze(1).broadcast_to([NSB, 4, 128]), start=False, stop=(j != i))
                        if j == i:
                            P.mm(sc[:], C.ident[:], negU[:], start=False, stop=True)
                        P.act(PTs[:, j, :], sc[:], AF.Exp, scale=0.125)
                    for h in range(4):
                        for j in range(i + 1):
                            P.mm(pOs[:, h, 0:65], PTs[:, j, h * 128:(h + 1) * 128], Vs[:, j, :], start=(j == 0), stop=(j == i))
                    P.I("dve", "reciprocal", out=rden[1][:], in_=pOs[:, :, 64])
                    P.tt("dve", coef[1][:], rden[1][:], gts[:, i, 1:12:3], ALU.mult)
                    P.tt("dve", tmpo[1][:], pOs[:, :, 0:64], bc4(coef[1][:], 64), ALU.mult)
                    P.tt("pool", oa[:], oa[:], tmpo[1][:], ALU.add)
                    r0 = sq_ * S + i * 128
                    P.dma("sp", o_d[r0:r0 + 128, g * 256:(g + 1) * 256], oa[:].rearrange("p h d -> p (h d)"))

                cmp_topk(0)
                for i in range(NQ):
                    if i + 1 < NQ:
                        cmp_topk(i + 1)
                    win(i)
                    sel(i)
        P.barrier()


DEPTH = 4
NSEQ_CORE = 2
W_SHAPES = {
    "norm_gains": [4, 4, 1024],
    "nsa_w_in": [2, 1024, 2608], "nsa_cmp_pos": [2, 2, 32, 64], "nsa_cmp_w1": [2, 2, 2048, 256],
    "nsa_cmp_w2": [2, 2, 256, 64], "nsa_w_out": [2, 1024, 1024],
    "ssd_w_in": [2, 1024, 5152], "ssd_conv_w": [2, 4, 3072], "ssd_conv_b": [2, 3072], "ssd_dt_bias": [2, 32],
    "ssd_a_log": [2, 32], "ssd_d": [2, 32], "ssd_norm_w": [2, 2048], "ssd_w_out": [2, 2048, 1024],
    "ffn_w_up": [4, 1024, 5632], "ffn_conv_w": [4, 3, 5632], "ffn_conv_b": [4, 5632], "ffn_w_down": [4, 2816, 1024],
}


def build_program(nseq=NSEQ_CORE, seq=SEQ, depth=DEPTH):
    nc = bass.Bass("TRN2", target_bir_lowering=False)
    ntok = nseq * seq
    nsb = seq // 64
    W = {k: nc.dram_tensor(k, v, F32, kind="ExternalInput").ap() for k, v in W_SHAPES.items()}
    x = nc.dram_tensor("x", [ntok, D_MODEL], F32, kind="ExternalInput").ap()
    c_ov = nc.dram_tensor("c_ov", [128, 1 + nsb], F32, kind="ExternalInput").ap()
    c_forced = nc.dram_tensor("c_forced", [seq, nsb], F32, kind="ExternalInput").ap()
    c_E = nc.dram_tensor("c_E", [nsb, seq], F32, kind="ExternalInput").ap()
    out = nc.dram_tensor("out", [ntok, D_MODEL], F32, kind="ExternalOutput").ap()

    def scratch(name, shape, dt=F32):
        return nc.dram_tensor(name, shape, dt, kind="Internal").ap()
    hA = scratch("hA", [ntok, D_MODEL])
    hB = scratch("hB", [ntok, D_MODEL])
    fpart = scratch("fpart", [ntok, D_MODEL])
    y_pre = scratch("y_pre", [ntok, SSD_DI])
    o_d = scratch("o_d", [ntok, D_MODEL])
    gates_d = scratch("gates_d", [ntok, 48])
    qT_d = scratch("qT_d", [nseq, NSA_H, 64, seq], BF16)
    kT_d = scratch("kT_d", [nseq, 4, NSA_G, 64, seq], BF16)
    v_d = scratch("v_d", [nseq, 2, seq, 256], BF16)

    P = Prog(nc)
    P.nodep |= set(W_SHAPES) | {"x", "c_ov", "c_forced", "c_E"}
    C = Ctx(nc, P)
    with ExitStack() as es:
        emit_consts(C, es)
        P.barrier()
        cur = x
        for i in range(depth):
            g = W["norm_gains"][i]
            slot = i // 2
            if i % 2 == 0:
                emit_nsa_a(C, cur, g[0], W["nsa_w_in"][slot], qT_d, kT_d, v_d, gates_d, ntok, seq=seq)
                emit_nsa_c(C, qT_d, kT_d, v_d, gates_d, o_d, W["nsa_cmp_pos"][slot], W["nsa_cmp_w1"][slot],
                           W["nsa_cmp_w2"][slot], c_ov, c_forced, c_E, nseq, seq=seq)
                emit_out(C, "nsa", cur, hA, o_d, W["nsa_w_out"][slot], g[1], ntok)
            else:
                emit_ssd_a(C, cur, y_pre, g[0], W["ssd_w_in"][slot], W["ssd_conv_w"][slot], W["ssd_conv_b"][slot],
                           W["ssd_dt_bias"][slot], W["ssd_a_log"][slot], W["ssd_d"][slot], ntok, seq=seq)
                emit_out(C, "ssd", cur, hA, y_pre, W["ssd_w_out"][slot], g[1], ntok,
                         g_pre=g[0], w_in=W["ssd_w_in"][slot], norm_w=W["ssd_norm_w"][slot])
            dst = out if i == depth - 1 else hB
            emit_ffn(C, hA, dst, fpart, g[2], g[3], W["ffn_w_up"][i], W["ffn_conv_w"][i], W["ffn_conv_b"][i],
                     W["ffn_w_down"][i], ntok, seq=seq)
            cur = hB
        P.barrier()
    return nc, P


def kernel(**inputs):
    x = np.ascontiguousarray(inputs["x"], dtype=np.float32)
    B, S, D = x.shape
    assert (B, S, D) == (N_CORES * NSEQ_CORE, SEQ, D_MODEL)
    nc, _ = build_program()
    consts = nsa_consts(SEQ)
    shared = {k: np.ascontiguousarray(inputs[k], dtype=np.float32) for k in W_SHAPES}
    shared.update(consts)
    in_maps = []
    for c in range(N_CORES):
        m = dict(shared)
        m["x"] = x[c * NSEQ_CORE:(c + 1) * NSEQ_CORE].reshape(NSEQ_CORE * SEQ, D_MODEL)
        in_maps.append(m)
    res = run_bass_kernel_spmd(nc, in_maps, core_ids=list(range(N_CORES)))
    outs = [r["out"].reshape(NSEQ_CORE, SEQ, D_MODEL) for r in res.results]
    return np.concatenate(outs, axis=0).astype(np.float32)
```

```python
import math
from contextlib import ExitStack

import numpy as np
import concourse.bass as bass
import concourse.mybir as mybir
from concourse.bass_utils import run_bass_kernel_spmd

F32 = mybir.dt.float32
BF16 = mybir.dt.bfloat16
AF = mybir.ActivationFunctionType
ALU = mybir.AluOpType
AX = mybir.AxisListType

D_MODEL = 1024
SEQ = 2048
N_CORES = 8
FFN_HIDDEN = 2816
RMS_EPS = 1e-6

WRITE_KEYS = ("out", "accum_out", "ap")
DT_SIZE = {F32: 4, BF16: 2}


def _box(ap):
    t = ap.tensor
    tn = type(t).__name__
    off = ap.offset
    dims = list(ap.ap)
    if tn.startswith("SB") or tn.startswith("PSum"):
        pstep = 1
        for s in list(t.shape)[1:]:
            pstep *= int(s)
        p0 = off // pstep
        f0 = off % pstep
        st0, cn0 = dims[0]
        pe = (cn0 - 1) * (st0 // pstep) if cn0 > 1 else 0
        fe = 0
        for st, cn in dims[1:]:
            if cn > 1:
                assert st >= 0
                fe += (cn - 1) * st
        f1 = f0 + fe + 1
        if tn.startswith("PSum"):
            be = 2048 // DT_SIZE[t.dtype]
            f0 = (f0 // be) * be
            f1 = -(-f1 // be) * be
        return (p0, p0 + pe + 1, f0, f1)
    lo = hi = off
    for st, cn in dims:
        if cn > 1:
            if st >= 0:
                hi += (cn - 1) * st
            else:
                lo += (cn - 1) * st
    return (0, 1, lo, hi + 1)


class Prog:
    def __init__(self, nc, n_dma_sems=10, same_engine_sync=True):
        self.nc = nc
        self.e = dict(pe=nc.tensor, act=nc.scalar, dve=nc.vector, pool=nc.gpsimd, sp=nc.sync)
        self.sem = {}
        self.cnt = {}
        self.allsems = []
        for k in self.e:
            self._new_sem(k)
        self.seen = {k: {} for k in self.e}
        self.acc = {}
        self.nodep = set()
        self.dsems = {}
        self.dnext = {}
        self.dval = {}
        self.semh = {}
        for q in ("sp", "act", "pool"):
            self.dsems[q] = []
            for i in range(n_dma_sems):
                s = nc.alloc_semaphore(name=f"d_{q}_{i}")
                self.dsems[q].append(s)
                self.dval[s.num] = 0
                self.semh[s.num] = s
            self.dnext[q] = 0
        self.same = same_engine_sync
        self.ninst = 0
        self.nwait = 0

    def _new_sem(self, k):
        s = self.nc.alloc_semaphore(name=f"s_{k}_{len(self.allsems)}")
        self.sem[k] = s
        self.cnt[k] = 0
        self.allsems.append(s)

    def _wait(self, ek, sem, val):
        sn = self.seen[ek]
        if sn.get(sem.num, 0) >= val:
            return
        self.e[ek].wait_ge(sem, val)
        sn[sem.num] = val
        self.nwait += 1

    def _sync(self, ek, reads, writes):
        need = {}
        for is_w, aps in ((False, reads), (True, writes)):
            for ap in aps:
                name = ap.tensor.name
                if name in self.nodep:
                    continue
                b = _box(ap)
                is_psum = type(ap.tensor).__name__.startswith("PSum")
                for r in self.acc.get(name, ()):
                    if not (is_w or r[0]):
                        if not (is_psum and r[7] != ek):
                            continue
                    if r[1] < b[1] and b[0] < r[2] and r[3] < b[3] and b[2] < r[4]:
                        if r[7] == ek and (ek == "pe" or not self.same) and r[7] != "dma":
                            continue
                        s, v = r[5], r[6]
                        if need.get(s.num, (None, 0))[1] < v:
                            need[s.num] = (s, v)
        for s, v in need.values():
            self._wait(ek, s, v)

    def _record(self, ek, reads, writes, sem, val, is_dma=False):
        tag = "dma" if is_dma else ek
        for is_w, aps in ((False, reads), (True, writes)):
            for ap in aps:
                name = ap.tensor.name
                if name in self.nodep:
                    continue
                b = _box(ap)
                lst = self.acc.setdefault(name, [])
                keep = []
                for r in lst:
                    inside = r[1] >= b[0] and r[2] <= b[1] and r[3] >= b[2] and r[4] <= b[3]
                    if inside and (is_w or ((not r[0]) and r[5].num == sem.num)):
                        continue
                    keep.append(r)
                keep.append((is_w, b[0], b[1], b[2], b[3], sem, val, tag))
                self.acc[name] = keep

    def I(self, ek, name, **kw):
        reads, writes = [], []
        for k, v in kw.items():
            if isinstance(v, bass.AP):
                (writes if k in WRITE_KEYS else reads).append(v)
        self._sync(ek, reads, writes)
        ins = getattr(self.e[ek], name)(**kw)
        self.cnt[ek] += 1
        ins.then_inc(self.sem[ek], 1)
        self._record(ek, reads, writes, self.sem[ek], self.cnt[ek])
        self.ninst += 1
        if self.cnt[ek] >= 30000:
            self._new_sem(ek)
        return ins

    def dma(self, q, out, in_, **kw):
        reads, writes = [in_], [out]
        self._sync(q, reads, writes)
        lst = self.dsems[q]
        s = lst[self.dnext[q] % len(lst)]
        self.dnext[q] += 1
        prev = self.dval[s.num]
        if prev:
            self._wait(q, s, prev)
        ins = self.e[q].dma_start(out=out, in_=in_, **kw)
        ins.then_inc(s, 16)
        self.dval[s.num] = prev + 16
        self._record(q, reads, writes, s, prev + 16, is_dma=True)
        self.ninst += 1
        return ins

    def barrier(self, engines=None):
        for x in (engines or self.e):
            for k in self.e:
                if self.cnt[k] and not (k == x):
                    self._wait(x, self.sem[k], self.cnt[k])
            for n, v in self.dval.items():
                if v:
                    self._wait(x, self.semh[n], v)
        self.acc = {}

    def mm(self, out, lhsT, rhs, start=True, stop=True, **kw):
        return self.I("pe", "matmul", out=out, lhsT=lhsT, rhs=rhs, start=start, stop=stop, **kw)

    def tr(self, out, in_, identity):
        return self.I("pe", "transpose", out=out, in_=in_, identity=identity)

    def act(self, out, in_, func, **kw):
        return self.I("act", "activation", out=out, in_=in_, func=func, **kw)

    def ts(self, ek, out, in0, s1, op0, s2=None, op1=None, **kw):
        if op1 is None:
            s2, op1 = 0.0, ALU.add
        return self.I(ek, "tensor_scalar", out=out, in0=in0, scalar1=s1, scalar2=s2, op0=op0, op1=op1, **kw)

    def tt(self, ek, out, in0, in1, op):
        return self.I(ek, "tensor_tensor", out=out, in0=in0, in1=in1, op=op)

    def stt(self, ek, out, in0, scalar, in1, op0, op1):
        return self.I(ek, "scalar_tensor_tensor", out=out, in0=in0, scalar=scalar, in1=in1, op0=op0, op1=op1)

    def copy(self, ek, out, in_):
        if ek == "act":
            return self.I("act", "activation", out=out, in_=in_, func=AF.Copy)
        return self.I(ek, "tensor_copy", out=out, in_=in_)

    def memset(self, ek, ap, val):
        return self.I(ek, "memset", ap=ap, constant=val)


class Ctx:
    def __init__(self, nc, P):
        self.nc = nc
        self.P = P
        self.uid = 0

    def name(self, base):
        self.uid += 1
        return f"{base}_{self.uid}"


def sb(C, es, base, shape, dtype):
    return es.enter_context(C.nc.sbuf_tensor(C.name(base), shape, dtype))


def ps(C, es, base, shape, dtype=F32):
    return es.enter_context(C.nc.psum_tensor(C.name(base), shape, dtype))


def bcast_rows(ap_row, nparts):
    return ap_row.partition_broadcast(nparts)


def emit_consts(C, es):
    P = C.P
    ident_f = sb(C, es, "identf", [128, 128], F32)
    ident = sb(C, es, "ident", [128, 128], BF16)
    P.memset("pool", ident_f[:], 0.0)
    P.I("pool", "affine_select", out=ident_f[:], in_=ident_f[:], pattern=[[-1, 128]],
        compare_op=ALU.not_equal, fill=1.0, base=0, channel_multiplier=1)
    P.copy("dve", ident[:], ident_f[:])
    C.ident = ident
    C.ident_f = ident_f


def rstd_from_ss(C, rstd, ss, width):
    C.P.act(rstd, ss, AF.Sqrt, scale=1.0 / width, bias=RMS_EPS)
    C.P.I("dve", "reciprocal", out=rstd, in_=rstd)


def rmsnorm_rows(C, x_ap, gain_bc, out_ap, ss, rstd, junk, width=D_MODEL, eng="dve"):
    P = C.P
    P.act(junk, x_ap, AF.Square, accum_out=ss)
    rstd_from_ss(C, rstd, ss, width)
    P.stt(eng, out_ap, x_ap, rstd, gain_bc, ALU.mult, ALU.mult)


def transpose_rows(C, hb, pt, hT, c0, kc, gain=None, ev="dve"):
    P = C.P
    for k0 in range(0, kc, 8):
        kn = min(8, kc - k0)
        for k in range(kn):
            P.tr(pt[:, k, :], hb[:, (k0 + k) * 128:(k0 + k + 1) * 128], C.ident[:])
        dst = hT[:, k0:k0 + kn, c0:c0 + 128]
        if gain is None:
            P.copy(ev, dst, pt[:, 0:kn, :])
        elif ev == "dve":
            P.tt("dve", dst, pt[:, 0:kn, :], gain[:, 0, k0:k0 + kn].unsqueeze(2).broadcast_to([128, kn, 128]), ALU.mult)
        else:
            for k in range(kn):
                P.act(hT[:, k0 + k, c0:c0 + 128], pt[:, k, :], AF.Identity, scale=gain[:, 0, k0 + k:k0 + k + 1])


def load_cast(C, dst, w_dram_rows):
    C.P.dma("pool", dst, w_dram_rows)


def load_featmajor_params(C, es, rows, nchunk, name):
    P = C.P
    nr = len(rows)
    out = sb(C, es, name, [128, nr, nchunk], F32)
    with ExitStack() as es2:
        raw = sb(C, es2, name + "_raw", [nchunk, nr, 128], F32)
        pp = ps(C, es2, name + "_ps", [128, nchunk], F32)
        for j, r in enumerate(rows):
            P.dma("sp", raw[:, j, :], r.rearrange("(m p) -> m p", p=128))
        for j in range(nr):
            P.tr(pp[:, :], raw[:, j, :], C.ident_f[0:nchunk, 0:nchunk])
            P.copy("dve", out[:, j, :], pp[:, :])
        P.barrier()
    return out


def emit_ffn(C, h_in, h_out, fpart, g_pre, g_post, w_up, conv_w, conv_b, w_down, ntok, seq=SEQ):
    P = C.P
    F = FFN_HIDDEN
    NM = F // 128
    NS = 2
    MH = NM // NS
    KC = D_MODEL // 128
    TG = 512
    with ExitStack() as es:
        cwb = load_featmajor_params(C, es, [conv_w[0], conv_w[1], conv_w[2], conv_b], 2 * NM, "cwb")
        gpre = load_featmajor_params(C, es, [g_pre], KC, "gpre")
        wu = sb(C, es, "wu", [128, KC, 2 * MH * 128], BF16)
        wd = sb(C, es, "wd", [128, MH, D_MODEL], BF16)
        gpost = sb(C, es, "gpost", [128, D_MODEL], F32)
        xt = [sb(C, es, "xt", [128, D_MODEL], F32) for _ in range(8)]
        hn = [sb(C, es, "hn", [128, D_MODEL], BF16) for _ in range(4)]
        hnT = [sb(C, es, "hnT", [128, KC, TG], BF16) for _ in range(2)]
        gT = [sb(C, es, "gT", [128, MH, TG], BF16) for _ in range(2)]
        uv = [sb(C, es, "uv", [128, TG], F32) for _ in range(2)]
        ug = [sb(C, es, "ug", [128, TG], F32) for _ in range(2)]
        halo = [sb(C, es, "halo", [128, 2 * NM, 2], F32) for _ in range(2)]
        junk = sb(C, es, "junk", [128, D_MODEL], BF16)
        ss = [sb(C, es, "ss", [128, 1], F32) for _ in range(4)]
        rstd = [sb(C, es, "rstd", [128, 1], F32) for _ in range(4)]
        ft = [sb(C, es, "ft", [128, D_MODEL], F32) for _ in range(3)]
        pT = [ps(C, es, "pT", [128, 8, 128], BF16) for _ in range(2)]
        pY = [ps(C, es, "pY", [128, TG], F32) for _ in range(4)]
        pO = ps(C, es, "pO", [128, D_MODEL], F32)

        P.dma("sp", gpost[:], g_post.partition_broadcast(128))
        ngroups = ntok // TG
        xi = 0
        fi = 0
        for s in range(NS):
            for k in range(KC):
                rows = w_up[k * 128:(k + 1) * 128, :]
                load_cast(C, wu[:, k, 0:MH * 128], rows[:, s * MH * 128:(s + 1) * MH * 128])
                load_cast(C, wu[:, k, MH * 128:2 * MH * 128], rows[:, F + s * MH * 128:F + (s + 1) * MH * 128])
            for m in range(MH):
                r0 = (s * MH + m) * 128
                load_cast(C, wd[:, m, :], w_down[r0:r0 + 128, :])
            def norm_part(g):
                nonlocal xi
                xs_ = []
                for t in range(TG // 128):
                    x = xt[xi % len(xt)]
                    xi += 1
                    xs_.append(x)
                    r0 = g * TG + t * 128
                    P.dma("sp", x[:], h_in[r0:r0 + 128, :])
                    hb = hn[t % 4]
                    sq, rs = ss[t % 4], rstd[t % 4]
                    P.act(junk[:], x[:], AF.Square, accum_out=sq[:])
                    rstd_from_ss(C, rs[:], sq[:], D_MODEL)
                    P.ts("dve", hb[:], x[:], rs[:], ALU.mult)
                return xs_

            def tr_part(g):
                for t in range(TG // 128):
                    transpose_rows(C, hn[t % 4], pT[t % 2], hnT[g % 2], t * 128, KC, gpre, ev=("dve" if t % 2 else "act"))

            xts_next = norm_part(0)
            tr_part(0)
            for g in range(ngroups):
                seq_start = (g * TG) % seq == 0
                hT = hnT[g % 2]
                xts = xts_next
                G = gT[g % 2]
                for m in range(MH):
                    yv = pY[(2 * m) % 4]
                    yg = pY[(2 * m + 1) % 4]
                    cv = s * MH + m
                    for (y, c0) in ((yv, m * 128), (yg, (MH + m) * 128)):
                        for k in range(KC):
                            P.mm(y[:], wu[:, k, c0:c0 + 128], hT[:, k, :], start=(k == 0), stop=(k == KC - 1))
                    for (y, u, ch) in ((yv, uv[m % 2], cv), (yg, ug[m % 2], NM + cv)):
                        P.act(u[:], y[:], AF.Identity, scale=cwb[:, 2, ch:ch + 1], bias=cwb[:, 3, ch:ch + 1])
                        P.copy("act", halo[g % 2][:, ch, :], y[:, TG - 2:TG])
                        P.stt("dve", u[:, 1:TG], y[:, 0:TG - 1], cwb[:, 1, ch:ch + 1], u[:, 1:TG], ALU.mult, ALU.add)
                        P.stt("dve", u[:, 2:TG], y[:, 0:TG - 2], cwb[:, 0, ch:ch + 1], u[:, 2:TG], ALU.mult, ALU.add)
                        if not seq_start:
                            hp = halo[(g + 1) % 2]
                            P.stt("dve", u[:, 0:2], hp[:, ch, :], cwb[:, 0, ch:ch + 1], u[:, 0:2], ALU.mult, ALU.add)
                            P.stt("dve", u[:, 0:1], hp[:, ch, 1:2], cwb[:, 1, ch:ch + 1], u[:, 0:1], ALU.mult, ALU.add)
                    P.act(ug[m % 2][:], ug[m % 2][:], AF.Silu)
                    P.tt("pool", G[:, m, :], ug[m % 2][:], uv[m % 2][:], ALU.mult)
                if g + 1 < ngroups:
                    xts_next = norm_part(g + 1)
                for t in range(TG // 128):
                    for half in range(2):
                        for m in range(MH):
                            P.mm(pO[:, half * 512:(half + 1) * 512], G[:, m, t * 128:(t + 1) * 128],
                                 wd[:, m, half * 512:(half + 1) * 512], start=(m == 0), stop=(m == MH - 1))
                    r0 = g * TG + t * 128
                    f = ft[fi % len(ft)]
                    fi += 1
                    if s == 0:
                        P.copy("act", f[:], pO[:])
                        P.dma("sp", fpart[r0:r0 + 128, :], f[:])
                    else:
                        P.dma("sp", f[:], fpart[r0:r0 + 128, :])
                        P.tt("dve", f[:], pO[:], f[:], ALU.add)
                        sq, rs = ss[t % 4], rstd[t % 4]
                        P.act(junk[:], f[:], AF.Square, accum_out=sq[:])
                        rstd_from_ss(C, rs[:], sq[:], D_MODEL)
                        P.stt("dve", f[:], f[:], rs[:], gpost[:], ALU.mult, ALU.mult)
                        P.tt("pool", f[:], f[:], xts[t][:], ALU.add)
                        P.dma("sp", h_out[r0:r0 + 128, :], f[:])
                if g + 1 < ngroups:
                    tr_part(g + 1)
        P.barrier()


SSD_DI = 2048
SSD_H = 32
SSD_P = 64
SSD_G = 4
SSD_N = 128
SSD_CONVD = SSD_DI + 2 * SSD_G * SSD_N


def emit_ssd_a(C, h_in, y_pre, g_pre, w_in, conv_w, conv_b, dt_bias, a_log, d_skip, ntok, seq=SEQ):
    P = C.P
    KC = D_MODEL // 128
    TG = 512
    NT = TG // 128
    NFC = SSD_CONVD // 128
    XO = SSD_DI
    NW = SSD_CONVD + SSD_H
    with ExitStack() as es:
        cwb = load_featmajor_params(C, es, [conv_w[0], conv_w[1], conv_w[2], conv_w[3], conv_b], NFC, "scwb")
        gpre = load_featmajor_params(C, es, [g_pre], KC, "sgpre")
        wx = sb(C, es, "wx", [128, KC, NW], BF16)
        for k in range(KC):
            load_cast(C, wx[:, k, :], w_in[k * 128:(k + 1) * 128, XO:XO + NW])
        dtb = sb(C, es, "dtb", [128, SSD_H], F32)
        a_bc = sb(C, es, "a_bc", [128, SSD_H], F32)
        d_bc = sb(C, es, "d_bc", [128, SSD_H], F32)
        P.dma("sp", dtb[:], dt_bias.partition_broadcast(128))
        P.dma("sp", a_bc[:], a_log.partition_broadcast(128))
        P.dma("sp", d_bc[:], d_skip.partition_broadcast(128))
        P.act(a_bc[:], a_bc[:], AF.Exp)
        P.ts("dve", a_bc[:], a_bc[:], -1.0, ALU.mult)
        Mf = sb(C, es, "Mf", [128, 128], F32)
        Uf = sb(C, es, "Uf", [128, 128], F32)
        onesf = sb(C, es, "onesf", [128, 128], F32)
        P.memset("pool", onesf[:], 1.0)
        P.I("pool", "affine_select", out=Mf[:], in_=onesf[:], pattern=[[1, 128]], compare_op=ALU.is_ge, fill=0.0,
            base=0, channel_multiplier=-1)
        P.I("pool", "affine_select", out=Uf[:], in_=onesf[:], pattern=[[-1, 128]], compare_op=ALU.is_gt, fill=0.0,
            base=0, channel_multiplier=1)

        xt = [sb(C, es, "sxt", [128, D_MODEL], F32) for _ in range(2)]
        hn = [sb(C, es, "shn", [128, D_MODEL], BF16) for _ in range(2)]
        junk = sb(C, es, "sjunk", [128, D_MODEL], BF16)
        ss = [sb(C, es, "sss", [128, 1], F32) for _ in range(2)]
        rstd = [sb(C, es, "srs", [128, 1], F32) for _ in range(2)]
        hnT = sb(C, es, "shnT", [128, KC, TG], BF16)
        dt_all = sb(C, es, "dt_all", [128, NT, SSD_H], F32)
        dtA_all = sb(C, es, "dtA_all", [128, NT, SSD_H], F32)
        dtmp = sb(C, es, "dtmp", [128, SSD_H], F32)
        u = [sb(C, es, "su", [128, TG], F32) for _ in range(2)]
        xs = [sb(C, es, "sxs", [128, TG], BF16) for _ in range(2)]
        halo = [sb(C, es, "shalo", [128, NFC, 3], F32) for _ in range(2)]
        x_tok = sb(C, es, "x_tok", [128, NT, SSD_DI], BF16)
        B_tok = sb(C, es, "B_tok", [128, NT, SSD_G * SSD_N], BF16)
        BT = sb(C, es, "BT", [128, SSD_G, TG], BF16)
        CT = sb(C, es, "CT", [128, SSD_G, TG], BF16)
        state = sb(C, es, "state", [128, SSD_DI], F32)
        state_bf = sb(C, es, "state_bf", [128, SSD_DI], BF16)
        Rg = [sb(C, es, "Rg", [128, 8, 128], F32) for _ in range(2)]
        lmT = [sb(C, es, "lmT", [128, 8, 128], F32) for _ in range(2)]
        wT = [sb(C, es, "wT", [128, 8, 128], BF16) for _ in range(2)]
        cbm = [sb(C, es, "cbm", [128, 128], F32) for _ in range(2)]
        xd = sb(C, es, "xd", [128, SSD_DI], BF16)
        xdsk = sb(C, es, "xdsk", [128, SSD_DI], F32)
        xdec = sb(C, es, "xdec", [128, SSD_DI], BF16)
        tmpg = [sb(C, es, "tmpg", [128, 512], F32) for _ in range(2)]
        y_sb = sb(C, es, "y_sb", [128, SSD_DI], F32)
        acs_sb = sb(C, es, "acs_sb", [128, SSD_H], F32)
        E_sb = sb(C, es, "E_sb", [128, SSD_H], F32)
        dec = sb(C, es, "dec", [128, SSD_H], F32)
        Etot = sb(C, es, "Etot", [128, SSD_H], F32)

        pT = ps(C, es, "spT", [128, 8, 128], BF16)
        pY = [ps(C, es, "spY", [128, TG], F32) for _ in range(2)]
        pS = ps(C, es, "spS", [128, 8, 128], F32)
        pC = ps(C, es, "spC", [128, 512], F32)
        pD = ps(C, es, "spD", [128, 512], F32)
        pD2 = ps(C, es, "spD2", [128, 512], F32)

        def bc_h(ap_h, n):
            return ap_h.unsqueeze(2).broadcast_to([128, ap_h.shape[1], n])

        ngroups = ntok // TG
        for g in range(ngroups):
            seq_start = (g * TG) % seq == 0
            for t in range(NT):
                x = xt[t % 2]
                r0 = g * TG + t * 128
                P.dma("sp", x[:], h_in[r0:r0 + 128, :])
                hb = hn[t % 2]
                P.act(junk[:], x[:], AF.Square, accum_out=ss[t % 2][:])
                rstd_from_ss(C, rstd[t % 2][:], ss[t % 2][:], D_MODEL)
                P.ts("pool", hb[:], x[:], rstd[t % 2][:], ALU.mult)
                transpose_rows(C, hb, pT, hnT, t * 128, KC, gpre, ev=("dve" if t % 2 else "act"))
            for t in range(NT):
                for k in range(KC):
                    P.mm(pD[:, 0:SSD_H], hnT[:, k, t * 128:(t + 1) * 128], wx[:, k, SSD_CONVD:NW],
                         start=(k == 0), stop=(k == KC - 1))
                P.tt("dve", dtmp[:], pD[:, 0:SSD_H], dtb[:], ALU.add)
                P.act(dtmp[:], dtmp[:], AF.Exp)
                P.act(dt_all[:, t, :], dtmp[:], AF.Ln, bias=1.0)
                P.tt("dve", dtA_all[:, t, :], dt_all[:, t, :], a_bc[:], ALU.mult)
            for fc in range(NFC):
                y = pY[fc % 2]
                for k in range(KC):
                    P.mm(y[:], wx[:, k, fc * 128:(fc + 1) * 128], hnT[:, k, :], start=(k == 0), stop=(k == KC - 1))
                uu = u[fc % 2]
                P.act(uu[:], y[:], AF.Identity, scale=cwb[:, 3, fc:fc + 1], bias=cwb[:, 4, fc:fc + 1])
                P.copy("act", halo[g % 2][:, fc, :], y[:, TG - 3:TG])
                for j in range(1, 4):
                    P.stt("dve", uu[:, j:TG], y[:, 0:TG - j], cwb[:, 3 - j, fc:fc + 1], uu[:, j:TG], ALU.mult, ALU.add)
                if not seq_start:
                    hp = halo[(g + 1) % 2]
                    for j in range(1, 4):
                        P.stt("dve", uu[:, 0:j], hp[:, fc, 3 - j:3], cwb[:, 3 - j, fc:fc + 1], uu[:, 0:j], ALU.mult, ALU.add)
                if fc < 16:
                    xx = xs[fc % 2]
                    P.act(xx[:], uu[:], AF.Silu)
                    for t in range(NT):
                        P.tr(pT[:, t, :], xx[:, t * 128:(t + 1) * 128], C.ident[:])
                    P.copy("dve" if fc % 2 else "act", x_tok[:, :, fc * 128:(fc + 1) * 128], pT[:, 0:NT, :])
                elif fc < 20:
                    P.act(BT[:, fc - 16, :], uu[:], AF.Silu)
                    for t in range(NT):
                        P.tr(pT[:, t, :], BT[:, fc - 16, t * 128:(t + 1) * 128], C.ident[:])
                    P.copy("dve" if fc % 2 else "act", B_tok[:, :, (fc - 16) * 128:(fc - 15) * 128], pT[:, 0:NT, :])
                else:
                    P.act(CT[:, fc - 20, :], uu[:], AF.Silu)
            def stage1(t, gg):
                R = Rg[gg % 2]
                hs = slice(gg * 8, (gg + 1) * 8)
                P.tt("dve" if gg % 2 else "pool", R[:], Mf[:].unsqueeze(1).broadcast_to([128, 8, 128]),
                     bc_h(dtA_all[:, t, hs], 128), ALU.mult)
                for half in range(2):
                    P.mm(pS[:, half * 4:(half + 1) * 4, :], Uf[:], R[:, half * 4:(half + 1) * 4, :], start=True, stop=True)
                P.act(lmT[gg % 2][:], pS[:], AF.Exp)

            def prologue(t):
                first = seq_start and t == 0
                c0 = t * 128
                dtA = dtA_all[:, t, :]
                dt = dt_all[:, t, :]
                xv = x_tok[:, t, :].rearrange("p (h d) -> p h d", d=SSD_P)
                if first:
                    P.memset("pool", state[:], 0.0)
                    P.memset("pool", state_bf[:], 0.0)
                P.mm(pD[:, 0:SSD_H], Mf[:], dtA, start=True, stop=True)
                P.mm(pD2[:, 0:SSD_H], onesf[:], dtA, start=True, stop=True)
                P.copy("act", acs_sb[:], pD[:, 0:SSD_H])
                P.act(E_sb[:], pD[:, 0:SSD_H], AF.Exp)
                P.tt("dve", dec[:], pD2[:, 0:SSD_H], acs_sb[:], ALU.subtract)
                P.act(dec[:], dec[:], AF.Exp)
                P.tt("dve", dec[:], dec[:], dt, ALU.mult)
                P.act(Etot[:], pD2[:, 0:SSD_H], AF.Exp)
                P.tt("dve", xd[:].rearrange("p (h d) -> p h d", d=SSD_P), xv, bc_h(dt, SSD_P), ALU.mult)
                P.tt("pool", xdsk[:].rearrange("p (h d) -> p h d", d=SSD_P), xv, bc_h(d_bc[:], SSD_P), ALU.mult)
                P.tt("dve", xdec[:].rearrange("p (h d) -> p h d", d=SSD_P), xv, bc_h(dec[:], SSD_P), ALU.mult)
                for g4 in range(SSD_G):
                    P.mm(pC[:, g4 * 128:(g4 + 1) * 128], BT[:, g4, c0:c0 + 128], CT[:, g4, c0:c0 + 128], start=True, stop=True)

            def stage2(t, gg):
                first = seq_start and t == 0
                c0 = t * 128
                hs = slice(gg * 8, (gg + 1) * 8)
                lm = lmT[gg % 2]
                cm = cbm[gg % 2]
                P.tt("dve", cm[:], pC[:, gg * 128:(gg + 1) * 128], Mf[:], ALU.mult)
                w = wT[gg % 2]
                P.tt("dve", w[:], lm[:], cm[:].unsqueeze(1).broadcast_to([128, 8, 128]), ALU.mult)
                yi = pY[0]
                for hg in range(8):
                    hh = gg * 8 + hg
                    P.mm(yi[:, hg * 64:(hg + 1) * 64], w[:, hg, :], xd[:, hh * 64:(hh + 1) * 64], start=True, stop=True)
                cs = slice(gg * 512, (gg + 1) * 512)
                if first:
                    P.tt("dve", y_sb[:, cs], yi[:], xdsk[:, cs], ALU.add)
                else:
                    yo = pY[1]
                    P.mm(yo[:], CT[:, gg, c0:c0 + 128], state_bf[:, cs], start=True, stop=True)
                    tg_ = tmpg[gg % 2]
                    P.tt("dve", tg_[:].rearrange("p (h d) -> p h d", d=SSD_P), yo[:].rearrange("p (h d) -> p h d", d=SSD_P),
                         bc_h(E_sb[:, hs], SSD_P), ALU.mult)
                    P.tt("dve", tg_[:], tg_[:], xdsk[:, cs], ALU.add)
                    P.tt("dve", y_sb[:, cs], yi[:], tg_[:], ALU.add)
                P.mm(pD[:], B_tok[:, t, gg * 128:(gg + 1) * 128], xdec[:, cs], start=True, stop=True)
                if first:
                    P.copy("act", state[:, cs], pD[:])
                else:
                    P.tt("dve", state[:, cs].rearrange("p (h d) -> p h d", d=SSD_P),
                         state[:, cs].rearrange("p (h d) -> p h d", d=SSD_P), bc_h(Etot[:, hs], SSD_P), ALU.mult)
                    P.tt("dve", state[:, cs], pD[:], state[:, cs], ALU.add)
                P.copy("act", state_bf[:, cs], state[:, cs])
                if gg == SSD_G - 1:
                    r0 = g * TG + t * 128
                    P.dma("sp", y_pre[r0:r0 + 128, :], y_sb[:])

            items = [(t, gg) for t in range(NT) for gg in range(SSD_G)]
            stage1(*items[0])
            for k, (t, gg) in enumerate(items):
                if gg == 0:
                    prologue(t)
                if k + 1 < len(items):
                    stage1(*items[k + 1])
                stage2(t, gg)
        P.barrier()


def emit_out(C, mode, h_in, h_out, src, w_out, g_post, ntok, g_pre=None, w_in=None, norm_w=None):
    P = C.P
    KC = D_MODEL // 128
    K = SSD_DI if mode == "ssd" else D_MODEL
    KO = K // 128
    with ExitStack() as es:
        wo = sb(C, es, "wo", [128, KO, D_MODEL], BF16)
        for k in range(KO):
            load_cast(C, wo[:, k, :], w_out[k * 128:(k + 1) * 128, :])
        gpost = sb(C, es, "ogpost", [128, D_MODEL], F32)
        P.dma("sp", gpost[:], g_post.partition_broadcast(128))
        if mode == "ssd":
            gpre = load_featmajor_params(C, es, [g_pre], KC, "ogpre")
            wz = sb(C, es, "wz", [128, KC, SSD_DI], BF16)
            for k in range(KC):
                load_cast(C, wz[:, k, :], w_in[k * 128:(k + 1) * 128, 0:SSD_DI])
            nw = sb(C, es, "nw", [128, SSD_DI], F32)
            P.dma("sp", nw[:], norm_w.partition_broadcast(128))
            hn = [sb(C, es, "ohn", [128, D_MODEL], BF16) for _ in range(2)]
            hnT = [sb(C, es, "ohnT", [128, KC, 128], BF16) for _ in range(2)]
            zs = [sb(C, es, "zs", [128, SSD_DI], F32) for _ in range(2)]
            ssg = [sb(C, es, "ssg", [128, 4], F32) for _ in range(2)]
            rsg = [sb(C, es, "rsg", [128, 4], F32) for _ in range(2)]
            pZ = ps(C, es, "pZ", [128, SSD_DI], F32)
        xt = [sb(C, es, "oxt", [128, D_MODEL], F32) for _ in range(3)]
        yt = [sb(C, es, "oyt", [128, K], F32) for _ in range(2)]
        yb = [sb(C, es, "oyb", [128, K], BF16) for _ in range(2)]
        yT = [sb(C, es, "oyT", [128, KO, 128], BF16) for _ in range(2)]
        junk = sb(C, es, "ojunk", [128, D_MODEL], BF16)
        ss = [sb(C, es, "oss", [128, 1], F32) for _ in range(2)]
        rstd = [sb(C, es, "ors", [128, 1], F32) for _ in range(2)]
        ft = [sb(C, es, "oft", [128, D_MODEL], F32) for _ in range(2)]
        pT = [ps(C, es, "opT", [128, 8, 128], BF16) for _ in range(2)]
        pO = ps(C, es, "opO", [128, D_MODEL], F32)
        junk2 = sb(C, es, "ojunk2", [128, 512], BF16)
        junk3 = sb(C, es, "ojunk3", [128, D_MODEL], BF16)
        ss2 = [sb(C, es, "oss2", [128, 1], F32) for _ in range(2)]
        rstd2 = [sb(C, es, "ors2", [128, 1], F32) for _ in range(2)]
        NTILE = ntok // 128

        def front(t):
            r0 = t * 128
            x = xt[t % 3]
            y = yt[t % 2]
            ybf = yb[t % 2]
            P.dma("sp", x[:], h_in[r0:r0 + 128, :])
            P.dma("sp", y[:], src[r0:r0 + 128, :])
            if mode == "ssd":
                hb = hn[t % 2]
                P.act(junk[:], x[:], AF.Square, accum_out=ss[t % 2][:])
                rstd_from_ss(C, rstd[t % 2][:], ss[t % 2][:], D_MODEL)
                P.ts("pool", hb[:], x[:], rstd[t % 2][:], ALU.mult)
                hT = hnT[t % 2]
                transpose_rows(C, hb, pT[0], hT, 0, KC, gpre, ev="dve")
                for nch in range(4):
                    for k in range(KC):
                        P.mm(pZ[:, nch * 512:(nch + 1) * 512], hT[:, k, :], wz[:, k, nch * 512:(nch + 1) * 512],
                             start=(k == 0), stop=(k == KC - 1))
                z = zs[t % 2]
                sg, rg = ssg[t % 2], rsg[t % 2]
                for nch in range(4):
                    cs = slice(nch * 512, (nch + 1) * 512)
                    P.act(z[:, cs], pZ[:, cs], AF.Silu)
                    P.tt("dve", y[:, cs], y[:, cs], z[:, cs], ALU.mult)
                    P.act(junk2[:, 0:512], y[:, cs], AF.Square, accum_out=sg[:, nch:nch + 1])
                P.act(rg[:], sg[:], AF.Sqrt, scale=1.0 / 512, bias=RMS_EPS)
                P.I("dve", "reciprocal", out=rg[:], in_=rg[:])
                y3 = y[:].rearrange("p (g d) -> p g d", g=4)
                P.tt("dve", y3, y3, rg[:].unsqueeze(2).broadcast_to([128, 4, 512]), ALU.mult)
                P.tt("dve", ybf[:], y[:], nw[:], ALU.mult)
            else:
                P.copy("pool", ybf[:], y[:])

        def back(t):
            r0 = t * 128
            x = xt[t % 3]
            transpose_rows(C, yb[t % 2], pT[1], yT[t % 2], 0, KO, None, ev="act")
            for half in range(2):
                for k in range(KO):
                    P.mm(pO[:, half * 512:(half + 1) * 512], yT[t % 2][:, k, :], wo[:, k, half * 512:(half + 1) * 512],
                         start=(k == 0), stop=(k == KO - 1))
            f = ft[t % 2]
            P.act(junk3[:], pO[:], AF.Square, accum_out=ss2[t % 2][:])
            rstd_from_ss(C, rstd2[t % 2][:], ss2[t % 2][:], D_MODEL)
            P.stt("dve", f[:], pO[:], rstd2[t % 2][:], gpost[:], ALU.mult, ALU.mult)
            P.tt("pool", f[:], f[:], x[:], ALU.add)
            P.dma("sp", h_out[r0:r0 + 128, :], f[:])

        front(0)
        for t in range(NTILE):
            if t + 1 < NTILE:
                front(t + 1)
            back(t)
        P.barrier()


NSA_H = 16
NSA_HD = 64
NSA_G = 4
NSA_PROJ = 2608
NEG = -30000.0


def nsa_consts(S):
    nsb = S // 64
    ncmp = (S - 32) // 16 + 1
    cs = np.arange(ncmp) * 16
    sblk = np.arange(nsb) * 64
    ov = np.minimum(cs[:, None] + 32, sblk[None, :] + 64) - np.maximum(cs[:, None], sblk[None, :])
    ov = np.clip(ov, 0, None).astype(np.float32) / 32
    ov_aug = np.zeros((128, 1 + nsb), np.float32)
    ov_aug[:, 0] = 1.0
    ov_aug[:ncmp, 1:] = ov
    pos = np.arange(S)
    blk = np.arange(nsb)[None, :]
    cur = (pos // 64)[:, None]
    forced = (blk == 0) | ((blk <= cur) & (blk > cur - 2))
    forced = np.where(forced, 1e9, 0.0).astype(np.float32)
    E = (np.arange(S)[None, :] // 64 == np.arange(nsb)[:, None]).astype(np.float32)
    return dict(c_ov=ov_aug, c_forced=forced, c_E=E)


def emit_nsa_a(C, h_in, g_pre, w_in, qT_d, kT_d, v_d, gates_d, ntok, seq=SEQ):
    P = C.P
    KC = D_MODEL // 128
    TG = 512
    NT = TG // 128
    with ExitStack() as es:
        gpre = load_featmajor_params(C, es, [g_pre], KC, "ngpre")
        wq = sb(C, es, "wq", [128, KC, NSA_PROJ], BF16)
        for k in range(KC):
            load_cast(C, wq[:, k, :], w_in[k * 128:(k + 1) * 128, :])
        xt = [sb(C, es, "nxt", [128, D_MODEL], F32) for _ in range(2)]
        hn = [sb(C, es, "nhn", [128, D_MODEL], BF16) for _ in range(2)]
        junk = sb(C, es, "njunk", [128, D_MODEL], BF16)
        ss = [sb(C, es, "nss", [128, 1], F32) for _ in range(2)]
        rstd = [sb(C, es, "nrs", [128, 1], F32) for _ in range(2)]
        hnT = sb(C, es, "nhnT", [128, KC, TG], BF16)
        fm = [sb(C, es, "nfm", [64, TG], BF16) for _ in range(4)]
        vt = [sb(C, es, "nvt", [128, 512], BF16) for _ in range(2)]
        gt = [sb(C, es, "ngt", [128, 48], F32) for _ in range(2)]
        pT = ps(C, es, "npT", [128, 8, 128], BF16)
        pY = [ps(C, es, "npY", [128, TG], F32) for _ in range(2)]
        pV = ps(C, es, "npV", [128, 512], F32)
        pG = ps(C, es, "npG", [128, 512], F32)
        for g in range(ntok // TG):
            sidx = (g * TG) // seq
            s0 = (g * TG) % seq
            for t in range(NT):
                x = xt[t % 2]
                r0 = g * TG + t * 128
                P.dma("sp", x[:], h_in[r0:r0 + 128, :])
                hb = hn[t % 2]
                P.act(junk[:], x[:], AF.Square, accum_out=ss[t % 2][:])
                rstd_from_ss(C, rstd[t % 2][:], ss[t % 2][:], D_MODEL)
                P.ts("pool", hb[:], x[:], rstd[t % 2][:], ALU.mult)
                transpose_rows(C, hb, pT, hnT, t * 128, KC, gpre, ev=("dve" if t % 2 else "act"))
            for blk in range(32):
                if blk < 16:
                    col = blk * 64
                    dst = qT_d[sidx, blk, :, s0:s0 + TG]
                else:
                    b2 = blk - 16
                    typ, gg = b2 // 4, b2 % 4
                    col = 1024 + (0, 1, 2, 4)[typ] * 256 + gg * 64
                    dst = kT_d[sidx, typ, gg, :, s0:s0 + TG]
                y = pY[blk % 2]
                for k in range(KC):
                    P.mm(y[0:64, :], wq[:, k, col:col + 64], hnT[:, k, :], start=(k == 0), stop=(k == KC - 1))
                f = fm[blk % 4]
                P.copy("act" if blk % 2 == 0 else "dve", f[:], y[0:64, :])
                P.dma("sp", dst, f[:])
            for t in range(NT):
                hsl = slice(t * 128, (t + 1) * 128)
                for vi, c0 in enumerate((1792, 2304)):
                    for k in range(KC):
                        P.mm(pV[:, vi * 256:(vi + 1) * 256], hnT[:, k, hsl], wq[:, k, c0:c0 + 256], start=(k == 0), stop=(k == KC - 1))
                for k in range(KC):
                    P.mm(pG[:, 0:48], hnT[:, k, hsl], wq[:, k, 2560:2608], start=(k == 0), stop=(k == KC - 1))
                v = vt[t % 2]
                P.copy("dve", v[:], pV[:])
                P.dma("sp", v_d[sidx, 0, s0 + t * 128:s0 + (t + 1) * 128, :], v[:, 0:256])
                P.dma("sp", v_d[sidx, 1, s0 + t * 128:s0 + (t + 1) * 128, :], v[:, 256:512])
                gg_ = gt[t % 2]
                P.act(gg_[:], pG[:, 0:48], AF.Sigmoid)
                r0 = g * TG + t * 128
                P.dma("sp", gates_d[r0:r0 + 128, :], gg_[:])
        P.barrier()


def emit_nsa_c(C, qT_d, kT_d, v_d, gates_d, o_d, cmp_pos, cmp_w1, cmp_w2, c_ov, c_forced, c_E, nseq, seq=SEQ):
    P = C.P
    S = seq
    NQ = S // 128
    NSB = S // 64
    NCMP = (S - 32) // 16 + 1
    assert NCMP <= 127 and NSB <= 32 and NSB >= 8
    NSEL = min(16, NSB)
    WT = 4
    VW = 65 + NSB
    with ExitStack() as es:
        E = sb(C, es, "cE", [NSB, S], BF16)
        P.dma("pool", E[:], c_E[:, :])
        forced = sb(C, es, "cforced", [128, NQ, NSB], F32)
        P.dma("sp", forced[:], c_forced.rearrange("(t p) j -> p t j", p=128))
        onesf = sb(C, es, "aones", [128, 512], F32)
        P.memset("pool", onesf[:], 0.0)
        negU = sb(C, es, "negU", [128, 4, 128], BF16)
        negL = sb(C, es, "negL", [128, 4, 128], BF16)
        zf = onesf[:].rearrange("p (a b) -> p a b", a=4)
        P.I("pool", "affine_select", out=negU[:], in_=zf, pattern=[[0, 4], [1, 128]], compare_op=ALU.is_ge, fill=NEG,
            base=0, channel_multiplier=-1)
        P.I("pool", "affine_select", out=negL[:], in_=zf, pattern=[[0, 4], [-1, 128]], compare_op=ALU.is_gt, fill=NEG,
            base=0, channel_multiplier=1)
        negC = sb(C, es, "negC", [128, NQ, 128], BF16)
        for i in range(NQ):
            P.I("pool", "affine_select", out=negC[:, i, :], in_=onesf[:, 0:128], pattern=[[1, 128]], compare_op=ALU.is_ge,
                fill=NEG, base=128 * i - 31, channel_multiplier=-16)
        w1s = [sb(C, es, "w1s", [64, 32, 256], BF16) for _ in range(2)]
        w2s = [sb(C, es, "w2s", [128, 2, 64], BF16) for _ in range(2)]
        posT = [sb(C, es, "posT", [64, 32], F32) for _ in range(2)]
        with ExitStack() as es2:
            praw = sb(C, es2, "praw", [32, 64], F32)
            pp = ps(C, es2, "ppos", [64, 32], F32)
            for kv in range(2):
                P.dma("pool", w1s[kv][:], cmp_w1[kv].rearrange("(l d) j -> d l j", d=64))
                P.dma("pool", w2s[kv][:], cmp_w2[kv].rearrange("(a p) d -> p a d", p=128))
                P.dma("sp", praw[:], cmp_pos[kv])
                P.tr(pp[:, :], praw[:, :], C.ident_f[0:32, 0:32])
                P.copy("dve", posT[kv][:], pp[:, :])
            P.barrier()
        qT = sb(C, es, "qT", [64, 4, S], BF16)
        kTs = sb(C, es, "kTs", [64, S], BF16)
        kTw = sb(C, es, "kTw", [64, S], BF16)
        tT = [sb(C, es, "tT", [64, S], BF16) for _ in range(2)]
        tl = sb(C, es, "tl", [64, 32, NCMP], BF16)
        hgT = sb(C, es, "hgT", [128, 2, 128], BF16)
        g_sq = sb(C, es, "g_sq", [128, NCMP], F32)
        g_in = sb(C, es, "g_in", [128, NCMP], F32)
        kcT = sb(C, es, "kcT", [64, 128], BF16)
        Vc = sb(C, es, "Vc", [128, VW], BF16)
        Vs = sb(C, es, "Vs", [128, NQ, 65], BF16)
        Vw = sb(C, es, "Vw", [128, NQ, 65], BF16)
        ovf = sb(C, es, "ovf", [128, 1 + NSB], F32)
        gts = sb(C, es, "gts", [128, NQ, 12], F32)
        PTc = sb(C, es, "PTc", [128, 512], BF16)
        PTs = sb(C, es, "PTs", [128, NQ, 512], BF16)
        PTw = sb(C, es, "PTw", [128, WT + 1, 512], BF16)
        rden = [sb(C, es, "rden", [128, 4], F32) for _ in range(3)]
        coef = [sb(C, es, "coef", [128, 4], F32) for _ in range(3)]
        o_acc = [sb(C, es, "o_acc", [128, 4, 64], F32) for _ in range(2)]
        tmpo = [sb(C, es, "tmpo", [128, 4, 64], F32) for _ in range(2)]
        tmp4 = sb(C, es, "tmp4", [128, 4, NSB], F32)
        score = sb(C, es, "score", [128, NSB], F32)
        score2 = sb(C, es, "score2", [128, NSB], F32)
        m8a = sb(C, es, "m8a", [128, 8], F32)
        m8b = sb(C, es, "m8b", [128, 8], F32)
        negq = sb(C, es, "negq", [128, NSB], F32)
        negT = [sb(C, es, "negT", [NSB, 128], BF16) for _ in range(2)]
        pS = [ps(C, es, "apS", [128, 512], F32) for _ in range(2)]
        pOc = ps(C, es, "apOc", [128, 4, 128], F32)
        pOs = ps(C, es, "apOs", [128, 4, 128], F32)
        pOw = ps(C, es, "apOw", [128, 4, 128], F32)
        pN = ps(C, es, "apN", [128, 512], F32)
        P.dma("sp", ovf[:], c_ov[:, :])
        P.memset("pool", hgT[:], 0.0)
        P.memset("pool", Vs[:], 1.0)
        P.memset("pool", Vw[:], 1.0)

        def bc4(ap4, n):
            return ap4.unsqueeze(2).broadcast_to([128, 4, n])

        for sq_ in range(nseq):
            for g in range(NSA_G):
                P.dma("sp", qT[:], qT_d[sq_, 4 * g:4 * g + 4].rearrange("h d s -> d h s"))
                P.dma("sp", tT[0][:], kT_d[sq_, 0, g])
                P.dma("sp", tT[1][:], kT_d[sq_, 1, g])
                P.dma("sp", kTs[:], kT_d[sq_, 2, g])
                P.dma("sp", kTw[:], kT_d[sq_, 3, g])
                P.dma("sp", Vs[:, :, 0:64], v_d[sq_, 0, :, g * 64:(g + 1) * 64].rearrange("(t p) d -> p t d", p=128))
                P.dma("sp", Vw[:, :, 0:64], v_d[sq_, 1, :, g * 64:(g + 1) * 64].rearrange("(t p) d -> p t d", p=128))
                P.dma("sp", gts[:], gates_d[sq_ * S:(sq_ + 1) * S, g * 12:(g + 1) * 12].rearrange("(t p) c -> p t c", p=128))
                for kv in range(2):
                    for l in range(32):
                        b0 = (l // 16) * 16
                        src = tT[kv][:, b0:b0 + NCMP * 16].rearrange("p (c r) -> p c r", r=16)[:, :, l % 16]
                        P.ts("dve" if l % 2 else "pool", tl[:, l, :], src, posT[kv][:, l:l + 1], ALU.add)
                    for jh in range(2):
                        for l in range(32):
                            P.mm(pS[jh][:, 0:NCMP], w1s[kv][:, l, jh * 128:(jh + 1) * 128], tl[:, l, :], start=(l == 0), stop=(l == 31))
                    for jh in range(2):
                        xg = pS[jh][:, 0:NCMP]
                        P.act(g_sq[:], xg, AF.Square)
                        P.ts("dve", g_sq[:], g_sq[:], 0.044715, ALU.mult, 1.0, ALU.add)
                        P.tt("dve", g_in[:], g_sq[:], xg, ALU.mult)
                        P.act(g_in[:], g_in[:], AF.Sigmoid, scale=1.5957691216057308)
                        P.tt("dve", hgT[:, jh, 0:NCMP], g_in[:], xg, ALU.mult)
                    if kv == 0:
                        for jh in range(2):
                            P.mm(pN[0:64, 0:128], w2s[0][:, jh, :], hgT[:, jh, :], start=(jh == 0), stop=(jh == 1))
                        P.copy("act", kcT[:], pN[0:64, 0:128])
                    else:
                        for jh in range(2):
                            P.mm(pN[:, 0:64], hgT[:, jh, :], w2s[1][:, jh, :], start=(jh == 0), stop=(jh == 1))
                        P.copy("act", Vc[:, 0:64], pN[:, 0:64])
                        P.copy("pool", Vc[:, 64:VW], ovf[:])
                def cmp_topk(i):
                    qv = qT[:, :, i * 128:(i + 1) * 128]
                    oa = o_acc[i % 2]
                    sc = pS[0]
                    P.mm(sc[:], kcT[:], qv, start=True, stop=False)
                    P.mm(sc[:], C.ident[:], negC[:, i, :].unsqueeze(1).broadcast_to([128, 4, 128]), start=False, stop=True)
                    P.act(PTc[:], sc[:], AF.Exp, scale=0.125)
                    for h in range(4):
                        P.mm(pOc[:, h, 0:VW], PTc[:, h * 128:(h + 1) * 128], Vc[:], start=True, stop=True)
                    P.ts("dve", rden[0][:], pOc[:, :, 64], 1e-30, ALU.add)
                    P.I("dve", "reciprocal", out=rden[0][:], in_=rden[0][:])
                    P.tt("dve", coef[0][:], rden[0][:], gts[:, i, 0:12:3], ALU.mult)
                    P.tt("dve", oa[:], pOc[:, :, 0:64], bc4(coef[0][:], 64), ALU.mult)
                    P.tt("dve", tmp4[:], pOc[:, :, 65:VW], bc4(rden[0][:], NSB), ALU.mult)
                    P.I("dve", "tensor_reduce", out=score[:], in_=tmp4[:].rearrange("p h j -> p j h"), axis=AX.X, op=ALU.add)
                    P.tt("dve", score[:], score[:], forced[:, i, :], ALU.add)
                    P.I("dve", "max", out=m8a[:], in_=score[:])
                    thr = m8a
                    if NSEL > 8:
                        P.I("dve", "match_replace", out=score2[:], in_to_replace=m8a[:], in_values=score[:], imm_value=-1.0)
                        P.I("dve", "max", out=m8b[:], in_=score2[:])
                        thr = m8b
                    P.ts("dve", negq[:], score[:], thr[:, 7:8], ALU.is_lt, NEG, ALU.mult)
                    P.tr(pN[0:NSB, 0:128], negq[:, :], C.ident_f[:, :])
                    P.copy("act", negT[i % 2][:], pN[0:NSB, 0:128])

                def win_qk(i):
                    qv = qT[:, :, i * 128:(i + 1) * 128]
                    j0 = max(0, i - WT)
                    for j in range(j0, i + 1):
                        sc = pS[j % 2]
                        ks = slice(j * 128, (j + 1) * 128)
                        last_plain = not (j == i or j == i - WT)
                        P.mm(sc[:], kTw[:, ks], qv, start=True, stop=last_plain)
                        if j == i:
                            P.mm(sc[:], C.ident[:], negU[:], start=False, stop=True)
                        elif j == i - WT:
                            P.mm(sc[:], C.ident[:], negL[:], start=False, stop=True)
                        P.act(PTw[:, j - j0, :], sc[:], AF.Exp, scale=0.125)

                def win_pv(i):
                    j0 = max(0, i - WT)
                    for h in range(4):
                        for j in range(j0, i + 1):
                            P.mm(pOw[:, h, 0:65], PTw[:, j - j0, h * 128:(h + 1) * 128], Vw[:, j, :], start=(j == j0), stop=(j == i))
                    P.I("dve", "reciprocal", out=rden[2][:], in_=pOw[:, :, 64])
                    P.tt("dve", coef[2][:], rden[2][:], gts[:, i, 2:12:3], ALU.mult)
                    P.tt("dve", tmpo[0][:], pOw[:, :, 0:64], bc4(coef[2][:], 64), ALU.mult)
                    P.tt("pool", o_acc[i % 2][:], o_acc[i % 2][:], tmpo[0][:], ALU.add)

                def sel_qk(i):
                    qv = qT[:, :, i * 128:(i + 1) * 128]
                    nT = negT[i % 2]
                    for j in range(i + 1):
                        sc = pS[(j + 1) % 2]
                        ks = slice(j * 128, (j + 1) * 128)
                        P.mm(sc[:], kTs[:, ks], qv, start=True, stop=False)
                        P.mm(sc[:], E[:, ks], nT[:].unsqueeze(1).broadcast_to([NSB, 4, 128]), start=False, stop=(j != i))
                        if j == i:
                            P.mm(sc[:], C.ident[:], negU[:], start=False, stop=True)
                        P.act(PTs[:, j, :], sc[:], AF.Exp, scale=0.125)

                def sel_pv(i):
                    oa = o_acc[i % 2]
                    for h in range(4):
                        for j in range(i + 1):
                            P.mm(pOs[:, h, 0:65], PTs[:, j, h * 128:(h + 1) * 128], Vs[:, j, :], start=(j == 0), stop=(j == i))
                    P.I("dve", "reciprocal", out=rden[1][:], in_=pOs[:, :, 64])
                    P.tt("dve", coef[1][:], rden[1][:], gts[:, i, 1:12:3], ALU.mult)
                    P.tt("dve", tmpo[1][:], pOs[:, :, 0:64], bc4(coef[1][:], 64), ALU.mult)
                    P.tt("pool", oa[:], oa[:], tmpo[1][:], ALU.add)
                    r0 = sq_ * S + i * 128
                    P.dma("sp", o_d[r0:r0 + 128, g * 256:(g + 1) * 256], oa[:].rearrange("p h d -> p (h d)"))

                cmp_topk(0)
                for i in range(NQ):
                    if i + 1 < NQ:
                        cmp_topk(i + 1)
                    win_qk(i)
                    sel_qk(i)
                    win_pv(i)
                    sel_pv(i)
        P.barrier()


DEPTH = 4
NSEQ_CORE = 2
W_SHAPES = {
    "norm_gains": [4, 4, 1024],
    "nsa_w_in": [2, 1024, 2608], "nsa_cmp_pos": [2, 2, 32, 64], "nsa_cmp_w1": [2, 2, 2048, 256],
    "nsa_cmp_w2": [2, 2, 256, 64], "nsa_w_out": [2, 1024, 1024],
    "ssd_w_in": [2, 1024, 5152], "ssd_conv_w": [2, 4, 3072], "ssd_conv_b": [2, 3072], "ssd_dt_bias": [2, 32],
    "ssd_a_log": [2, 32], "ssd_d": [2, 32], "ssd_norm_w": [2, 2048], "ssd_w_out": [2, 2048, 1024],
    "ffn_w_up": [4, 1024, 5632], "ffn_conv_w": [4, 3, 5632], "ffn_conv_b": [4, 5632], "ffn_w_down": [4, 2816, 1024],
}


def build_program(nseq=NSEQ_CORE, seq=SEQ, depth=DEPTH):
    nc = bass.Bass("TRN2", target_bir_lowering=False)
    ntok = nseq * seq
    nsb = seq // 64
    W = {k: nc.dram_tensor(k, v, F32, kind="ExternalInput").ap() for k, v in W_SHAPES.items()}
    x = nc.dram_tensor("x", [ntok, D_MODEL], F32, kind="ExternalInput").ap()
    c_ov = nc.dram_tensor("c_ov", [128, 1 + nsb], F32, kind="ExternalInput").ap()
    c_forced = nc.dram_tensor("c_forced", [seq, nsb], F32, kind="ExternalInput").ap()
    c_E = nc.dram_tensor("c_E", [nsb, seq], F32, kind="ExternalInput").ap()
    out = nc.dram_tensor("out", [ntok, D_MODEL], F32, kind="ExternalOutput").ap()

    def scratch(name, shape, dt=F32):
        return nc.dram_tensor(name, shape, dt, kind="Internal").ap()
    hA = scratch("hA", [ntok, D_MODEL])
    hB = scratch("hB", [ntok, D_MODEL])
    fpart = scratch("fpart", [ntok, D_MODEL])
    y_pre = scratch("y_pre", [ntok, SSD_DI])
    o_d = scratch("o_d", [ntok, D_MODEL])
    gates_d = scratch("gates_d", [ntok, 48])
    qT_d = scratch("qT_d", [nseq, NSA_H, 64, seq], BF16)
    kT_d = scratch("kT_d", [nseq, 4, NSA_G, 64, seq], BF16)
    v_d = scratch("v_d", [nseq, 2, seq, 256], BF16)

    P = Prog(nc)
    P.nodep |= set(W_SHAPES) | {"x", "c_ov", "c_forced", "c_E"}
    C = Ctx(nc, P)
    with ExitStack() as es:
        emit_consts(C, es)
        P.barrier()
        cur = x
        for i in range(depth):
            g = W["norm_gains"][i]
            slot = i // 2
            if i % 2 == 0:
                emit_nsa_a(C, cur, g[0], W["nsa_w_in"][slot], qT_d, kT_d, v_d, gates_d, ntok, seq=seq)
                emit_nsa_c(C, qT_d, kT_d, v_d, gates_d, o_d, W["nsa_cmp_pos"][slot], W["nsa_cmp_w1"][slot],
                           W["nsa_cmp_w2"][slot], c_ov, c_forced, c_E, nseq, seq=seq)
                emit_out(C, "nsa", cur, hA, o_d, W["nsa_w_out"][slot], g[1], ntok)
            else:
                emit_ssd_a(C, cur, y_pre, g[0], W["ssd_w_in"][slot], W["ssd_conv_w"][slot], W["ssd_conv_b"][slot],
                           W["ssd_dt_bias"][slot], W["ssd_a_log"][slot], W["ssd_d"][slot], ntok, seq=seq)
                emit_out(C, "ssd", cur, hA, y_pre, W["ssd_w_out"][slot], g[1], ntok,
                         g_pre=g[0], w_in=W["ssd_w_in"][slot], norm_w=W["ssd_norm_w"][slot])
            dst = out if i == depth - 1 else hB
            emit_ffn(C, hA, dst, fpart, g[2], g[3], W["ffn_w_up"][i], W["ffn_conv_w"][i], W["ffn_conv_b"][i],
                     W["ffn_w_down"][i], ntok, seq=seq)
            cur = hB
        P.barrier()
    return nc, P


def kernel(**inputs):
    x = np.ascontiguousarray(inputs["x"], dtype=np.float32)
    B, S, D = x.shape
    assert (B, S, D) == (N_CORES * NSEQ_CORE, SEQ, D_MODEL)
    nc, _ = build_program()
    consts = nsa_consts(SEQ)
    shared = {k: np.ascontiguousarray(inputs[k], dtype=np.float32) for k in W_SHAPES}
    shared.update(consts)
    in_maps = []
    for c in range(N_CORES):
        m = dict(shared)
        m["x"] = x[c * NSEQ_CORE:(c + 1) * NSEQ_CORE].reshape(NSEQ_CORE * SEQ, D_MODEL)
        in_maps.append(m)
    res = run_bass_kernel_spmd(nc, in_maps, core_ids=list(range(N_CORES)))
    outs = [r["out"].reshape(NSEQ_CORE, SEQ, D_MODEL) for r in res.results]
    return np.concatenate(outs, axis=0).astype(np.float32)
```

```python
import math
from contextlib import ExitStack

import numpy as np
import concourse.bass as bass
import concourse.mybir as mybir
from concourse.bass_utils import run_bass_kernel_spmd

F32 = mybir.dt.float32
BF16 = mybir.dt.bfloat16
AF = mybir.ActivationFunctionType
ALU = mybir.AluOpType
AX = mybir.AxisListType

D_MODEL = 1024
SEQ = 2048
N_CORES = 8
FFN_HIDDEN = 2816
RMS_EPS = 1e-6

WRITE_KEYS = ("out", "accum_out", "ap")
DT_SIZE = {F32: 4, BF16: 2}


def _box(ap):
    t = ap.tensor
    tn = type(t).__name__
    off = ap.offset
    dims = list(ap.ap)
    if tn.startswith("SB") or tn.startswith("PSum"):
        pstep = 1
        for s in list(t.shape)[1:]:
            pstep *= int(s)
        p0 = off // pstep
        f0 = off % pstep
        st0, cn0 = dims[0]
        pe = (cn0 - 1) * (st0 // pstep) if cn0 > 1 else 0
        fe = 0
        for st, cn in dims[1:]:
            if cn > 1:
                assert st >= 0
                fe += (cn - 1) * st
        f1 = f0 + fe + 1
        if tn.startswith("PSum"):
            be = 2048 // DT_SIZE[t.dtype]
            f0 = (f0 // be) * be
            f1 = -(-f1 // be) * be
        return (p0, p0 + pe + 1, f0, f1)
    lo = hi = off
    for st, cn in dims:
        if cn > 1:
            if st >= 0:
                hi += (cn - 1) * st
            else:
                lo += (cn - 1) * st
    return (0, 1, lo, hi + 1)


class Prog:
    def __init__(self, nc, n_dma_sems=10, same_engine_sync=True):
        self.nc = nc
        self.e = dict(pe=nc.tensor, act=nc.scalar, dve=nc.vector, pool=nc.gpsimd, sp=nc.sync)
        self.sem = {}
        self.cnt = {}
        self.allsems = []
        for k in self.e:
            self._new_sem(k)
        self.seen = {k: {} for k in self.e}
        self.acc = {}
        self.nodep = set()
        self.dsems = {}
        self.dnext = {}
        self.dval = {}
        self.semh = {}
        for q in ("sp", "act", "pool"):
            self.dsems[q] = []
            for i in range(n_dma_sems):
                s = nc.alloc_semaphore(name=f"d_{q}_{i}")
                self.dsems[q].append(s)
                self.dval[s.num] = 0
                self.semh[s.num] = s
            self.dnext[q] = 0
        self.same = same_engine_sync
        self.ninst = 0
        self.nwait = 0

    def _new_sem(self, k):
        s = self.nc.alloc_semaphore(name=f"s_{k}_{len(self.allsems)}")
        self.sem[k] = s
        self.cnt[k] = 0
        self.allsems.append(s)

    def _wait(self, ek, sem, val):
        sn = self.seen[ek]
        if sn.get(sem.num, 0) >= val:
            return
        self.e[ek].wait_ge(sem, val)
        sn[sem.num] = val
        self.nwait += 1

    def _sync(self, ek, reads, writes):
        need = {}
        for is_w, aps in ((False, reads), (True, writes)):
            for ap in aps:
                name = ap.tensor.name
                if name in self.nodep:
                    continue
                b = _box(ap)
                is_psum = type(ap.tensor).__name__.startswith("PSum")
                for r in self.acc.get(name, ()):
                    if not (is_w or r[0]):
                        if not (is_psum and r[7] != ek):
                            continue
                    if r[1] < b[1] and b[0] < r[2] and r[3] < b[3] and b[2] < r[4]:
                        if r[7] == ek and (ek == "pe" or not self.same) and r[7] != "dma":
                            continue
                        s, v = r[5], r[6]
                        if need.get(s.num, (None, 0))[1] < v:
                            need[s.num] = (s, v)
        for s, v in need.values():
            self._wait(ek, s, v)

    def _record(self, ek, reads, writes, sem, val, is_dma=False):
        tag = "dma" if is_dma else ek
        for is_w, aps in ((False, reads), (True, writes)):
            for ap in aps:
                name = ap.tensor.name
                if name in self.nodep:
                    continue
                b = _box(ap)
                lst = self.acc.setdefault(name, [])
                keep = []
                for r in lst:
                    inside = r[1] >= b[0] and r[2] <= b[1] and r[3] >= b[2] and r[4] <= b[3]
                    if inside and (is_w or ((not r[0]) and r[5].num == sem.num)):
                        continue
                    keep.append(r)
                keep.append((is_w, b[0], b[1], b[2], b[3], sem, val, tag))
                self.acc[name] = keep

    def I(self, ek, name, **kw):
        reads, writes = [], []
        for k, v in kw.items():
            if isinstance(v, bass.AP):
                (writes if k in WRITE_KEYS else reads).append(v)
        self._sync(ek, reads, writes)
        ins = getattr(self.e[ek], name)(**kw)
        self.cnt[ek] += 1
        ins.then_inc(self.sem[ek], 1)
        self._record(ek, reads, writes, self.sem[ek], self.cnt[ek])
        self.ninst += 1
        if self.cnt[ek] >= 30000:
            self._new_sem(ek)
        return ins

    def dma(self, q, out, in_, **kw):
        reads, writes = [in_], [out]
        self._sync(q, reads, writes)
        lst = self.dsems[q]
        s = lst[self.dnext[q] % len(lst)]
        self.dnext[q] += 1
        prev = self.dval[s.num]
        if prev:
            self._wait(q, s, prev)
        ins = self.e[q].dma_start(out=out, in_=in_, **kw)
        ins.then_inc(s, 16)
        self.dval[s.num] = prev + 16
        self._record(q, reads, writes, s, prev + 16, is_dma=True)
        self.ninst += 1
        return ins

    def barrier(self, engines=None):
        for x in (engines or self.e):
            for k in self.e:
                if self.cnt[k] and not (k == x):
                    self._wait(x, self.sem[k], self.cnt[k])
            for n, v in self.dval.items():
                if v:
                    self._wait(x, self.semh[n], v)
        self.acc = {}

    def mm(self, out, lhsT, rhs, start=True, stop=True, **kw):
        return self.I("pe", "matmul", out=out, lhsT=lhsT, rhs=rhs, start=start, stop=stop, **kw)

    def tr(self, out, in_, identity):
        return self.I("pe", "transpose", out=out, in_=in_, identity=identity)

    def act(self, out, in_, func, **kw):
        return self.I("act", "activation", out=out, in_=in_, func=func, **kw)

    def ts(self, ek, out, in0, s1, op0, s2=None, op1=None, **kw):
        if op1 is None:
            s2, op1 = 0.0, ALU.add
        return self.I(ek, "tensor_scalar", out=out, in0=in0, scalar1=s1, scalar2=s2, op0=op0, op1=op1, **kw)

    def tt(self, ek, out, in0, in1, op):
        return self.I(ek, "tensor_tensor", out=out, in0=in0, in1=in1, op=op)

    def stt(self, ek, out, in0, scalar, in1, op0, op1):
        return self.I(ek, "scalar_tensor_tensor", out=out, in0=in0, scalar=scalar, in1=in1, op0=op0, op1=op1)

    def copy(self, ek, out, in_):
        if ek == "act":
            return self.I("act", "activation", out=out, in_=in_, func=AF.Copy)
        return self.I(ek, "tensor_copy", out=out, in_=in_)

    def memset(self, ek, ap, val):
        return self.I(ek, "memset", ap=ap, constant=val)


class Ctx:
    def __init__(self, nc, P):
        self.nc = nc
        self.P = P
        self.uid = 0

    def name(self, base):
        self.uid += 1
        return f"{base}_{self.uid}"


def sb(C, es, base, shape, dtype):
    return es.enter_context(C.nc.sbuf_tensor(C.name(base), shape, dtype))


def ps(C, es, base, shape, dtype=F32):
    return es.enter_context(C.nc.psum_tensor(C.name(base), shape, dtype))


def bcast_rows(ap_row, nparts):
    return ap_row.partition_broadcast(nparts)


def emit_consts(C, es):
    P = C.P
    ident_f = sb(C, es, "identf", [128, 128], F32)
    ident = sb(C, es, "ident", [128, 128], BF16)
    P.memset("pool", ident_f[:], 0.0)
    P.I("pool", "affine_select", out=ident_f[:], in_=ident_f[:], pattern=[[-1, 128]],
        compare_op=ALU.not_equal, fill=1.0, base=0, channel_multiplier=1)
    P.copy("dve", ident[:], ident_f[:])
    C.ident = ident
    C.ident_f = ident_f


def rstd_from_ss(C, rstd, ss, width):
    C.P.act(rstd, ss, AF.Sqrt, scale=1.0 / width, bias=RMS_EPS)
    C.P.I("dve", "reciprocal", out=rstd, in_=rstd)


def rmsnorm_rows(C, x_ap, gain_bc, out_ap, ss, rstd, junk, width=D_MODEL, eng="dve"):
    P = C.P
    P.act(junk, x_ap, AF.Square, accum_out=ss)
    rstd_from_ss(C, rstd, ss, width)
    P.stt(eng, out_ap, x_ap, rstd, gain_bc, ALU.mult, ALU.mult)


def transpose_rows(C, hb, pt, hT, c0, kc, gain=None, ev="dve"):
    P = C.P
    for k0 in range(0, kc, 8):
        kn = min(8, kc - k0)
        for k in range(kn):
            P.tr(pt[:, k, :], hb[:, (k0 + k) * 128:(k0 + k + 1) * 128], C.ident[:])
        dst = hT[:, k0:k0 + kn, c0:c0 + 128]
        if gain is None:
            P.copy(ev, dst, pt[:, 0:kn, :])
        elif ev == "dve":
            P.tt("dve", dst, pt[:, 0:kn, :], gain[:, 0, k0:k0 + kn].unsqueeze(2).broadcast_to([128, kn, 128]), ALU.mult)
        else:
            for k in range(kn):
                P.act(hT[:, k0 + k, c0:c0 + 128], pt[:, k, :], AF.Identity, scale=gain[:, 0, k0 + k:k0 + k + 1])


def load_cast(C, dst, w_dram_rows):
    C.P.dma("pool", dst, w_dram_rows)


def load_featmajor_params(C, es, rows, nchunk, name):
    P = C.P
    nr = len(rows)
    out = sb(C, es, name, [128, nr, nchunk], F32)
    with ExitStack() as es2:
        raw = sb(C, es2, name + "_raw", [nchunk, nr, 128], F32)
        pp = ps(C, es2, name + "_ps", [128, nchunk], F32)
        for j, r in enumerate(rows):
            P.dma("sp", raw[:, j, :], r.rearrange("(m p) -> m p", p=128))
        for j in range(nr):
            P.tr(pp[:, :], raw[:, j, :], C.ident_f[0:nchunk, 0:nchunk])
            P.copy("dve", out[:, j, :], pp[:, :])
        P.barrier()
    return out


def emit_ffn(C, h_in, h_out, fpart, g_pre, g_post, w_up, conv_w, conv_b, w_down, ntok, seq=SEQ):
    P = C.P
    F = FFN_HIDDEN
    NM = F // 128
    NS = 2
    MH = NM // NS
    KC = D_MODEL // 128
    TG = 512
    with ExitStack() as es:
        cwb = load_featmajor_params(C, es, [conv_w[0], conv_w[1], conv_w[2], conv_b], 2 * NM, "cwb")
        gpre = load_featmajor_params(C, es, [g_pre], KC, "gpre")
        wu = sb(C, es, "wu", [128, KC, 2 * MH * 128], BF16)
        wd = sb(C, es, "wd", [128, MH, D_MODEL], BF16)
        gpost = sb(C, es, "gpost", [128, D_MODEL], F32)
        xt = [sb(C, es, "xt", [128, D_MODEL], F32) for _ in range(8)]
        hn = [sb(C, es, "hn", [128, D_MODEL], BF16) for _ in range(4)]
        hnT = [sb(C, es, "hnT", [128, KC, TG], BF16) for _ in range(2)]
        gT = [sb(C, es, "gT", [128, MH, TG], BF16) for _ in range(2)]
        uv = [sb(C, es, "uv", [128, TG], F32) for _ in range(2)]
        ug = [sb(C, es, "ug", [128, TG], F32) for _ in range(2)]
        halo = [sb(C, es, "halo", [128, 2 * NM, 2], F32) for _ in range(2)]
        junk = sb(C, es, "junk", [128, D_MODEL], BF16)
        ss = [sb(C, es, "ss", [128, 1], F32) for _ in range(4)]
        rstd = [sb(C, es, "rstd", [128, 1], F32) for _ in range(4)]
        ft = [sb(C, es, "ft", [128, D_MODEL], F32) for _ in range(3)]
        pT = [ps(C, es, "pT", [128, 8, 128], BF16) for _ in range(2)]
        pY = [ps(C, es, "pY", [128, TG], F32) for _ in range(4)]
        pO = ps(C, es, "pO", [128, D_MODEL], F32)

        P.dma("sp", gpost[:], g_post.partition_broadcast(128))
        ngroups = ntok // TG
        xi = 0
        fi = 0
        for s in range(NS):
            for k in range(KC):
                rows = w_up[k * 128:(k + 1) * 128, :]
                load_cast(C, wu[:, k, 0:MH * 128], rows[:, s * MH * 128:(s + 1) * MH * 128])
                load_cast(C, wu[:, k, MH * 128:2 * MH * 128], rows[:, F + s * MH * 128:F + (s + 1) * MH * 128])
            for m in range(MH):
                r0 = (s * MH + m) * 128
                load_cast(C, wd[:, m, :], w_down[r0:r0 + 128, :])
            def norm_part(g):
                nonlocal xi
                xs_ = []
                for t in range(TG // 128):
                    x = xt[xi % len(xt)]
                    xi += 1
                    xs_.append(x)
                    r0 = g * TG + t * 128
                    P.dma("sp", x[:], h_in[r0:r0 + 128, :])
                    hb = hn[t % 4]
                    sq, rs = ss[t % 4], rstd[t % 4]
                    P.act(junk[:], x[:], AF.Square, accum_out=sq[:])
                    rstd_from_ss(C, rs[:], sq[:], D_MODEL)
                    P.ts("dve", hb[:], x[:], rs[:], ALU.mult)
                return xs_

            def tr_part(g):
                for t in range(TG // 128):
                    transpose_rows(C, hn[t % 4], pT[t % 2], hnT[g % 2], t * 128, KC, gpre, ev=("dve" if t % 2 else "act"))

            xts_next = norm_part(0)
            tr_part(0)
            for g in range(ngroups):
                seq_start = (g * TG) % seq == 0
                hT = hnT[g % 2]
                xts = xts_next
                G = gT[g % 2]
                for m in range(MH):
                    yv = pY[(2 * m) % 4]
                    yg = pY[(2 * m + 1) % 4]
                    cv = s * MH + m
                    for (y, c0) in ((yv, m * 128), (yg, (MH + m) * 128)):
                        for k in range(KC):
                            P.mm(y[:], wu[:, k, c0:c0 + 128], hT[:, k, :], start=(k == 0), stop=(k == KC - 1))
                    for (y, u, ch) in ((yv, uv[m % 2], cv), (yg, ug[m % 2], NM + cv)):
                        P.act(u[:], y[:], AF.Identity, scale=cwb[:, 2, ch:ch + 1], bias=cwb[:, 3, ch:ch + 1])
                        P.copy("act", halo[g % 2][:, ch, :], y[:, TG - 2:TG])
                        P.stt("dve", u[:, 1:TG], y[:, 0:TG - 1], cwb[:, 1, ch:ch + 1], u[:, 1:TG], ALU.mult, ALU.add)
                        P.stt("dve", u[:, 2:TG], y[:, 0:TG - 2], cwb[:, 0, ch:ch + 1], u[:, 2:TG], ALU.mult, ALU.add)
                        if not seq_start:
                            hp = halo[(g + 1) % 2]
                            P.stt("dve", u[:, 0:2], hp[:, ch, :], cwb[:, 0, ch:ch + 1], u[:, 0:2], ALU.mult, ALU.add)
                            P.stt("dve", u[:, 0:1], hp[:, ch, 1:2], cwb[:, 1, ch:ch + 1], u[:, 0:1], ALU.mult, ALU.add)
                    P.act(ug[m % 2][:], ug[m % 2][:], AF.Silu)
                    P.tt("pool", G[:, m, :], ug[m % 2][:], uv[m % 2][:], ALU.mult)
                if g + 1 < ngroups:
                    xts_next = norm_part(g + 1)
                for t in range(TG // 128):
                    for half in range(2):
                        for m in range(MH):
                            P.mm(pO[:, half * 512:(half + 1) * 512], G[:, m, t * 128:(t + 1) * 128],
                                 wd[:, m, half * 512:(half + 1) * 512], start=(m == 0), stop=(m == MH - 1))
                    r0 = g * TG + t * 128
                    f = ft[fi % len(ft)]
                    fi += 1
                    if s == 0:
                        P.copy("act", f[:], pO[:])
                        P.dma("sp", fpart[r0:r0 + 128, :], f[:])
                    else:
                        P.dma("sp", f[:], fpart[r0:r0 + 128, :])
                        P.tt("dve", f[:], pO[:], f[:], ALU.add)
                        sq, rs = ss[t % 4], rstd[t % 4]
                        P.act(junk[:], f[:], AF.Square, accum_out=sq[:])
                        rstd_from_ss(C, rs[:], sq[:], D_MODEL)
                        P.stt("dve", f[:], f[:], rs[:], gpost[:], ALU.mult, ALU.mult)
                        P.tt("pool", f[:], f[:], xts[t][:], ALU.add)
                        P.dma("sp", h_out[r0:r0 + 128, :], f[:])
                if g + 1 < ngroups:
                    tr_part(g + 1)
        P.barrier()


SSD_DI = 2048
SSD_H = 32
SSD_P = 64
SSD_G = 4
SSD_N = 128
SSD_CONVD = SSD_DI + 2 * SSD_G * SSD_N


def emit_ssd_a(C, h_in, y_pre, g_pre, w_in, conv_w, conv_b, dt_bias, a_log, d_skip, ntok, seq=SEQ):
    P = C.P
    KC = D_MODEL // 128
    TG = 512
    NT = TG // 128
    NFC = SSD_CONVD // 128
    XO = SSD_DI
    NW = SSD_CONVD + SSD_H
    with ExitStack() as es:
        cwb = load_featmajor_params(C, es, [conv_w[0], conv_w[1], conv_w[2], conv_w[3], conv_b], NFC, "scwb")
        gpre = load_featmajor_params(C, es, [g_pre], KC, "sgpre")
        wx = sb(C, es, "wx", [128, KC, NW], BF16)
        for k in range(KC):
            load_cast(C, wx[:, k, :], w_in[k * 128:(k + 1) * 128, XO:XO + NW])
        dtb = sb(C, es, "dtb", [128, SSD_H], F32)
        a_bc = sb(C, es, "a_bc", [128, SSD_H], F32)
        d_bc = sb(C, es, "d_bc", [128, SSD_H], F32)
        P.dma("sp", dtb[:], dt_bias.partition_broadcast(128))
        P.dma("sp", a_bc[:], a_log.partition_broadcast(128))
        P.dma("sp", d_bc[:], d_skip.partition_broadcast(128))
        P.act(a_bc[:], a_bc[:], AF.Exp)
        P.ts("dve", a_bc[:], a_bc[:], -1.0, ALU.mult)
        Mf = sb(C, es, "Mf", [128, 128], F32)
        Uf = sb(C, es, "Uf", [128, 128], F32)
        onesf = sb(C, es, "onesf", [128, 128], F32)
        P.memset("pool", onesf[:], 1.0)
        P.I("pool", "affine_select", out=Mf[:], in_=onesf[:], pattern=[[1, 128]], compare_op=ALU.is_ge, fill=0.0,
            base=0, channel_multiplier=-1)
        P.I("pool", "affine_select", out=Uf[:], in_=onesf[:], pattern=[[-1, 128]], compare_op=ALU.is_gt, fill=0.0,
            base=0, channel_multiplier=1)

        xt = [sb(C, es, "sxt", [128, D_MODEL], F32) for _ in range(2)]
        hn = [sb(C, es, "shn", [128, D_MODEL], BF16) for _ in range(2)]
        junk = sb(C, es, "sjunk", [128, D_MODEL], BF16)
        ss = [sb(C, es, "sss", [128, 1], F32) for _ in range(2)]
        rstd = [sb(C, es, "srs", [128, 1], F32) for _ in range(2)]
        hnT = sb(C, es, "shnT", [128, KC, TG], BF16)
        dt_all = sb(C, es, "dt_all", [128, NT, SSD_H], F32)
        dtA_all = sb(C, es, "dtA_all", [128, NT, SSD_H], F32)
        dtmp = sb(C, es, "dtmp", [128, SSD_H], F32)
        u = [sb(C, es, "su", [128, TG], F32) for _ in range(2)]
        xs = [sb(C, es, "sxs", [128, TG], BF16) for _ in range(2)]
        halo = [sb(C, es, "shalo", [128, NFC, 3], F32) for _ in range(2)]
        x_tok = sb(C, es, "x_tok", [128, NT, SSD_DI], BF16)
        B_tok = sb(C, es, "B_tok", [128, NT, SSD_G * SSD_N], BF16)
        BT = sb(C, es, "BT", [128, SSD_G, TG], BF16)
        CT = sb(C, es, "CT", [128, SSD_G, TG], BF16)
        state = sb(C, es, "state", [128, SSD_DI], F32)
        state_bf = sb(C, es, "state_bf", [128, SSD_DI], BF16)
        Rg = [sb(C, es, "Rg", [128, 8, 128], F32) for _ in range(2)]
        lmT = [sb(C, es, "lmT", [128, 8, 128], F32) for _ in range(2)]
        wT = [sb(C, es, "wT", [128, 8, 128], BF16) for _ in range(2)]
        cbm = [sb(C, es, "cbm", [128, 128], F32) for _ in range(2)]
        xd = sb(C, es, "xd", [128, SSD_DI], BF16)
        xdsk = sb(C, es, "xdsk", [128, SSD_DI], F32)
        xdec = sb(C, es, "xdec", [128, SSD_DI], BF16)
        tmpg = [sb(C, es, "tmpg", [128, 512], F32) for _ in range(2)]
        y_sb = sb(C, es, "y_sb", [128, SSD_DI], F32)
        acs_sb = sb(C, es, "acs_sb", [128, SSD_H], F32)
        E_sb = sb(C, es, "E_sb", [128, SSD_H], F32)
        dec = sb(C, es, "dec", [128, SSD_H], F32)
        Etot = sb(C, es, "Etot", [128, SSD_H], F32)

        pT = ps(C, es, "spT", [128, 8, 128], BF16)
        pY = [ps(C, es, "spY", [128, TG], F32) for _ in range(2)]
        pS = ps(C, es, "spS", [128, 8, 128], F32)
        pC = ps(C, es, "spC", [128, 512], F32)
        pD = ps(C, es, "spD", [128, 512], F32)
        pD2 = ps(C, es, "spD2", [128, 512], F32)

        def bc_h(ap_h, n):
            return ap_h.unsqueeze(2).broadcast_to([128, ap_h.shape[1], n])

        ngroups = ntok // TG
        for g in range(ngroups):
            seq_start = (g * TG) % seq == 0
            for t in range(NT):
                x = xt[t % 2]
                r0 = g * TG + t * 128
                P.dma("sp", x[:], h_in[r0:r0 + 128, :])
                hb = hn[t % 2]
                P.act(junk[:], x[:], AF.Square, accum_out=ss[t % 2][:])
                rstd_from_ss(C, rstd[t % 2][:], ss[t % 2][:], D_MODEL)
                P.ts("pool", hb[:], x[:], rstd[t % 2][:], ALU.mult)
                transpose_rows(C, hb, pT, hnT, t * 128, KC, gpre, ev=("dve" if t % 2 else "act"))
            for t in range(NT):
                for k in range(KC):
                    P.mm(pD[:, 0:SSD_H], hnT[:, k, t * 128:(t + 1) * 128], wx[:, k, SSD_CONVD:NW],
                         start=(k == 0), stop=(k == KC - 1))
                P.tt("dve", dtmp[:], pD[:, 0:SSD_H], dtb[:], ALU.add)
                P.act(dtmp[:], dtmp[:], AF.Exp)
                P.act(dt_all[:, t, :], dtmp[:], AF.Ln, bias=1.0)
                P.tt("dve", dtA_all[:, t, :], dt_all[:, t, :], a_bc[:], ALU.mult)
            for fc in range(NFC):
                y = pY[fc % 2]
                for k in range(KC):
                    P.mm(y[:], wx[:, k, fc * 128:(fc + 1) * 128], hnT[:, k, :], start=(k == 0), stop=(k == KC - 1))
                uu = u[fc % 2]
                P.act(uu[:], y[:], AF.Identity, scale=cwb[:, 3, fc:fc + 1], bias=cwb[:, 4, fc:fc + 1])
                P.copy("act", halo[g % 2][:, fc, :], y[:, TG - 3:TG])
                for j in range(1, 4):
                    P.stt("dve", uu[:, j:TG], y[:, 0:TG - j], cwb[:, 3 - j, fc:fc + 1], uu[:, j:TG], ALU.mult, ALU.add)
                if not seq_start:
                    hp = halo[(g + 1) % 2]
                    for j in range(1, 4):
                        P.stt("dve", uu[:, 0:j], hp[:, fc, 3 - j:3], cwb[:, 3 - j, fc:fc + 1], uu[:, 0:j], ALU.mult, ALU.add)
                if fc < 16:
                    xx = xs[fc % 2]
                    P.act(xx[:], uu[:], AF.Silu)
                    for t in range(NT):
                        P.tr(pT[:, t, :], xx[:, t * 128:(t + 1) * 128], C.ident[:])
                    P.copy("dve" if fc % 2 else "act", x_tok[:, :, fc * 128:(fc + 1) * 128], pT[:, 0:NT, :])
                elif fc < 20:
                    P.act(BT[:, fc - 16, :], uu[:], AF.Silu)
                    for t in range(NT):
                        P.tr(pT[:, t, :], BT[:, fc - 16, t * 128:(t + 1) * 128], C.ident[:])
                    P.copy("dve" if fc % 2 else "act", B_tok[:, :, (fc - 16) * 128:(fc - 15) * 128], pT[:, 0:NT, :])
                else:
                    P.act(CT[:, fc - 20, :], uu[:], AF.Silu)
            def stage1(t, gg):
                R = Rg[gg % 2]
                hs = slice(gg * 8, (gg + 1) * 8)
                P.tt("dve" if gg % 2 else "pool", R[:], Mf[:].unsqueeze(1).broadcast_to([128, 8, 128]),
                     bc_h(dtA_all[:, t, hs], 128), ALU.mult)
                for half in range(2):
                    P.mm(pS[:, half * 4:(half + 1) * 4, :], Uf[:], R[:, half * 4:(half + 1) * 4, :], start=True, stop=True)
                P.act(lmT[gg % 2][:], pS[:], AF.Exp)

            def prologue(t):
                first = seq_start and t == 0
                c0 = t * 128
                dtA = dtA_all[:, t, :]
                dt = dt_all[:, t, :]
                xv = x_tok[:, t, :].rearrange("p (h d) -> p h d", d=SSD_P)
                if first:
                    P.memset("pool", state[:], 0.0)
                    P.memset("pool", state_bf[:], 0.0)
                P.mm(pD[:, 0:SSD_H], Mf[:], dtA, start=True, stop=True)
                P.mm(pD2[:, 0:SSD_H], onesf[:], dtA, start=True, stop=True)
                P.copy("act", acs_sb[:], pD[:, 0:SSD_H])
                P.act(E_sb[:], pD[:, 0:SSD_H], AF.Exp)
                P.tt("dve", dec[:], pD2[:, 0:SSD_H], acs_sb[:], ALU.subtract)
                P.act(dec[:], dec[:], AF.Exp)
                P.tt("dve", dec[:], dec[:], dt, ALU.mult)
                P.act(Etot[:], pD2[:, 0:SSD_H], AF.Exp)
                P.tt("dve", xd[:].rearrange("p (h d) -> p h d", d=SSD_P), xv, bc_h(dt, SSD_P), ALU.mult)
                P.tt("pool", xdsk[:].rearrange("p (h d) -> p h d", d=SSD_P), xv, bc_h(d_bc[:], SSD_P), ALU.mult)
                P.tt("dve", xdec[:].rearrange("p (h d) -> p h d", d=SSD_P), xv, bc_h(dec[:], SSD_P), ALU.mult)
                for g4 in range(SSD_G):
                    P.mm(pC[:, g4 * 128:(g4 + 1) * 128], BT[:, g4, c0:c0 + 128], CT[:, g4, c0:c0 + 128], start=True, stop=True)

            def stage2(t, gg):
                first = seq_start and t == 0
                c0 = t * 128
                hs = slice(gg * 8, (gg + 1) * 8)
                lm = lmT[gg % 2]
                cm = cbm[gg % 2]
                P.tt("dve", cm[:], pC[:, gg * 128:(gg + 1) * 128], Mf[:], ALU.mult)
                w = wT[gg % 2]
                P.tt("dve", w[:], lm[:], cm[:].unsqueeze(1).broadcast_to([128, 8, 128]), ALU.mult)
                yi = pY[0]
                for hg in range(8):
                    hh = gg * 8 + hg
                    P.mm(yi[:, hg * 64:(hg + 1) * 64], w[:, hg, :], xd[:, hh * 64:(hh + 1) * 64], start=True, stop=True)
                cs = slice(gg * 512, (gg + 1) * 512)
                if first:
                    P.tt("dve", y_sb[:, cs], yi[:], xdsk[:, cs], ALU.add)
                else:
                    yo = pY[1]
                    P.mm(yo[:], CT[:, gg, c0:c0 + 128], state_bf[:, cs], start=True, stop=True)
                    tg_ = tmpg[gg % 2]
                    P.tt("dve", tg_[:].rearrange("p (h d) -> p h d", d=SSD_P), yo[:].rearrange("p (h d) -> p h d", d=SSD_P),
                         bc_h(E_sb[:, hs], SSD_P), ALU.mult)
                    P.tt("dve", tg_[:], tg_[:], xdsk[:, cs], ALU.add)
                    P.tt("dve", y_sb[:, cs], yi[:], tg_[:], ALU.add)
                P.mm(pD[:], B_tok[:, t, gg * 128:(gg + 1) * 128], xdec[:, cs], start=True, stop=True)
                if first:
                    P.copy("act", state[:, cs], pD[:])
                else:
                    P.tt("dve", state[:, cs].rearrange("p (h d) -> p h d", d=SSD_P),
                         state[:, cs].rearrange("p (h d) -> p h d", d=SSD_P), bc_h(Etot[:, hs], SSD_P), ALU.mult)
                    P.tt("dve", state[:, cs], pD[:], state[:, cs], ALU.add)
                P.copy("act", state_bf[:, cs], state[:, cs])
                if gg == SSD_G - 1:
                    r0 = g * TG + t * 128
                    P.dma("sp", y_pre[r0:r0 + 128, :], y_sb[:])

            items = [(t, gg) for t in range(NT) for gg in range(SSD_G)]
            stage1(*items[0])
            for k, (t, gg) in enumerate(items):
                if gg == 0:
                    prologue(t)
                if k + 1 < len(items):
                    stage1(*items[k + 1])
                stage2(t, gg)
        P.barrier()


def emit_out(C, mode, h_in, h_out, src, w_out, g_post, ntok, g_pre=None, w_in=None, norm_w=None):
    P = C.P
    KC = D_MODEL // 128
    K = SSD_DI if mode == "ssd" else D_MODEL
    KO = K // 128
    with ExitStack() as es:
        wo = sb(C, es, "wo", [128, KO, D_MODEL], BF16)
        for k in range(KO):
            load_cast(C, wo[:, k, :], w_out[k * 128:(k + 1) * 128, :])
        gpost = sb(C, es, "ogpost", [128, D_MODEL], F32)
        P.dma("sp", gpost[:], g_post.partition_broadcast(128))
        if mode == "ssd":
            gpre = load_featmajor_params(C, es, [g_pre], KC, "ogpre")
            wz = sb(C, es, "wz", [128, KC, SSD_DI], BF16)
            for k in range(KC):
                load_cast(C, wz[:, k, :], w_in[k * 128:(k + 1) * 128, 0:SSD_DI])
            nw = sb(C, es, "nw", [128, SSD_DI], F32)
            P.dma("sp", nw[:], norm_w.partition_broadcast(128))
            hn = [sb(C, es, "ohn", [128, D_MODEL], BF16) for _ in range(2)]
            hnT = [sb(C, es, "ohnT", [128, KC, 128], BF16) for _ in range(2)]
            zs = [sb(C, es, "zs", [128, SSD_DI], F32) for _ in range(2)]
            ssg = [sb(C, es, "ssg", [128, 4], F32) for _ in range(2)]
            rsg = [sb(C, es, "rsg", [128, 4], F32) for _ in range(2)]
            pZ = ps(C, es, "pZ", [128, SSD_DI], F32)
        xt = [sb(C, es, "oxt", [128, D_MODEL], F32) for _ in range(3)]
        yt = [sb(C, es, "oyt", [128, K], F32) for _ in range(2)]
        yb = [sb(C, es, "oyb", [128, K], BF16) for _ in range(2)]
        yT = [sb(C, es, "oyT", [128, KO, 128], BF16) for _ in range(2)]
        junk = sb(C, es, "ojunk", [128, D_MODEL], BF16)
        ss = [sb(C, es, "oss", [128, 1], F32) for _ in range(2)]
        rstd = [sb(C, es, "ors", [128, 1], F32) for _ in range(2)]
        ft = [sb(C, es, "oft", [128, D_MODEL], F32) for _ in range(2)]
        pT = [ps(C, es, "opT", [128, 8, 128], BF16) for _ in range(2)]
        pO = ps(C, es, "opO", [128, D_MODEL], F32)
        junk2 = sb(C, es, "ojunk2", [128, 512], BF16)
        junk3 = sb(C, es, "ojunk3", [128, D_MODEL], BF16)
        ss2 = [sb(C, es, "oss2", [128, 1], F32) for _ in range(2)]
        rstd2 = [sb(C, es, "ors2", [128, 1], F32) for _ in range(2)]
        NTILE = ntok // 128

        def front(t):
            r0 = t * 128
            x = xt[t % 3]
            y = yt[t % 2]
            ybf = yb[t % 2]
            P.dma("sp", x[:], h_in[r0:r0 + 128, :])
            P.dma("sp", y[:], src[r0:r0 + 128, :])
            if mode == "ssd":
                hb = hn[t % 2]
                P.act(junk[:], x[:], AF.Square, accum_out=ss[t % 2][:])
                rstd_from_ss(C, rstd[t % 2][:], ss[t % 2][:], D_MODEL)
                P.ts("pool", hb[:], x[:], rstd[t % 2][:], ALU.mult)
                hT = hnT[t % 2]
                transpose_rows(C, hb, pT[0], hT, 0, KC, gpre, ev="dve")
                for nch in range(4):
                    for k in range(KC):
                        P.mm(pZ[:, nch * 512:(nch + 1) * 512], hT[:, k, :], wz[:, k, nch * 512:(nch + 1) * 512],
                             start=(k == 0), stop=(k == KC - 1))
                z = zs[t % 2]
                sg, rg = ssg[t % 2], rsg[t % 2]
                for nch in range(4):
                    cs = slice(nch * 512, (nch + 1) * 512)
                    P.act(z[:, cs], pZ[:, cs], AF.Silu)
                    P.tt("dve", y[:, cs], y[:, cs], z[:, cs], ALU.mult)
                    P.act(junk2[:, 0:512], y[:, cs], AF.Square, accum_out=sg[:, nch:nch + 1])
                P.act(rg[:], sg[:], AF.Sqrt, scale=1.0 / 512, bias=RMS_EPS)
                P.I("dve", "reciprocal", out=rg[:], in_=rg[:])
                y3 = y[:].rearrange("p (g d) -> p g d", g=4)
                P.tt("dve", y3, y3, rg[:].unsqueeze(2).broadcast_to([128, 4, 512]), ALU.mult)
                P.tt("dve", ybf[:], y[:], nw[:], ALU.mult)
            else:
                P.copy("pool", ybf[:], y[:])

        def back(t):
            r0 = t * 128
            x = xt[t % 3]
            transpose_rows(C, yb[t % 2], pT[1], yT[t % 2], 0, KO, None, ev="act")
            for half in range(2):
                for k in range(KO):
                    P.mm(pO[:, half * 512:(half + 1) * 512], yT[t % 2][:, k, :], wo[:, k, half * 512:(half + 1) * 512],
                         start=(k == 0), stop=(k == KO - 1))
            f = ft[t % 2]
            P.act(junk3[:], pO[:], AF.Square, accum_out=ss2[t % 2][:])
            rstd_from_ss(C, rstd2[t % 2][:], ss2[t % 2][:], D_MODEL)
            P.stt("dve", f[:], pO[:], rstd2[t % 2][:], gpost[:], ALU.mult, ALU.mult)
            P.tt("pool", f[:], f[:], x[:], ALU.add)
            P.dma("sp", h_out[r0:r0 + 128, :], f[:])

        front(0)
        for t in range(NTILE):
            if t + 1 < NTILE:
                front(t + 1)
            back(t)
        P.barrier()


NSA_H = 16
NSA_HD = 64
NSA_G = 4
NSA_PROJ = 2608
NEG = -30000.0


def nsa_consts(S):
    nsb = S // 64
    ncmp = (S - 32) // 16 + 1
    cs = np.arange(ncmp) * 16
    sblk = np.arange(nsb) * 64
    ov = np.minimum(cs[:, None] + 32, sblk[None, :] + 64) - np.maximum(cs[:, None], sblk[None, :])
    ov = np.clip(ov, 0, None).astype(np.float32) / 32
    ov_aug = np.zeros((128, 1 + nsb), np.float32)
    ov_aug[:, 0] = 1.0
    ov_aug[:ncmp, 1:] = ov
    pos = np.arange(S)
    blk = np.arange(nsb)[None, :]
    cur = (pos // 64)[:, None]
    forced = (blk == 0) | ((blk <= cur) & (blk > cur - 2))
    forced = np.where(forced, 1e9, 0.0).astype(np.float32)
    E = (np.arange(S)[None, :] // 64 == np.arange(nsb)[:, None]).astype(np.float32)
    return dict(c_ov=ov_aug, c_forced=forced, c_E=E)


def emit_nsa_a(C, h_in, g_pre, w_in, qT_d, kT_d, v_d, gates_d, ntok, seq=SEQ):
    P = C.P
    KC = D_MODEL // 128
    TG = 512
    NT = TG // 128
    with ExitStack() as es:
        gpre = load_featmajor_params(C, es, [g_pre], KC, "ngpre")
        wq = sb(C, es, "wq", [128, KC, NSA_PROJ], BF16)
        for k in range(KC):
            load_cast(C, wq[:, k, :], w_in[k * 128:(k + 1) * 128, :])
        xt = [sb(C, es, "nxt", [128, D_MODEL], F32) for _ in range(2)]
        hn = [sb(C, es, "nhn", [128, D_MODEL], BF16) for _ in range(2)]
        junk = sb(C, es, "njunk", [128, D_MODEL], BF16)
        ss = [sb(C, es, "nss", [128, 1], F32) for _ in range(2)]
        rstd = [sb(C, es, "nrs", [128, 1], F32) for _ in range(2)]
        hnT = sb(C, es, "nhnT", [128, KC, TG], BF16)
        fm = [sb(C, es, "nfm", [128, TG], BF16) for _ in range(4)]
        vt = [sb(C, es, "nvt", [128, 512], BF16) for _ in range(2)]
        gt = [sb(C, es, "ngt", [128, 48], F32) for _ in range(2)]
        pT = ps(C, es, "npT", [128, 8, 128], BF16)
        pY = [ps(C, es, "npY", [128, TG], F32) for _ in range(2)]
        pV = ps(C, es, "npV", [128, 512], F32)
        pG = ps(C, es, "npG", [128, 512], F32)
        for g in range(ntok // TG):
            sidx = (g * TG) // seq
            s0 = (g * TG) % seq
            for t in range(NT):
                x = xt[t % 2]
                r0 = g * TG + t * 128
                P.dma("sp", x[:], h_in[r0:r0 + 128, :])
                hb = hn[t % 2]
                P.act(junk[:], x[:], AF.Square, accum_out=ss[t % 2][:])
                rstd_from_ss(C, rstd[t % 2][:], ss[t % 2][:], D_MODEL)
                P.ts("pool", hb[:], x[:], rstd[t % 2][:], ALU.mult)
                transpose_rows(C, hb, pT, hnT, t * 128, KC, gpre, ev=("dve" if t % 2 else "act"))
            for bp in range(16):
                if bp < 8:
                    col = bp * 128
                    dsts = [qT_d[sidx, 2 * bp + e, :, s0:s0 + TG] for e in range(2)]
                else:
                    b2 = (bp - 8) * 2
                    typ, gg = b2 // 4, b2 % 4
                    col = 1024 + (0, 1, 2, 4)[typ] * 256 + gg * 64
                    dsts = [kT_d[sidx, typ, gg + e, :, s0:s0 + TG] for e in range(2)]
                y = pY[bp % 2]
                for k in range(KC):
                    P.mm(y[:, :], wq[:, k, col:col + 128], hnT[:, k, :], start=(k == 0), stop=(k == KC - 1))
                f = fm[bp % 4]
                P.copy("act" if bp % 2 == 0 else "dve", f[:], y[:, :])
                P.dma("sp", dsts[0], f[0:64, :])
                P.dma("sp", dsts[1], f[64:128, :])
            for t in range(NT):
                hsl = slice(t * 128, (t + 1) * 128)
                for vi, c0 in enumerate((1792, 2304)):
                    for k in range(KC):
                        P.mm(pV[:, vi * 256:(vi + 1) * 256], hnT[:, k, hsl], wq[:, k, c0:c0 + 256], start=(k == 0), stop=(k == KC - 1))
                for k in range(KC):
                    P.mm(pG[:, 0:48], hnT[:, k, hsl], wq[:, k, 2560:2608], start=(k == 0), stop=(k == KC - 1))
                v = vt[t % 2]
                P.copy("dve", v[:], pV[:])
                P.dma("sp", v_d[sidx, 0, s0 + t * 128:s0 + (t + 1) * 128, :], v[:, 0:256])
                P.dma("sp", v_d[sidx, 1, s0 + t * 128:s0 + (t + 1) * 128, :], v[:, 256:512])
                gg_ = gt[t % 2]
                P.act(gg_[:], pG[:, 0:48], AF.Sigmoid)
                r0 = g * TG + t * 128
                P.dma("sp", gates_d[r0:r0 + 128, :], gg_[:])
        P.barrier()


def emit_nsa_c(C, qT_d, kT_d, v_d, gates_d, o_d, cmp_pos, cmp_w1, cmp_w2, c_ov, c_forced, c_E, nseq, seq=SEQ):
    P = C.P
    S = seq
    NQ = S // 128
    NSB = S // 64
    NCMP = (S - 32) // 16 + 1
    assert NCMP <= 127 and NSB <= 32 and NSB >= 8
    NSEL = min(16, NSB)
    WT = 4
    VW = 65 + NSB
    with ExitStack() as es:
        E = sb(C, es, "cE", [NSB, S], BF16)
        P.dma("pool", E[:], c_E[:, :])
        forced = sb(C, es, "cforced", [128, NQ, NSB], F32)
        P.dma("sp", forced[:], c_forced.rearrange("(t p) j -> p t j", p=128))
        onesf = sb(C, es, "aones", [128, 512], F32)
        P.memset("pool", onesf[:], 0.0)
        negU = sb(C, es, "negU", [128, 4, 128], BF16)
        negL = sb(C, es, "negL", [128, 4, 128], BF16)
        zf = onesf[:].rearrange("p (a b) -> p a b", a=4)
        P.I("pool", "affine_select", out=negU[:], in_=zf, pattern=[[0, 4], [1, 128]], compare_op=ALU.is_ge, fill=NEG,
            base=0, channel_multiplier=-1)
        P.I("pool", "affine_select", out=negL[:], in_=zf, pattern=[[0, 4], [-1, 128]], compare_op=ALU.is_gt, fill=NEG,
            base=0, channel_multiplier=1)
        negC = sb(C, es, "negC", [128, NQ, 128], BF16)
        for i in range(NQ):
            P.I("pool", "affine_select", out=negC[:, i, :], in_=onesf[:, 0:128], pattern=[[1, 128]], compare_op=ALU.is_ge,
                fill=NEG, base=128 * i - 31, channel_multiplier=-16)
        w1s = [sb(C, es, "w1s", [64, 32, 256], BF16) for _ in range(2)]
        w2s = [sb(C, es, "w2s", [128, 2, 64], BF16) for _ in range(2)]
        posT = [sb(C, es, "posT", [64, 32], F32) for _ in range(2)]
        with ExitStack() as es2:
            praw = sb(C, es2, "praw", [32, 64], F32)
            pp = ps(C, es2, "ppos", [64, 32], F32)
            for kv in range(2):
                P.dma("pool", w1s[kv][:], cmp_w1[kv].rearrange("(l d) j -> d l j", d=64))
                P.dma("pool", w2s[kv][:], cmp_w2[kv].rearrange("(a p) d -> p a d", p=128))
                P.dma("sp", praw[:], cmp_pos[kv])
                P.tr(pp[:, :], praw[:, :], C.ident_f[0:32, 0:32])
                P.copy("dve", posT[kv][:], pp[:, :])
            P.barrier()
        qT = sb(C, es, "qT", [64, 4, S], BF16)
        kTs = sb(C, es, "kTs", [64, S], BF16)
        kTw = sb(C, es, "kTw", [64, S], BF16)
        tT = [sb(C, es, "tT", [64, S], BF16) for _ in range(2)]
        tl = sb(C, es, "tl", [64, 32, NCMP], BF16)
        hgT = sb(C, es, "hgT", [128, 2, 128], BF16)
        g_sq = sb(C, es, "g_sq", [128, NCMP], F32)
        g_in = sb(C, es, "g_in", [128, NCMP], F32)
        kcT = sb(C, es, "kcT", [64, 128], BF16)
        Vc = sb(C, es, "Vc", [128, VW], BF16)
        Vs = sb(C, es, "Vs", [128, NQ, 65], BF16)
        Vw = sb(C, es, "Vw", [128, NQ, 65], BF16)
        ovf = sb(C, es, "ovf", [128, 1 + NSB], F32)
        gts = sb(C, es, "gts", [128, NQ, 12], F32)
        PTc = sb(C, es, "PTc", [128, 512], BF16)
        PTs = sb(C, es, "PTs", [128, NQ, 512], BF16)
        PTw = sb(C, es, "PTw", [128, WT + 1, 512], BF16)
        rden = [sb(C, es, "rden", [128, 4], F32) for _ in range(3)]
        coef = [sb(C, es, "coef", [128, 4], F32) for _ in range(3)]
        o_acc = [sb(C, es, "o_acc", [128, 4, 64], F32) for _ in range(2)]
        tmpo = [sb(C, es, "tmpo", [128, 4, 64], F32) for _ in range(2)]
        tmp4 = sb(C, es, "tmp4", [128, 4, NSB], F32)
        score = sb(C, es, "score", [128, NSB], F32)
        score2 = sb(C, es, "score2", [128, NSB], F32)
        m8a = sb(C, es, "m8a", [128, 8], F32)
        m8b = sb(C, es, "m8b", [128, 8], F32)
        negq = sb(C, es, "negq", [128, NSB], F32)
        negT = [sb(C, es, "negT", [NSB, 128], BF16) for _ in range(2)]
        pS = [ps(C, es, "apS", [128, 512], F32) for _ in range(2)]
        pOc = ps(C, es, "apOc", [128, 4, 128], F32)
        pOs = ps(C, es, "apOs", [128, 4, 128], F32)
        pOw = ps(C, es, "apOw", [128, 4, 128], F32)
        pN = ps(C, es, "apN", [128, 512], F32)
        P.dma("sp", ovf[:], c_ov[:, :])
        P.memset("pool", hgT[:], 0.0)
        P.memset("pool", Vs[:], 1.0)
        P.memset("pool", Vw[:], 1.0)

        def bc4(ap4, n):
            return ap4.unsqueeze(2).broadcast_to([128, 4, n])

        for sq_ in range(nseq):
            for g in range(NSA_G):
                P.dma("sp", qT[:], qT_d[sq_, 4 * g:4 * g + 4].rearrange("h d s -> d h s"))
                P.dma("sp", tT[0][:], kT_d[sq_, 0, g])
                P.dma("sp", tT[1][:], kT_d[sq_, 1, g])
                P.dma("sp", kTs[:], kT_d[sq_, 2, g])
                P.dma("sp", kTw[:], kT_d[sq_, 3, g])
                P.dma("sp", Vs[:, :, 0:64], v_d[sq_, 0, :, g * 64:(g + 1) * 64].rearrange("(t p) d -> p t d", p=128))
                P.dma("sp", Vw[:, :, 0:64], v_d[sq_, 1, :, g * 64:(g + 1) * 64].rearrange("(t p) d -> p t d", p=128))
                P.dma("sp", gts[:], gates_d[sq_ * S:(sq_ + 1) * S, g * 12:(g + 1) * 12].rearrange("(t p) c -> p t c", p=128))
                for kv in range(2):
                    for l in range(32):
                        b0 = (l // 16) * 16
                        src = tT[kv][:, b0:b0 + NCMP * 16].rearrange("p (c r) -> p c r", r=16)[:, :, l % 16]
                        P.ts("dve" if l % 2 else "pool", tl[:, l, :], src, posT[kv][:, l:l + 1], ALU.add)
                    for jh in range(2):
                        for l in range(32):
                            P.mm(pS[jh][:, 0:NCMP], w1s[kv][:, l, jh * 128:(jh + 1) * 128], tl[:, l, :], start=(l == 0), stop=(l == 31))
                    for jh in range(2):
                        xg = pS[jh][:, 0:NCMP]
                        P.act(g_sq[:], xg, AF.Square)
                        P.ts("dve", g_sq[:], g_sq[:], 0.044715, ALU.mult, 1.0, ALU.add)
                        P.tt("dve", g_in[:], g_sq[:], xg, ALU.mult)
                        P.act(g_in[:], g_in[:], AF.Sigmoid, scale=1.5957691216057308)
                        P.tt("dve", hgT[:, jh, 0:NCMP], g_in[:], xg, ALU.mult)
                    if kv == 0:
                        for jh in range(2):
                            P.mm(pN[0:64, 0:128], w2s[0][:, jh, :], hgT[:, jh, :], start=(jh == 0), stop=(jh == 1))
                        P.copy("act", kcT[:], pN[0:64, 0:128])
                    else:
                        for jh in range(2):
                            P.mm(pN[:, 0:64], hgT[:, jh, :], w2s[1][:, jh, :], start=(jh == 0), stop=(jh == 1))
                        P.copy("act", Vc[:, 0:64], pN[:, 0:64])
                        P.copy("pool", Vc[:, 64:VW], ovf[:])
                def cmp_topk(i):
                    qv = qT[:, :, i * 128:(i + 1) * 128]
                    oa = o_acc[i % 2]
                    sc = pS[0]
                    P.mm(sc[:], kcT[:], qv, start=True, stop=False)
                    P.mm(sc[:], C.ident[:], negC[:, i, :].unsqueeze(1).broadcast_to([128, 4, 128]), start=False, stop=True)
                    P.act(PTc[:], sc[:], AF.Exp, scale=0.125)
                    for h in range(4):
                        P.mm(pOc[:, h, 0:VW], PTc[:, h * 128:(h + 1) * 128], Vc[:], start=True, stop=True)
                    P.ts("dve", rden[0][:], pOc[:, :, 64], 1e-30, ALU.add)
                    P.I("dve", "reciprocal", out=rden[0][:], in_=rden[0][:])
                    P.tt("dve", coef[0][:], rden[0][:], gts[:, i, 0:12:3], ALU.mult)
                    P.tt("dve", oa[:], pOc[:, :, 0:64], bc4(coef[0][:], 64), ALU.mult)
                    P.tt("dve", tmp4[:], pOc[:, :, 65:VW], bc4(rden[0][:], NSB), ALU.mult)
                    P.I("dve", "tensor_reduce", out=score[:], in_=tmp4[:].rearrange("p h j -> p j h"), axis=AX.X, op=ALU.add)
                    P.tt("dve", score[:], score[:], forced[:, i, :], ALU.add)
                    P.I("dve", "max", out=m8a[:], in_=score[:])
                    thr = m8a
                    if NSEL > 8:
                        P.I("dve", "match_replace", out=score2[:], in_to_replace=m8a[:], in_values=score[:], imm_value=-1.0)
                        P.I("dve", "max", out=m8b[:], in_=score2[:])
                        thr = m8b
                    P.ts("dve", negq[:], score[:], thr[:, 7:8], ALU.is_lt, NEG, ALU.mult)
                    P.tr(pN[0:NSB, 0:128], negq[:, :], C.ident_f[:, :])
                    P.copy("act", negT[i % 2][:], pN[0:NSB, 0:128])

                def win_qk(i):
                    qv = qT[:, :, i * 128:(i + 1) * 128]
                    j0 = max(0, i - WT)
                    for j in range(j0, i + 1):
                        sc = pS[j % 2]
                        ks = slice(j * 128, (j + 1) * 128)
                        last_plain = not (j == i or j == i - WT)
                        P.mm(sc[:], kTw[:, ks], qv, start=True, stop=last_plain)
                        if j == i:
                            P.mm(sc[:], C.ident[:], negU[:], start=False, stop=True)
                        elif j == i - WT:
                            P.mm(sc[:], C.ident[:], negL[:], start=False, stop=True)
                        P.act(PTw[:, j - j0, :], sc[:], AF.Exp, scale=0.125)

                def win_pv(i):
                    j0 = max(0, i - WT)
                    for h in range(4):
                        for j in range(j0, i + 1):
                            P.mm(pOw[:, h, 0:65], PTw[:, j - j0, h * 128:(h + 1) * 128], Vw[:, j, :], start=(j == j0), stop=(j == i))
                    P.I("dve", "reciprocal", out=rden[2][:], in_=pOw[:, :, 64])
                    P.tt("dve", coef[2][:], rden[2][:], gts[:, i, 2:12:3], ALU.mult)
                    P.tt("dve", tmpo[0][:], pOw[:, :, 0:64], bc4(coef[2][:], 64), ALU.mult)
                    P.tt("pool", o_acc[i % 2][:], o_acc[i % 2][:], tmpo[0][:], ALU.add)

                def sel_qk(i):
                    qv = qT[:, :, i * 128:(i + 1) * 128]
                    nT = negT[i % 2]
                    for j in range(i + 1):
                        sc = pS[(j + 1) % 2]
                        ks = slice(j * 128, (j + 1) * 128)
                        P.mm(sc[:], kTs[:, ks], qv, start=True, stop=False)
                        P.mm(sc[:], E[:, ks], nT[:].unsqueeze(1).broadcast_to([NSB, 4, 128]), start=False, stop=(j != i))
                        if j == i:
                            P.mm(sc[:], C.ident[:], negU[:], start=False, stop=True)
                        P.act(PTs[:, j, :], sc[:], AF.Exp, scale=0.125)

                def sel_pv(i):
                    oa = o_acc[i % 2]
                    for h in range(4):
                        for j in range(i + 1):
                            P.mm(pOs[:, h, 0:65], PTs[:, j, h * 128:(h + 1) * 128], Vs[:, j, :], start=(j == 0), stop=(j == i))
                    P.I("dve", "reciprocal", out=rden[1][:], in_=pOs[:, :, 64])
                    P.tt("dve", coef[1][:], rden[1][:], gts[:, i, 1:12:3], ALU.mult)
                    P.tt("dve", tmpo[1][:], pOs[:, :, 0:64], bc4(coef[1][:], 64), ALU.mult)
                    P.tt("pool", oa[:], oa[:], tmpo[1][:], ALU.add)
                    r0 = sq_ * S + i * 128
                    P.dma("sp", o_d[r0:r0 + 128, g * 256:(g + 1) * 256], oa[:].rearrange("p h d -> p (h d)"))

                cmp_topk(0)
                for i in range(NQ):
                    if i + 1 < NQ:
                        cmp_topk(i + 1)
                    win_qk(i)
                    sel_qk(i)
                    win_pv(i)
                    sel_pv(i)
        P.barrier()


DEPTH = 4
NSEQ_CORE = 2
W_SHAPES = {
    "norm_gains": [4, 4, 1024],
    "nsa_w_in": [2, 1024, 2608], "nsa_cmp_pos": [2, 2, 32, 64], "nsa_cmp_w1": [2, 2, 2048, 256],
    "nsa_cmp_w2": [2, 2, 256, 64], "nsa_w_out": [2, 1024, 1024],
    "ssd_w_in": [2, 1024, 5152], "ssd_conv_w": [2, 4, 3072], "ssd_conv_b": [2, 3072], "ssd_dt_bias": [2, 32],
    "ssd_a_log": [2, 32], "ssd_d": [2, 32], "ssd_norm_w": [2, 2048], "ssd_w_out": [2, 2048, 1024],
    "ffn_w_up": [4, 1024, 5632], "ffn_conv_w": [4, 3, 5632], "ffn_conv_b": [4, 5632], "ffn_w_down": [4, 2816, 1024],
}


def build_program(nseq=NSEQ_CORE, seq=SEQ, depth=DEPTH):
    nc = bass.Bass("TRN2", target_bir_lowering=False)
    ntok = nseq * seq
    nsb = seq // 64
    W = {k: nc.dram_tensor(k, v, F32, kind="ExternalInput").ap() for k, v in W_SHAPES.items()}
    x = nc.dram_tensor("x", [ntok, D_MODEL], F32, kind="ExternalInput").ap()
    c_ov = nc.dram_tensor("c_ov", [128, 1 + nsb], F32, kind="ExternalInput").ap()
    c_forced = nc.dram_tensor("c_forced", [seq, nsb], F32, kind="ExternalInput").ap()
    c_E = nc.dram_tensor("c_E", [nsb, seq], F32, kind="ExternalInput").ap()
    out = nc.dram_tensor("out", [ntok, D_MODEL], F32, kind="ExternalOutput").ap()

    def scratch(name, shape, dt=F32):
        return nc.dram_tensor(name, shape, dt, kind="Internal").ap()
    hA = scratch("hA", [ntok, D_MODEL])
    hB = scratch("hB", [ntok, D_MODEL])
    fpart = scratch("fpart", [ntok, D_MODEL])
    y_pre = scratch("y_pre", [ntok, SSD_DI])
    o_d = scratch("o_d", [ntok, D_MODEL])
    gates_d = scratch("gates_d", [ntok, 48])
    qT_d = scratch("qT_d", [nseq, NSA_H, 64, seq], BF16)
    kT_d = scratch("kT_d", [nseq, 4, NSA_G, 64, seq], BF16)
    v_d = scratch("v_d", [nseq, 2, seq, 256], BF16)

    P = Prog(nc)
    P.nodep |= set(W_SHAPES) | {"x", "c_ov", "c_forced", "c_E"}
    C = Ctx(nc, P)
    with ExitStack() as es:
        emit_consts(C, es)
        P.barrier()
        cur = x
        for i in range(depth):
            g = W["norm_gains"][i]
            slot = i // 2
            if i % 2 == 0:
                emit_nsa_a(C, cur, g[0], W["nsa_w_in"][slot], qT_d, kT_d, v_d, gates_d, ntok, seq=seq)
                emit_nsa_c(C, qT_d, kT_d, v_d, gates_d, o_d, W["nsa_cmp_pos"][slot], W["nsa_cmp_w1"][slot],
                           W["nsa_cmp_w2"][slot], c_ov, c_forced, c_E, nseq, seq=seq)
                emit_out(C, "nsa", cur, hA, o_d, W["nsa_w_out"][slot], g[1], ntok)
            else:
                emit_ssd_a(C, cur, y_pre, g[0], W["ssd_w_in"][slot], W["ssd_conv_w"][slot], W["ssd_conv_b"][slot],
                           W["ssd_dt_bias"][slot], W["ssd_a_log"][slot], W["ssd_d"][slot], ntok, seq=seq)
                emit_out(C, "ssd", cur, hA, y_pre, W["ssd_w_out"][slot], g[1], ntok,
                         g_pre=g[0], w_in=W["ssd_w_in"][slot], norm_w=W["ssd_norm_w"][slot])
            dst = out if i == depth - 1 else hB
            emit_ffn(C, hA, dst, fpart, g[2], g[3], W["ffn_w_up"][i], W["ffn_conv_w"][i], W["ffn_conv_b"][i],
                     W["ffn_w_down"][i], ntok, seq=seq)
            cur = hB
        P.barrier()
    return nc, P


def kernel(**inputs):
    x = np.ascontiguousarray(inputs["x"], dtype=np.float32)
    B, S, D = x.shape
    assert (B, S, D) == (N_CORES * NSEQ_CORE, SEQ, D_MODEL)
    nc, _ = build_program()
    consts = nsa_consts(SEQ)
    shared = {k: np.ascontiguousarray(inputs[k], dtype=np.float32) for k in W_SHAPES}
    shared.update(consts)
    in_maps = []
    for c in range(N_CORES):
        m = dict(shared)
        m["x"] = x[c * NSEQ_CORE:(c + 1) * NSEQ_CORE].reshape(NSEQ_CORE * SEQ, D_MODEL)
        in_maps.append(m)
    res = run_bass_kernel_spmd(nc, in_maps, core_ids=list(range(N_CORES)))
    outs = [r["out"].reshape(NSEQ_CORE, SEQ, D_MODEL) for r in res.results]
    return np.concatenate(outs, axis=0).astype(np.float32)
```

```python
import math
from contextlib import ExitStack

import numpy as np
import concourse.bass as bass
import concourse.mybir as mybir
from concourse.bass_utils import run_bass_kernel_spmd

F32 = mybir.dt.float32
BF16 = mybir.dt.bfloat16
AF = mybir.ActivationFunctionType
ALU = mybir.AluOpType
AX = mybir.AxisListType

D_MODEL = 1024
SEQ = 2048
N_CORES = 8
FFN_HIDDEN = 2816
RMS_EPS = 1e-6

WRITE_KEYS = ("out", "accum_out", "ap")
DT_SIZE = {F32: 4, BF16: 2}


def _box(ap):
    t = ap.tensor
    tn = type(t).__name__
    off = ap.offset
    dims = list(ap.ap)
    if tn.startswith("SB") or tn.startswith("PSum"):
        pstep = 1
        for s in list(t.shape)[1:]:
            pstep *= int(s)
        p0 = off // pstep
        f0 = off % pstep
        st0, cn0 = dims[0]
        pe = (cn0 - 1) * (st0 // pstep) if cn0 > 1 else 0
        fe = 0
        for st, cn in dims[1:]:
            if cn > 1:
                assert st >= 0
                fe += (cn - 1) * st
        f1 = f0 + fe + 1
        if tn.startswith("PSum"):
            be = 2048 // DT_SIZE[t.dtype]
            f0 = (f0 // be) * be
            f1 = -(-f1 // be) * be
        return (p0, p0 + pe + 1, f0, f1)
    lo = hi = off
    for st, cn in dims:
        if cn > 1:
            if st >= 0:
                hi += (cn - 1) * st
            else:
                lo += (cn - 1) * st
    return (0, 1, lo, hi + 1)


class Prog:
    def __init__(self, nc, n_dma_sems=10, same_engine_sync=True):
        self.nc = nc
        self.e = dict(pe=nc.tensor, act=nc.scalar, dve=nc.vector, pool=nc.gpsimd, sp=nc.sync)
        self.sem = {}
        self.cnt = {}
        self.allsems = []
        for k in self.e:
            self._new_sem(k)
        self.seen = {k: {} for k in self.e}
        self.acc = {}
        self.nodep = set()
        self.dsems = {}
        self.dnext = {}
        self.dval = {}
        self.semh = {}
        for q in ("sp", "act", "pool"):
            self.dsems[q] = []
            for i in range(n_dma_sems):
                s = nc.alloc_semaphore(name=f"d_{q}_{i}")
                self.dsems[q].append(s)
                self.dval[s.num] = 0
                self.semh[s.num] = s
            self.dnext[q] = 0
        self.same = same_engine_sync
        self.ninst = 0
        self.nwait = 0

    def _new_sem(self, k):
        s = self.nc.alloc_semaphore(name=f"s_{k}_{len(self.allsems)}")
        self.sem[k] = s
        self.cnt[k] = 0
        self.allsems.append(s)

    def _wait(self, ek, sem, val):
        sn = self.seen[ek]
        if sn.get(sem.num, 0) >= val:
            return
        self.e[ek].wait_ge(sem, val)
        sn[sem.num] = val
        self.nwait += 1

    def _sync(self, ek, reads, writes):
        need = {}
        for is_w, aps in ((False, reads), (True, writes)):
            for ap in aps:
                name = ap.tensor.name
                if name in self.nodep:
                    continue
                b = _box(ap)
                is_psum = type(ap.tensor).__name__.startswith("PSum")
                for r in self.acc.get(name, ()):
                    if not (is_w or r[0]):
                        if not (is_psum and r[7] != ek):
                            continue
                    if r[1] < b[1] and b[0] < r[2] and r[3] < b[3] and b[2] < r[4]:
                        if r[7] == ek and (ek == "pe" or not self.same) and r[7] != "dma":
                            continue
                        s, v = r[5], r[6]
                        if need.get(s.num, (None, 0))[1] < v:
                            need[s.num] = (s, v)
        for s, v in need.values():
            self._wait(ek, s, v)

    def _record(self, ek, reads, writes, sem, val, is_dma=False):
        tag = "dma" if is_dma else ek
        for is_w, aps in ((False, reads), (True, writes)):
            for ap in aps:
                name = ap.tensor.name
                if name in self.nodep:
                    continue
                b = _box(ap)
                lst = self.acc.setdefault(name, [])
                keep = []
                for r in lst:
                    inside = r[1] >= b[0] and r[2] <= b[1] and r[3] >= b[2] and r[4] <= b[3]
                    if inside and (is_w or ((not r[0]) and r[5].num == sem.num)):
                        continue
                    keep.append(r)
                keep.append((is_w, b[0], b[1], b[2], b[3], sem, val, tag))
                self.acc[name] = keep

    def I(self, ek, name, **kw):
        reads, writes = [], []
        for k, v in kw.items():
            if isinstance(v, bass.AP):
                (writes if k in WRITE_KEYS else reads).append(v)
        self._sync(ek, reads, writes)
        ins = getattr(self.e[ek], name)(**kw)
        self.cnt[ek] += 1
        ins.then_inc(self.sem[ek], 1)
        self._record(ek, reads, writes, self.sem[ek], self.cnt[ek])
        self.ninst += 1
        if self.cnt[ek] >= 30000:
            self._new_sem(ek)
        return ins

    def dma(self, q, out, in_, **kw):
        reads, writes = [in_], [out]
        self._sync(q, reads, writes)
        lst = self.dsems[q]
        s = lst[self.dnext[q] % len(lst)]
        self.dnext[q] += 1
        prev = self.dval[s.num]
        if prev:
            self._wait(q, s, prev)
        ins = self.e[q].dma_start(out=out, in_=in_, **kw)
        ins.then_inc(s, 16)
        self.dval[s.num] = prev + 16
        self._record(q, reads, writes, s, prev + 16, is_dma=True)
        self.ninst += 1
        return ins

    def barrier(self, engines=None):
        for x in (engines or self.e):
            for k in self.e:
                if self.cnt[k] and not (k == x):
                    self._wait(x, self.sem[k], self.cnt[k])
            for n, v in self.dval.items():
                if v:
                    self._wait(x, self.semh[n], v)
        self.acc = {}

    def mm(self, out, lhsT, rhs, start=True, stop=True, **kw):
        return self.I("pe", "matmul", out=out, lhsT=lhsT, rhs=rhs, start=start, stop=stop, **kw)

    def tr(self, out, in_, identity):
        return self.I("pe", "transpose", out=out, in_=in_, identity=identity)

    def act(self, out, in_, func, **kw):
        return self.I("act", "activation", out=out, in_=in_, func=func, **kw)

    def ts(self, ek, out, in0, s1, op0, s2=None, op1=None, **kw):
        if op1 is None:
            s2, op1 = 0.0, ALU.add
        return self.I(ek, "tensor_scalar", out=out, in0=in0, scalar1=s1, scalar2=s2, op0=op0, op1=op1, **kw)

    def tt(self, ek, out, in0, in1, op):
        return self.I(ek, "tensor_tensor", out=out, in0=in0, in1=in1, op=op)

    def stt(self, ek, out, in0, scalar, in1, op0, op1):
        return self.I(ek, "scalar_tensor_tensor", out=out, in0=in0, scalar=scalar, in1=in1, op0=op0, op1=op1)

    def copy(self, ek, out, in_):
        if ek == "act":
            return self.I("act", "activation", out=out, in_=in_, func=AF.Copy)
        return self.I(ek, "tensor_copy", out=out, in_=in_)

    def memset(self, ek, ap, val):
        return self.I(ek, "memset", ap=ap, constant=val)


class Ctx:
    def __init__(self, nc, P):
        self.nc = nc
        self.P = P
        self.uid = 0

    def name(self, base):
        self.uid += 1
        return f"{base}_{self.uid}"


def sb(C, es, base, shape, dtype):
    return es.enter_context(C.nc.sbuf_tensor(C.name(base), shape, dtype))


def ps(C, es, base, shape, dtype=F32):
    return es.enter_context(C.nc.psum_tensor(C.name(base), shape, dtype))


def bcast_rows(ap_row, nparts):
    return ap_row.partition_broadcast(nparts)


def emit_consts(C, es):
    P = C.P
    ident_f = sb(C, es, "identf", [128, 128], F32)
    ident = sb(C, es, "ident", [128, 128], BF16)
    P.memset("pool", ident_f[:], 0.0)
    P.I("pool", "affine_select", out=ident_f[:], in_=ident_f[:], pattern=[[-1, 128]],
        compare_op=ALU.not_equal, fill=1.0, base=0, channel_multiplier=1)
    P.copy("dve", ident[:], ident_f[:])
    C.ident = ident
    C.ident_f = ident_f


def rstd_from_ss(C, rstd, ss, width):
    C.P.act(rstd, ss, AF.Sqrt, scale=1.0 / width, bias=RMS_EPS)
    C.P.I("dve", "reciprocal", out=rstd, in_=rstd)


def rmsnorm_rows(C, x_ap, gain_bc, out_ap, ss, rstd, junk, width=D_MODEL, eng="dve"):
    P = C.P
    P.act(junk, x_ap, AF.Square, accum_out=ss)
    rstd_from_ss(C, rstd, ss, width)
    P.stt(eng, out_ap, x_ap, rstd, gain_bc, ALU.mult, ALU.mult)


def transpose_rows(C, hb, pt, hT, c0, kc, gain=None, ev="dve"):
    P = C.P
    for k0 in range(0, kc, 8):
        kn = min(8, kc - k0)
        for k in range(kn):
            P.tr(pt[:, k, :], hb[:, (k0 + k) * 128:(k0 + k + 1) * 128], C.ident[:])
        dst = hT[:, k0:k0 + kn, c0:c0 + 128]
        if gain is None:
            P.copy(ev, dst, pt[:, 0:kn, :])
        elif ev == "dve":
            P.tt("dve", dst, pt[:, 0:kn, :], gain[:, 0, k0:k0 + kn].unsqueeze(2).broadcast_to([128, kn, 128]), ALU.mult)
        else:
            for k in range(kn):
                P.act(hT[:, k0 + k, c0:c0 + 128], pt[:, k, :], AF.Identity, scale=gain[:, 0, k0 + k:k0 + k + 1])


def load_cast(C, dst, w_dram_rows):
    C.P.dma("pool", dst, w_dram_rows)


def load_featmajor_params(C, es, rows, nchunk, name):
    P = C.P
    nr = len(rows)
    out = sb(C, es, name, [128, nr, nchunk], F32)
    with ExitStack() as es2:
        raw = sb(C, es2, name + "_raw", [nchunk, nr, 128], F32)
        pp = ps(C, es2, name + "_ps", [128, nchunk], F32)
        for j, r in enumerate(rows):
            P.dma("sp", raw[:, j, :], r.rearrange("(m p) -> m p", p=128))
        for j in range(nr):
            P.tr(pp[:, :], raw[:, j, :], C.ident_f[0:nchunk, 0:nchunk])
            P.copy("dve", out[:, j, :], pp[:, :])
        P.barrier()
    return out


def emit_ffn(C, h_in, h_out, fpart, g_pre, g_post, w_up, conv_w, conv_b, w_down, ntok, seq=SEQ):
    P = C.P
    F = FFN_HIDDEN
    NM = F // 128
    NS = 2
    MH = NM // NS
    KC = D_MODEL // 128
    TG = 512
    with ExitStack() as es:
        cwb = load_featmajor_params(C, es, [conv_w[0], conv_w[1], conv_w[2], conv_b], 2 * NM, "cwb")
        gpre = load_featmajor_params(C, es, [g_pre], KC, "gpre")
        wu = sb(C, es, "wu", [128, KC, 2 * MH * 128], BF16)
        wd = sb(C, es, "wd", [128, MH, D_MODEL], BF16)
        gpost = sb(C, es, "gpost", [128, D_MODEL], F32)
        xt = [sb(C, es, "xt", [128, D_MODEL], F32) for _ in range(8)]
        hn = [sb(C, es, "hn", [128, D_MODEL], BF16) for _ in range(4)]
        hnT = [sb(C, es, "hnT", [128, KC, TG], BF16) for _ in range(2)]
        gT = [sb(C, es, "gT", [128, MH, TG], BF16) for _ in range(2)]
        uv = [sb(C, es, "uv", [128, TG], F32) for _ in range(2)]
        ug = [sb(C, es, "ug", [128, TG], F32) for _ in range(2)]
        halo = [sb(C, es, "halo", [128, 2 * NM, 2], F32) for _ in range(2)]
        junk = sb(C, es, "junk", [128, D_MODEL], BF16)
        ss = [sb(C, es, "ss", [128, 1], F32) for _ in range(4)]
        rstd = [sb(C, es, "rstd", [128, 1], F32) for _ in range(4)]
        ft = [sb(C, es, "ft", [128, D_MODEL], F32) for _ in range(3)]
        pT = [ps(C, es, "pT", [128, 8, 128], BF16) for _ in range(2)]
        pY = [ps(C, es, "pY", [128, TG], F32) for _ in range(4)]
        pO = ps(C, es, "pO", [128, D_MODEL], F32)

        P.dma("sp", gpost[:], g_post.partition_broadcast(128))
        ngroups = ntok // TG
        xi = 0
        fi = 0
        for s in range(NS):
            for k in range(KC):
                rows = w_up[k * 128:(k + 1) * 128, :]
                load_cast(C, wu[:, k, 0:MH * 128], rows[:, s * MH * 128:(s + 1) * MH * 128])
                load_cast(C, wu[:, k, MH * 128:2 * MH * 128], rows[:, F + s * MH * 128:F + (s + 1) * MH * 128])
            for m in range(MH):
                r0 = (s * MH + m) * 128
                load_cast(C, wd[:, m, :], w_down[r0:r0 + 128, :])
            def norm_part(g):
                nonlocal xi
                xs_ = []
                for t in range(TG // 128):
                    x = xt[xi % len(xt)]
                    xi += 1
                    xs_.append(x)
                    r0 = g * TG + t * 128
                    P.dma("sp", x[:], h_in[r0:r0 + 128, :])
                    hb = hn[t % 4]
                    sq, rs = ss[t % 4], rstd[t % 4]
                    P.act(junk[:], x[:], AF.Square, accum_out=sq[:])
                    rstd_from_ss(C, rs[:], sq[:], D_MODEL)
                    P.ts("dve", hb[:], x[:], rs[:], ALU.mult)
                return xs_

            def tr_part(g):
                for t in range(TG // 128):
                    transpose_rows(C, hn[t % 4], pT[t % 2], hnT[g % 2], t * 128, KC, gpre, ev=("dve" if t % 2 else "act"))

            xts_next = norm_part(0)
            tr_part(0)
            for g in range(ngroups):
                seq_start = (g * TG) % seq == 0
                hT = hnT[g % 2]
                xts = xts_next
                G = gT[g % 2]
                for m in range(MH):
                    yv = pY[(2 * m) % 4]
                    yg = pY[(2 * m + 1) % 4]
                    cv = s * MH + m
                    for (y, c0) in ((yv, m * 128), (yg, (MH + m) * 128)):
                        for k in range(KC):
                            P.mm(y[:], wu[:, k, c0:c0 + 128], hT[:, k, :], start=(k == 0), stop=(k == KC - 1))
                    for (y, u, ch) in ((yv, uv[m % 2], cv), (yg, ug[m % 2], NM + cv)):
                        P.act(u[:], y[:], AF.Identity, scale=cwb[:, 2, ch:ch + 1], bias=cwb[:, 3, ch:ch + 1])
                        P.copy("act", halo[g % 2][:, ch, :], y[:, TG - 2:TG])
                        P.stt("dve", u[:, 1:TG], y[:, 0:TG - 1], cwb[:, 1, ch:ch + 1], u[:, 1:TG], ALU.mult, ALU.add)
                        P.stt("dve", u[:, 2:TG], y[:, 0:TG - 2], cwb[:, 0, ch:ch + 1], u[:, 2:TG], ALU.mult, ALU.add)
                        if not seq_start:
                            hp = halo[(g + 1) % 2]
                            P.stt("dve", u[:, 0:2], hp[:, ch, :], cwb[:, 0, ch:ch + 1], u[:, 0:2], ALU.mult, ALU.add)
                            P.stt("dve", u[:, 0:1], hp[:, ch, 1:2], cwb[:, 1, ch:ch + 1], u[:, 0:1], ALU.mult, ALU.add)
                    P.act(ug[m % 2][:], ug[m % 2][:], AF.Silu)
                    P.tt("pool", G[:, m, :], ug[m % 2][:], uv[m % 2][:], ALU.mult)
                if g + 1 < ngroups:
                    xts_next = norm_part(g + 1)
                for t in range(TG // 128):
                    for half in range(2):
                        for m in range(MH):
                            P.mm(pO[:, half * 512:(half + 1) * 512], G[:, m, t * 128:(t + 1) * 128],
                                 wd[:, m, half * 512:(half + 1) * 512], start=(m == 0), stop=(m == MH - 1))
                    r0 = g * TG + t * 128
                    f = ft[fi % len(ft)]
                    fi += 1
                    if s == 0:
                        P.copy("act", f[:], pO[:])
                        P.dma("sp", fpart[r0:r0 + 128, :], f[:])
                    else:
                        P.dma("sp", f[:], fpart[r0:r0 + 128, :])
                        P.tt("dve", f[:], pO[:], f[:], ALU.add)
                        sq, rs = ss[t % 4], rstd[t % 4]
                        P.act(junk[:], f[:], AF.Square, accum_out=sq[:])
                        rstd_from_ss(C, rs[:], sq[:], D_MODEL)
                        P.stt("dve", f[:], f[:], rs[:], gpost[:], ALU.mult, ALU.mult)
                        P.tt("pool", f[:], f[:], xts[t][:], ALU.add)
                        P.dma("sp", h_out[r0:r0 + 128, :], f[:])
                if g + 1 < ngroups:
                    tr_part(g + 1)
        P.barrier()


SSD_DI = 2048
SSD_H = 32
SSD_P = 64
SSD_G = 4
SSD_N = 128
SSD_CONVD = SSD_DI + 2 * SSD_G * SSD_N


def emit_ssd_a(C, h_in, y_pre, g_pre, w_in, conv_w, conv_b, dt_bias, a_log, d_skip, ntok, seq=SEQ):
    P = C.P
    KC = D_MODEL // 128
    TG = 512
    NT = TG // 128
    NFC = SSD_CONVD // 128
    XO = SSD_DI
    NW = SSD_CONVD + SSD_H
    with ExitStack() as es:
        cwb = load_featmajor_params(C, es, [conv_w[0], conv_w[1], conv_w[2], conv_w[3], conv_b], NFC, "scwb")
        gpre = load_featmajor_params(C, es, [g_pre], KC, "sgpre")
        wx = sb(C, es, "wx", [128, KC, NW], BF16)
        for k in range(KC):
            load_cast(C, wx[:, k, :], w_in[k * 128:(k + 1) * 128, XO:XO + NW])
        dtb = sb(C, es, "dtb", [128, SSD_H], F32)
        a_bc = sb(C, es, "a_bc", [128, SSD_H], F32)
        d_bc = sb(C, es, "d_bc", [128, SSD_H], F32)
        P.dma("sp", dtb[:], dt_bias.partition_broadcast(128))
        P.dma("sp", a_bc[:], a_log.partition_broadcast(128))
        P.dma("sp", d_bc[:], d_skip.partition_broadcast(128))
        P.act(a_bc[:], a_bc[:], AF.Exp)
        P.ts("dve", a_bc[:], a_bc[:], -1.0, ALU.mult)
        Mf = sb(C, es, "Mf", [128, 128], F32)
        Uf = sb(C, es, "Uf", [128, 128], F32)
        onesf = sb(C, es, "onesf", [128, 128], F32)
        P.memset("pool", onesf[:], 1.0)
        P.I("pool", "affine_select", out=Mf[:], in_=onesf[:], pattern=[[1, 128]], compare_op=ALU.is_ge, fill=0.0,
            base=0, channel_multiplier=-1)
        P.I("pool", "affine_select", out=Uf[:], in_=onesf[:], pattern=[[-1, 128]], compare_op=ALU.is_gt, fill=0.0,
            base=0, channel_multiplier=1)

        xt = [sb(C, es, "sxt", [128, D_MODEL], F32) for _ in range(2)]
        hn = [sb(C, es, "shn", [128, D_MODEL], BF16) for _ in range(2)]
        junk = sb(C, es, "sjunk", [128, D_MODEL], BF16)
        ss = [sb(C, es, "sss", [128, 1], F32) for _ in range(2)]
        rstd = [sb(C, es, "srs", [128, 1], F32) for _ in range(2)]
        hnT = sb(C, es, "shnT", [128, KC, TG], BF16)
        dt_all = sb(C, es, "dt_all", [128, NT, SSD_H], F32)
        dtA_all = sb(C, es, "dtA_all", [128, NT, SSD_H], F32)
        dtmp = sb(C, es, "dtmp", [128, SSD_H], F32)
        u = [sb(C, es, "su", [128, TG], F32) for _ in range(2)]
        xs = [sb(C, es, "sxs", [128, TG], BF16) for _ in range(2)]
        halo = [sb(C, es, "shalo", [128, NFC, 3], F32) for _ in range(2)]
        x_tok = sb(C, es, "x_tok", [128, NT, SSD_DI], BF16)
        B_tok = sb(C, es, "B_tok", [128, NT, SSD_G * SSD_N], BF16)
        BT = sb(C, es, "BT", [128, SSD_G, TG], BF16)
        CT = sb(C, es, "CT", [128, SSD_G, TG], BF16)
        state = sb(C, es, "state", [128, SSD_DI], F32)
        state_bf = sb(C, es, "state_bf", [128, SSD_DI], BF16)
        Rg = [sb(C, es, "Rg", [128, 8, 128], F32) for _ in range(2)]
        lmT = [sb(C, es, "lmT", [128, 8, 128], F32) for _ in range(2)]
        wT = [sb(C, es, "wT", [128, 8, 128], BF16) for _ in range(2)]
        cbm = [sb(C, es, "cbm", [128, 128], F32) for _ in range(2)]
        xd = sb(C, es, "xd", [128, SSD_DI], BF16)
        xdsk = sb(C, es, "xdsk", [128, SSD_DI], F32)
        xdec = sb(C, es, "xdec", [128, SSD_DI], BF16)
        tmpg = [sb(C, es, "tmpg", [128, 512], F32) for _ in range(2)]
        y_sb = sb(C, es, "y_sb", [128, SSD_DI], F32)
        acs_sb = sb(C, es, "acs_sb", [128, SSD_H], F32)
        E_sb = sb(C, es, "E_sb", [128, SSD_H], F32)
        dec = sb(C, es, "dec", [128, SSD_H], F32)
        Etot = sb(C, es, "Etot", [128, SSD_H], F32)

        pT = ps(C, es, "spT", [128, 8, 128], BF16)
        pY = [ps(C, es, "spY", [128, TG], F32) for _ in range(2)]
        pS = ps(C, es, "spS", [128, 8, 128], F32)
        pC = ps(C, es, "spC", [128, 512], F32)
        pD = ps(C, es, "spD", [128, 512], F32)
        pD2 = ps(C, es, "spD2", [128, 512], F32)

        def bc_h(ap_h, n):
            return ap_h.unsqueeze(2).broadcast_to([128, ap_h.shape[1], n])

        ngroups = ntok // TG
        for g in range(ngroups):
            seq_start = (g * TG) % seq == 0
            for t in range(NT):
                x = xt[t % 2]
                r0 = g * TG + t * 128
                P.dma("sp", x[:], h_in[r0:r0 + 128, :])
                hb = hn[t % 2]
                P.act(junk[:], x[:], AF.Square, accum_out=ss[t % 2][:])
                rstd_from_ss(C, rstd[t % 2][:], ss[t % 2][:], D_MODEL)
                P.ts("pool", hb[:], x[:], rstd[t % 2][:], ALU.mult)
                transpose_rows(C, hb, pT, hnT, t * 128, KC, gpre, ev=("dve" if t % 2 else "act"))
            for t in range(NT):
                for k in range(KC):
                    P.mm(pD[:, 0:SSD_H], hnT[:, k, t * 128:(t + 1) * 128], wx[:, k, SSD_CONVD:NW],
                         start=(k == 0), stop=(k == KC - 1))
                P.tt("dve", dtmp[:], pD[:, 0:SSD_H], dtb[:], ALU.add)
                P.act(dtmp[:], dtmp[:], AF.Exp)
                P.act(dt_all[:, t, :], dtmp[:], AF.Ln, bias=1.0)
                P.tt("dve", dtA_all[:, t, :], dt_all[:, t, :], a_bc[:], ALU.mult)
            for fc in range(NFC):
                y = pY[fc % 2]
                for k in range(KC):
                    P.mm(y[:], wx[:, k, fc * 128:(fc + 1) * 128], hnT[:, k, :], start=(k == 0), stop=(k == KC - 1))
                uu = u[fc % 2]
                P.act(uu[:], y[:], AF.Identity, scale=cwb[:, 3, fc:fc + 1], bias=cwb[:, 4, fc:fc + 1])
                P.copy("act", halo[g % 2][:, fc, :], y[:, TG - 3:TG])
                for j in range(1, 4):
                    P.stt("dve", uu[:, j:TG], y[:, 0:TG - j], cwb[:, 3 - j, fc:fc + 1], uu[:, j:TG], ALU.mult, ALU.add)
                if not seq_start:
                    hp = halo[(g + 1) % 2]
                    for j in range(1, 4):
                        P.stt("dve", uu[:, 0:j], hp[:, fc, 3 - j:3], cwb[:, 3 - j, fc:fc + 1], uu[:, 0:j], ALU.mult, ALU.add)
                if fc < 16:
                    xx = xs[fc % 2]
                    P.act(xx[:], uu[:], AF.Silu)
                    for t in range(NT):
                        P.tr(pT[:, t, :], xx[:, t * 128:(t + 1) * 128], C.ident[:])
                    P.copy("dve" if fc % 2 else "act", x_tok[:, :, fc * 128:(fc + 1) * 128], pT[:, 0:NT, :])
                elif fc < 20:
                    P.act(BT[:, fc - 16, :], uu[:], AF.Silu)
                    for t in range(NT):
                        P.tr(pT[:, t, :], BT[:, fc - 16, t * 128:(t + 1) * 128], C.ident[:])
                    P.copy("dve" if fc % 2 else "act", B_tok[:, :, (fc - 16) * 128:(fc - 15) * 128], pT[:, 0:NT, :])
                else:
                    P.act(CT[:, fc - 20, :], uu[:], AF.Silu)
            def stage1(t, gg):
                R = Rg[gg % 2]
                hs = slice(gg * 8, (gg + 1) * 8)
                P.tt("dve" if gg % 2 else "pool", R[:], Mf[:].unsqueeze(1).broadcast_to([128, 8, 128]),
                     bc_h(dtA_all[:, t, hs], 128), ALU.mult)
                for half in range(2):
                    P.mm(pS[:, half * 4:(half + 1) * 4, :], Uf[:], R[:, half * 4:(half + 1) * 4, :], start=True, stop=True)
                P.act(lmT[gg % 2][:], pS[:], AF.Exp)

            def prologue(t):
                first = seq_start and t == 0
                c0 = t * 128
                dtA = dtA_all[:, t, :]
                dt = dt_all[:, t, :]
                xv = x_tok[:, t, :].rearrange("p (h d) -> p h d", d=SSD_P)
                if first:
                    P.memset("pool", state[:], 0.0)
                    P.memset("pool", state_bf[:], 0.0)
                for g4 in range(SSD_G):
                    P.mm(pC[:, g4 * 128:(g4 + 1) * 128], BT[:, g4, c0:c0 + 128], CT[:, g4, c0:c0 + 128], start=True, stop=True)
                P.tt("dve", xd[:].rearrange("p (h d) -> p h d", d=SSD_P), xv, bc_h(dt, SSD_P), ALU.mult)
                P.tt("pool", xdsk[:].rearrange("p (h d) -> p h d", d=SSD_P), xv, bc_h(d_bc[:], SSD_P), ALU.mult)
                P.mm(pD[:, 0:SSD_H], Mf[:], dtA, start=True, stop=True)
                P.mm(pD2[:, 0:SSD_H], onesf[:], dtA, start=True, stop=True)
                P.copy("act", acs_sb[:], pD[:, 0:SSD_H])
                P.act(E_sb[:], pD[:, 0:SSD_H], AF.Exp)
                P.tt("dve", dec[:], pD2[:, 0:SSD_H], acs_sb[:], ALU.subtract)
                P.act(dec[:], dec[:], AF.Exp)
                P.tt("dve", dec[:], dec[:], dt, ALU.mult)
                P.act(Etot[:], pD2[:, 0:SSD_H], AF.Exp)
                P.tt("dve", xdec[:].rearrange("p (h d) -> p h d", d=SSD_P), xv, bc_h(dec[:], SSD_P), ALU.mult)

            def stage2(t, gg):
                first = seq_start and t == 0
                c0 = t * 128
                hs = slice(gg * 8, (gg + 1) * 8)
                lm = lmT[gg % 2]
                cm = cbm[gg % 2]
                P.tt("dve", cm[:], pC[:, gg * 128:(gg + 1) * 128], Mf[:], ALU.mult)
                w = wT[gg % 2]
                P.tt("dve", w[:], lm[:], cm[:].unsqueeze(1).broadcast_to([128, 8, 128]), ALU.mult)
                yi = pY[0]
                for hg in range(8):
                    hh = gg * 8 + hg
                    P.mm(yi[:, hg * 64:(hg + 1) * 64], w[:, hg, :], xd[:, hh * 64:(hh + 1) * 64], start=True, stop=True)
                cs = slice(gg * 512, (gg + 1) * 512)
                if first:
                    P.tt("dve", y_sb[:, cs], yi[:], xdsk[:, cs], ALU.add)
                else:
                    yo = pY[1]
                    P.mm(yo[:], CT[:, gg, c0:c0 + 128], state_bf[:, cs], start=True, stop=True)
                    tg_ = tmpg[gg % 2]
                    P.tt("dve", tg_[:].rearrange("p (h d) -> p h d", d=SSD_P), yo[:].rearrange("p (h d) -> p h d", d=SSD_P),
                         bc_h(E_sb[:, hs], SSD_P), ALU.mult)
                    P.tt("dve", tg_[:], tg_[:], xdsk[:, cs], ALU.add)
                    P.tt("dve", y_sb[:, cs], yi[:], tg_[:], ALU.add)
                P.mm(pD[:], B_tok[:, t, gg * 128:(gg + 1) * 128], xdec[:, cs], start=True, stop=True)
                if first:
                    P.copy("act", state[:, cs], pD[:])
                else:
                    P.tt("dve", state[:, cs].rearrange("p (h d) -> p h d", d=SSD_P),
                         state[:, cs].rearrange("p (h d) -> p h d", d=SSD_P), bc_h(Etot[:, hs], SSD_P), ALU.mult)
                    P.tt("dve", state[:, cs], pD[:], state[:, cs], ALU.add)
                P.copy("act", state_bf[:, cs], state[:, cs])
                if gg == SSD_G - 1:
                    r0 = g * TG + t * 128
                    P.dma("sp", y_pre[r0:r0 + 128, :], y_sb[:])

            items = [(t, gg) for t in range(NT) for gg in range(SSD_G)]
            stage1(*items[0])
            for k, (t, gg) in enumerate(items):
                if gg == 0:
                    prologue(t)
                if k + 1 < len(items):
                    stage1(*items[k + 1])
                stage2(t, gg)
        P.barrier()


def emit_out(C, mode, h_in, h_out, src, w_out, g_post, ntok, g_pre=None, w_in=None, norm_w=None):
    P = C.P
    KC = D_MODEL // 128
    K = SSD_DI if mode == "ssd" else D_MODEL
    KO = K // 128
    with ExitStack() as es:
        wo = sb(C, es, "wo", [128, KO, D_MODEL], BF16)
        for k in range(KO):
            load_cast(C, wo[:, k, :], w_out[k * 128:(k + 1) * 128, :])
        gpost = sb(C, es, "ogpost", [128, D_MODEL], F32)
        P.dma("sp", gpost[:], g_post.partition_broadcast(128))
        if mode == "ssd":
            gpre = load_featmajor_params(C, es, [g_pre], KC, "ogpre")
            wz = sb(C, es, "wz", [128, KC, SSD_DI], BF16)
            for k in range(KC):
                load_cast(C, wz[:, k, :], w_in[k * 128:(k + 1) * 128, 0:SSD_DI])
            nw = sb(C, es, "nw", [128, SSD_DI], F32)
            P.dma("sp", nw[:], norm_w.partition_broadcast(128))
            hn = [sb(C, es, "ohn", [128, D_MODEL], BF16) for _ in range(2)]
            hnT = [sb(C, es, "ohnT", [128, KC, 128], BF16) for _ in range(2)]
            zs = [sb(C, es, "zs", [128, SSD_DI], F32) for _ in range(2)]
            ssg = [sb(C, es, "ssg", [128, 4], F32) for _ in range(2)]
            rsg = [sb(C, es, "rsg", [128, 4], F32) for _ in range(2)]
            pZ = ps(C, es, "pZ", [128, SSD_DI], F32)
        xt = [sb(C, es, "oxt", [128, D_MODEL], F32) for _ in range(3)]
        yt = [sb(C, es, "oyt", [128, K], F32) for _ in range(2)]
        yb = [sb(C, es, "oyb", [128, K], BF16) for _ in range(2)]
        yT = [sb(C, es, "oyT", [128, KO, 128], BF16) for _ in range(2)]
        junk = sb(C, es, "ojunk", [128, D_MODEL], BF16)
        ss = [sb(C, es, "oss", [128, 1], F32) for _ in range(2)]
        rstd = [sb(C, es, "ors", [128, 1], F32) for _ in range(2)]
        ft = [sb(C, es, "oft", [128, D_MODEL], F32) for _ in range(2)]
        pT = [ps(C, es, "opT", [128, 8, 128], BF16) for _ in range(2)]
        pO = ps(C, es, "opO", [128, D_MODEL], F32)
        junk2 = sb(C, es, "ojunk2", [128, 512], BF16)
        junk3 = sb(C, es, "ojunk3", [128, D_MODEL], BF16)
        ss2 = [sb(C, es, "oss2", [128, 1], F32) for _ in range(2)]
        rstd2 = [sb(C, es, "ors2", [128, 1], F32) for _ in range(2)]
        NTILE = ntok // 128

        def front(t):
            r0 = t * 128
            x = xt[t % 3]
            y = yt[t % 2]
            ybf = yb[t % 2]
            P.dma("sp", x[:], h_in[r0:r0 + 128, :])
            P.dma("sp", y[:], src[r0:r0 + 128, :])
            if mode == "ssd":
                hb = hn[t % 2]
                P.act(junk[:], x[:], AF.Square, accum_out=ss[t % 2][:])
                rstd_from_ss(C, rstd[t % 2][:], ss[t % 2][:], D_MODEL)
                P.ts("pool", hb[:], x[:], rstd[t % 2][:], ALU.mult)
                hT = hnT[t % 2]
                transpose_rows(C, hb, pT[0], hT, 0, KC, gpre, ev="dve")
                for nch in range(4):
                    for k in range(KC):
                        P.mm(pZ[:, nch * 512:(nch + 1) * 512], hT[:, k, :], wz[:, k, nch * 512:(nch + 1) * 512],
                             start=(k == 0), stop=(k == KC - 1))
                z = zs[t % 2]
                sg, rg = ssg[t % 2], rsg[t % 2]
                for nch in range(4):
                    cs = slice(nch * 512, (nch + 1) * 512)
                    P.act(z[:, cs], pZ[:, cs], AF.Silu)
                    P.tt("dve", y[:, cs], y[:, cs], z[:, cs], ALU.mult)
                    P.act(junk2[:, 0:512], y[:, cs], AF.Square, accum_out=sg[:, nch:nch + 1])
                P.act(rg[:], sg[:], AF.Sqrt, scale=1.0 / 512, bias=RMS_EPS)
                P.I("dve", "reciprocal", out=rg[:], in_=rg[:])
                y3 = y[:].rearrange("p (g d) -> p g d", g=4)
                P.tt("dve", y3, y3, rg[:].unsqueeze(2).broadcast_to([128, 4, 512]), ALU.mult)
                P.tt("dve", ybf[:], y[:], nw[:], ALU.mult)
            else:
                P.copy("pool", ybf[:], y[:])

        def back(t):
            r0 = t * 128
            x = xt[t % 3]
            transpose_rows(C, yb[t % 2], pT[1], yT[t % 2], 0, KO, None, ev="act")
            for half in range(2):
                for k in range(KO):
                    P.mm(pO[:, half * 512:(half + 1) * 512], yT[t % 2][:, k, :], wo[:, k, half * 512:(half + 1) * 512],
                         start=(k == 0), stop=(k == KO - 1))
            f = ft[t % 2]
            P.act(junk3[:], pO[:], AF.Square, accum_out=ss2[t % 2][:])
            rstd_from_ss(C, rstd2[t % 2][:], ss2[t % 2][:], D_MODEL)
            P.stt("dve", f[:], pO[:], rstd2[t % 2][:], gpost[:], ALU.mult, ALU.mult)
            P.tt("pool", f[:], f[:], x[:], ALU.add)
            P.dma("sp", h_out[r0:r0 + 128, :], f[:])

        front(0)
        for t in range(NTILE):
            if t + 1 < NTILE:
                front(t + 1)
            back(t)
        P.barrier()


NSA_H = 16
NSA_HD = 64
NSA_G = 4
NSA_PROJ = 2608
NEG = -30000.0


def nsa_consts(S):
    nsb = S // 64
    ncmp = (S - 32) // 16 + 1
    cs = np.arange(ncmp) * 16
    sblk = np.arange(nsb) * 64
    ov = np.minimum(cs[:, None] + 32, sblk[None, :] + 64) - np.maximum(cs[:, None], sblk[None, :])
    ov = np.clip(ov, 0, None).astype(np.float32) / 32
    ov_aug = np.zeros((128, 1 + nsb), np.float32)
    ov_aug[:, 0] = 1.0
    ov_aug[:ncmp, 1:] = ov
    pos = np.arange(S)
    blk = np.arange(nsb)[None, :]
    cur = (pos // 64)[:, None]
    forced = (blk == 0) | ((blk <= cur) & (blk > cur - 2))
    forced = np.where(forced, 1e9, 0.0).astype(np.float32)
    E = (np.arange(S)[None, :] // 64 == np.arange(nsb)[:, None]).astype(np.float32)
    return dict(c_ov=ov_aug, c_forced=forced, c_E=E)


def emit_nsa_a(C, h_in, g_pre, w_in, qT_d, kT_d, v_d, gates_d, ntok, seq=SEQ):
    P = C.P
    KC = D_MODEL // 128
    TG = 512
    NT = TG // 128
    with ExitStack() as es:
        gpre = load_featmajor_params(C, es, [g_pre], KC, "ngpre")
        wq = sb(C, es, "wq", [128, KC, NSA_PROJ], BF16)
        for k in range(KC):
            load_cast(C, wq[:, k, :], w_in[k * 128:(k + 1) * 128, :])
        xt = [sb(C, es, "nxt", [128, D_MODEL], F32) for _ in range(2)]
        hn = [sb(C, es, "nhn", [128, D_MODEL], BF16) for _ in range(2)]
        junk = sb(C, es, "njunk", [128, D_MODEL], BF16)
        ss = [sb(C, es, "nss", [128, 1], F32) for _ in range(2)]
        rstd = [sb(C, es, "nrs", [128, 1], F32) for _ in range(2)]
        hnT = sb(C, es, "nhnT", [128, KC, TG], BF16)
        fm = [sb(C, es, "nfm", [128, TG], BF16) for _ in range(4)]
        vt = [sb(C, es, "nvt", [128, 512], BF16) for _ in range(2)]
        gt = [sb(C, es, "ngt", [128, 48], F32) for _ in range(2)]
        pT = ps(C, es, "npT", [128, 8, 128], BF16)
        pY = [ps(C, es, "npY", [128, TG], F32) for _ in range(2)]
        pV = ps(C, es, "npV", [128, 512], F32)
        pG = ps(C, es, "npG", [128, 512], F32)
        for g in range(ntok // TG):
            sidx = (g * TG) // seq
            s0 = (g * TG) % seq
            for t in range(NT):
                x = xt[t % 2]
                r0 = g * TG + t * 128
                P.dma("sp", x[:], h_in[r0:r0 + 128, :])
                hb = hn[t % 2]
                P.act(junk[:], x[:], AF.Square, accum_out=ss[t % 2][:])
                rstd_from_ss(C, rstd[t % 2][:], ss[t % 2][:], D_MODEL)
                P.ts("pool", hb[:], x[:], rstd[t % 2][:], ALU.mult)
                transpose_rows(C, hb, pT, hnT, t * 128, KC, gpre, ev=("dve" if t % 2 else "act"))
            for bp in range(16):
                if bp < 8:
                    col = bp * 128
                    dsts = [qT_d[sidx, 2 * bp + e, :, s0:s0 + TG] for e in range(2)]
                else:
                    b2 = (bp - 8) * 2
                    typ, gg = b2 // 4, b2 % 4
                    col = 1024 + (0, 1, 2, 4)[typ] * 256 + gg * 64
                    dsts = [kT_d[sidx, typ, gg + e, :, s0:s0 + TG] for e in range(2)]
                y = pY[bp % 2]
                for k in range(KC):
                    P.mm(y[:, :], wq[:, k, col:col + 128], hnT[:, k, :], start=(k == 0), stop=(k == KC - 1))
                f = fm[bp % 4]
                P.copy("act" if bp % 2 == 0 else "dve", f[:], y[:, :])
                P.dma("sp", dsts[0], f[0:64, :])
                P.dma("sp", dsts[1], f[64:128, :])
            for t in range(NT):
                hsl = slice(t * 128, (t + 1) * 128)
                for vi, c0 in enumerate((1792, 2304)):
                    for k in range(KC):
                        P.mm(pV[:, vi * 256:(vi + 1) * 256], hnT[:, k, hsl], wq[:, k, c0:c0 + 256], start=(k == 0), stop=(k == KC - 1))
                for k in range(KC):
                    P.mm(pG[:, 0:48], hnT[:, k, hsl], wq[:, k, 2560:2608], start=(k == 0), stop=(k == KC - 1))
                v = vt[t % 2]
                P.copy("dve", v[:], pV[:])
                P.dma("sp", v_d[sidx, 0, s0 + t * 128:s0 + (t + 1) * 128, :], v[:, 0:256])
                P.dma("sp", v_d[sidx, 1, s0 + t * 128:s0 + (t + 1) * 128, :], v[:, 256:512])
                gg_ = gt[t % 2]
                P.act(gg_[:], pG[:, 0:48], AF.Sigmoid)
                r0 = g * TG + t * 128
                P.dma("sp", gates_d[r0:r0 + 128, :], gg_[:])
        P.barrier()


def emit_nsa_c(C, qT_d, kT_d, v_d, gates_d, o_d, cmp_pos, cmp_w1, cmp_w2, c_ov, c_forced, c_E, nseq, seq=SEQ):
    P = C.P
    S = seq
    NQ = S // 128
    NSB = S // 64
    NCMP = (S - 32) // 16 + 1
    assert NCMP <= 127 and NSB <= 32 and NSB >= 8
    NSEL = min(16, NSB)
    WT = 4
    VW = 65 + NSB
    with ExitStack() as es:
        E = sb(C, es, "cE", [NSB, S], BF16)
        P.dma("pool", E[:], c_E[:, :])
        forced = sb(C, es, "cforced", [128, NQ, NSB], F32)
        P.dma("sp", forced[:], c_forced.rearrange("(t p) j -> p t j", p=128))
        onesf = sb(C, es, "aones", [128, 512], F32)
        P.memset("pool", onesf[:], 0.0)
        negU = sb(C, es, "negU", [128, 4, 128], BF16)
        negL = sb(C, es, "negL", [128, 4, 128], BF16)
        zf = onesf[:].rearrange("p (a b) -> p a b", a=4)
        P.I("pool", "affine_select", out=negU[:], in_=zf, pattern=[[0, 4], [1, 128]], compare_op=ALU.is_ge, fill=NEG,
            base=0, channel_multiplier=-1)
        P.I("pool", "affine_select", out=negL[:], in_=zf, pattern=[[0, 4], [-1, 128]], compare_op=ALU.is_gt, fill=NEG,
            base=0, channel_multiplier=1)
        negC = sb(C, es, "negC", [128, NQ, 128], BF16)
        for i in range(NQ):
            P.I("pool", "affine_select", out=negC[:, i, :], in_=onesf[:, 0:128], pattern=[[1, 128]], compare_op=ALU.is_ge,
                fill=NEG, base=128 * i - 31, channel_multiplier=-16)
        w1s = [sb(C, es, "w1s", [64, 32, 256], BF16) for _ in range(2)]
        w2s = [sb(C, es, "w2s", [128, 2, 64], BF16) for _ in range(2)]
        posT = [sb(C, es, "posT", [64, 32], F32) for _ in range(2)]
        with ExitStack() as es2:
            praw = sb(C, es2, "praw", [32, 64], F32)
            pp = ps(C, es2, "ppos", [64, 32], F32)
            for kv in range(2):
                P.dma("pool", w1s[kv][:], cmp_w1[kv].rearrange("(l d) j -> d l j", d=64))
                P.dma("pool", w2s[kv][:], cmp_w2[kv].rearrange("(a p) d -> p a d", p=128))
                P.dma("sp", praw[:], cmp_pos[kv])
                P.tr(pp[:, :], praw[:, :], C.ident_f[0:32, 0:32])
                P.copy("dve", posT[kv][:], pp[:, :])
            P.barrier()
        qT = sb(C, es, "qT", [64, 4, S], BF16)
        kTs = sb(C, es, "kTs", [64, S], BF16)
        kTw = sb(C, es, "kTw", [64, S], BF16)
        tT = [sb(C, es, "tT", [64, S], BF16) for _ in range(2)]
        tl = sb(C, es, "tl", [64, 32, NCMP], BF16)
        hgT = sb(C, es, "hgT", [128, 2, 128], BF16)
        g_sq = sb(C, es, "g_sq", [128, NCMP], F32)
        g_in = sb(C, es, "g_in", [128, NCMP], F32)
        kcT = sb(C, es, "kcT", [64, 128], BF16)
        Vc = sb(C, es, "Vc", [128, VW], BF16)
        Vs = sb(C, es, "Vs", [128, NQ, 65], BF16)
        Vw = sb(C, es, "Vw", [128, NQ, 65], BF16)
        ovf = sb(C, es, "ovf", [128, 1 + NSB], F32)
        gts = sb(C, es, "gts", [128, NQ, 12], F32)
        PTc = sb(C, es, "PTc", [128, 512], BF16)
        PTs = sb(C, es, "PTs", [128, NQ, 512], BF16)
        PTw = sb(C, es, "PTw", [128, WT + 1, 512], BF16)
        rden = [sb(C, es, "rden", [128, 4], F32) for _ in range(3)]
        coef = [sb(C, es, "coef", [128, 4], F32) for _ in range(3)]
        o_acc = [sb(C, es, "o_acc", [128, 4, 64], F32) for _ in range(2)]
        tmpo = [sb(C, es, "tmpo", [128, 4, 64], F32) for _ in range(2)]
        tmp4 = sb(C, es, "tmp4", [128, 4, NSB], F32)
        score = sb(C, es, "score", [128, NSB], F32)
        score2 = sb(C, es, "score2", [128, NSB], F32)
        m8a = sb(C, es, "m8a", [128, 8], F32)
        m8b = sb(C, es, "m8b", [128, 8], F32)
        negq = sb(C, es, "negq", [128, NSB], F32)
        negT = [sb(C, es, "negT", [NSB, 128], BF16) for _ in range(2)]
        pS = [ps(C, es, "apS", [128, 512], F32) for _ in range(2)]
        pOc = ps(C, es, "apOc", [128, 4, 128], F32)
        pOs = ps(C, es, "apOs", [128, 4, 128], F32)
        pOw = ps(C, es, "apOw", [128, 4, 128], F32)
        pN = ps(C, es, "apN", [128, 512], F32)
        P.dma("sp", ovf[:], c_ov[:, :])
        P.memset("pool", hgT[:], 0.0)
        P.memset("pool", Vs[:], 1.0)
        P.memset("pool", Vw[:], 1.0)

        def bc4(ap4, n):
            return ap4.unsqueeze(2).broadcast_to([128, 4, n])

        for sq_ in range(nseq):
            for g in range(NSA_G):
                P.dma("sp", qT[:], qT_d[sq_, 4 * g:4 * g + 4].rearrange("h d s -> d h s"))
                P.dma("sp", tT[0][:], kT_d[sq_, 0, g])
                P.dma("sp", tT[1][:], kT_d[sq_, 1, g])
                P.dma("sp", kTs[:], kT_d[sq_, 2, g])
                P.dma("sp", kTw[:], kT_d[sq_, 3, g])
                P.dma("sp", Vs[:, :, 0:64], v_d[sq_, 0, :, g * 64:(g + 1) * 64].rearrange("(t p) d -> p t d", p=128))
                P.dma("sp", Vw[:, :, 0:64], v_d[sq_, 1, :, g * 64:(g + 1) * 64].rearrange("(t p) d -> p t d", p=128))
                P.dma("sp", gts[:], gates_d[sq_ * S:(sq_ + 1) * S, g * 12:(g + 1) * 12].rearrange("(t p) c -> p t c", p=128))
                for kv in range(2):
                    for l in range(32):
                        b0 = (l // 16) * 16
                        src = tT[kv][:, b0:b0 + NCMP * 16].rearrange("p (c r) -> p c r", r=16)[:, :, l % 16]
                        P.ts("dve" if l % 2 else "pool", tl[:, l, :], src, posT[kv][:, l:l + 1], ALU.add)
                    for jh in range(2):
                        for l in range(32):
                            P.mm(pS[jh][:, 0:NCMP], w1s[kv][:, l, jh * 128:(jh + 1) * 128], tl[:, l, :], start=(l == 0), stop=(l == 31))
                    for jh in range(2):
                        xg = pS[jh][:, 0:NCMP]
                        P.act(g_sq[:], xg, AF.Square)
                        P.ts("dve", g_sq[:], g_sq[:], 0.044715, ALU.mult, 1.0, ALU.add)
                        P.tt("dve", g_in[:], g_sq[:], xg, ALU.mult)
                        P.act(g_in[:], g_in[:], AF.Sigmoid, scale=1.5957691216057308)
                        P.tt("dve", hgT[:, jh, 0:NCMP], g_in[:], xg, ALU.mult)
                    if kv == 0:
                        for jh in range(2):
                            P.mm(pN[0:64, 0:128], w2s[0][:, jh, :], hgT[:, jh, :], start=(jh == 0), stop=(jh == 1))
                        P.copy("act", kcT[:], pN[0:64, 0:128])
                    else:
                        for jh in range(2):
                            P.mm(pN[:, 0:64], hgT[:, jh, :], w2s[1][:, jh, :], start=(jh == 0), stop=(jh == 1))
                        P.copy("act", Vc[:, 0:64], pN[:, 0:64])
                        P.copy("pool", Vc[:, 64:VW], ovf[:])
                def cmp_topk(i):
                    qv = qT[:, :, i * 128:(i + 1) * 128]
                    oa = o_acc[i % 2]
                    sc = pS[0]
                    P.mm(sc[:], kcT[:], qv, start=True, stop=False)
                    P.mm(sc[:], C.ident[:], negC[:, i, :].unsqueeze(1).broadcast_to([128, 4, 128]), start=False, stop=True)
                    P.act(PTc[:], sc[:], AF.Exp, scale=0.125)
                    for h in range(4):
                        P.mm(pOc[:, h, 0:VW], PTc[:, h * 128:(h + 1) * 128], Vc[:], start=True, stop=True)
                    P.ts("dve", rden[0][:], pOc[:, :, 64], 1e-30, ALU.add)
                    P.I("dve", "reciprocal", out=rden[0][:], in_=rden[0][:])
                    P.tt("dve", coef[0][:], rden[0][:], gts[:, i, 0:12:3], ALU.mult)
                    P.tt("dve", oa[:], pOc[:, :, 0:64], bc4(coef[0][:], 64), ALU.mult)
                    P.tt("dve", tmp4[:], pOc[:, :, 65:VW], bc4(rden[0][:], NSB), ALU.mult)
                    P.I("dve", "tensor_reduce", out=score[:], in_=tmp4[:].rearrange("p h j -> p j h"), axis=AX.X, op=ALU.add)
                    P.tt("dve", score[:], score[:], forced[:, i, :], ALU.add)
                    P.I("dve", "max", out=m8a[:], in_=score[:])
                    thr = m8a
                    if NSEL > 8:
                        P.I("dve", "match_replace", out=score2[:], in_to_replace=m8a[:], in_values=score[:], imm_value=-1.0)
                        P.I("dve", "max", out=m8b[:], in_=score2[:])
                        thr = m8b
                    P.ts("dve", negq[:], score[:], thr[:, 7:8], ALU.is_lt, NEG, ALU.mult)
                    P.tr(pN[0:NSB, 0:128], negq[:, :], C.ident_f[:, :])
                    P.copy("act", negT[i % 2][:], pN[0:NSB, 0:128])

                def win_qk(i):
                    qv = qT[:, :, i * 128:(i + 1) * 128]
                    j0 = max(0, i - WT)
                    for j in range(j0, i + 1):
                        sc = pS[j % 2]
                        ks = slice(j * 128, (j + 1) * 128)
                        last_plain = not (j == i or j == i - WT)
                        P.mm(sc[:], kTw[:, ks], qv, start=True, stop=last_plain)
                        if j == i:
                            P.mm(sc[:], C.ident[:], negU[:], start=False, stop=True)
                        elif j == i - WT:
                            P.mm(sc[:], C.ident[:], negL[:], start=False, stop=True)
                        P.act(PTw[:, j - j0, :], sc[:], AF.Exp, scale=0.125)

                def win_pv(i):
                    j0 = max(0, i - WT)
                    for h in range(4):
                        for j in range(j0, i + 1):
                            P.mm(pOw[:, h, 0:65], PTw[:, j - j0, h * 128:(h + 1) * 128], Vw[:, j, :], start=(j == j0), stop=(j == i))
                    P.I("dve", "reciprocal", out=rden[2][:], in_=pOw[:, :, 64])
                    P.tt("dve", coef[2][:], rden[2][:], gts[:, i, 2:12:3], ALU.mult)
                    P.tt("dve", tmpo[0][:], pOw[:, :, 0:64], bc4(coef[2][:], 64), ALU.mult)
                    P.tt("pool", o_acc[i % 2][:], o_acc[i % 2][:], tmpo[0][:], ALU.add)

                def sel_qk(i):
                    qv = qT[:, :, i * 128:(i + 1) * 128]
                    nT = negT[i % 2]
                    for j in range(i + 1):
                        sc = pS[(j + 1) % 2]
                        ks = slice(j * 128, (j + 1) * 128)
                        P.mm(sc[:], kTs[:, ks], qv, start=True, stop=False)
                        P.mm(sc[:], E[:, ks], nT[:].unsqueeze(1).broadcast_to([NSB, 4, 128]), start=False, stop=(j != i))
                        if j == i:
                            P.mm(sc[:], C.ident[:], negU[:], start=False, stop=True)
                        P.act(PTs[:, j, :], sc[:], AF.Exp, scale=0.125)

                def sel_pv(i):
                    oa = o_acc[i % 2]
                    for h in range(4):
                        for j in range(i + 1):
                            P.mm(pOs[:, h, 0:65], PTs[:, j, h * 128:(h + 1) * 128], Vs[:, j, :], start=(j == 0), stop=(j == i))
                    P.I("dve", "reciprocal", out=rden[1][:], in_=pOs[:, :, 64])
                    P.tt("dve", coef[1][:], rden[1][:], gts[:, i, 1:12:3], ALU.mult)
                    P.tt("dve", tmpo[1][:], pOs[:, :, 0:64], bc4(coef[1][:], 64), ALU.mult)
                    P.tt("pool", oa[:], oa[:], tmpo[1][:], ALU.add)
                    r0 = sq_ * S + i * 128
                    P.dma("sp", o_d[r0:r0 + 128, g * 256:(g + 1) * 256], oa[:].rearrange("p h d -> p (h d)"))

                cmp_topk(0)
                for i in range(NQ):
                    if i + 1 < NQ:
                        cmp_topk(i + 1)
                    win_qk(i)
                    sel_qk(i)
                    win_pv(i)
                    sel_pv(i)
        P.barrier()


DEPTH = 4
NSEQ_CORE = 2
W_SHAPES = {
    "norm_gains": [4, 4, 1024],
    "nsa_w_in": [2, 1024, 2608], "nsa_cmp_pos": [2, 2, 32, 64], "nsa_cmp_w1": [2, 2, 2048, 256],
    "nsa_cmp_w2": [2, 2, 256, 64], "nsa_w_out": [2, 1024, 1024],
    "ssd_w_in": [2, 1024, 5152], "ssd_conv_w": [2, 4, 3072], "ssd_conv_b": [2, 3072], "ssd_dt_bias": [2, 32],
    "ssd_a_log": [2, 32], "ssd_d": [2, 32], "ssd_norm_w": [2, 2048], "ssd_w_out": [2, 2048, 1024],
    "ffn_w_up": [4, 1024, 5632], "ffn_conv_w": [4, 3, 5632], "ffn_conv_b": [4, 5632], "ffn_w_down": [4, 2816, 1024],
}


def build_program(nseq=NSEQ_CORE, seq=SEQ, depth=DEPTH):
    nc = bass.Bass("TRN2", target_bir_lowering=False)
    ntok = nseq * seq
    nsb = seq // 64
    W = {k: nc.dram_tensor(k, v, F32, kind="ExternalInput").ap() for k, v in W_SHAPES.items()}
    x = nc.dram_tensor("x", [ntok, D_MODEL], F32, kind="ExternalInput").ap()
    c_ov = nc.dram_tensor("c_ov", [128, 1 + nsb], F32, kind="ExternalInput").ap()
    c_forced = nc.dram_tensor("c_forced", [seq, nsb], F32, kind="ExternalInput").ap()
    c_E = nc.dram_tensor("c_E", [nsb, seq], F32, kind="ExternalInput").ap()
    out = nc.dram_tensor("out", [ntok, D_MODEL], F32, kind="ExternalOutput").ap()

    def scratch(name, shape, dt=F32):
        return nc.dram_tensor(name, shape, dt, kind="Internal").ap()
    hA = scratch("hA", [ntok, D_MODEL])
    hB = scratch("hB", [ntok, D_MODEL])
    fpart = scratch("fpart", [ntok, D_MODEL])
    y_pre = scratch("y_pre", [ntok, SSD_DI])
    o_d = scratch("o_d", [ntok, D_MODEL])
    gates_d = scratch("gates_d", [ntok, 48])
    qT_d = scratch("qT_d", [nseq, NSA_H, 64, seq], BF16)
    kT_d = scratch("kT_d", [nseq, 4, NSA_G, 64, seq], BF16)
    v_d = scratch("v_d", [nseq, 2, seq, 256], BF16)

    P = Prog(nc)
    P.nodep |= set(W_SHAPES) | {"x", "c_ov", "c_forced", "c_E"}
    C = Ctx(nc, P)
    with ExitStack() as es:
        emit_consts(C, es)
        P.barrier()
        cur = x
        for i in range(depth):
            g = W["norm_gains"][i]
            slot = i // 2
            if i % 2 == 0:
                emit_nsa_a(C, cur, g[0], W["nsa_w_in"][slot], qT_d, kT_d, v_d, gates_d, ntok, seq=seq)
                emit_nsa_c(C, qT_d, kT_d, v_d, gates_d, o_d, W["nsa_cmp_pos"][slot], W["nsa_cmp_w1"][slot],
                           W["nsa_cmp_w2"][slot], c_ov, c_forced, c_E, nseq, seq=seq)
                emit_out(C, "nsa", cur, hA, o_d, W["nsa_w_out"][slot], g[1], ntok)
            else:
                emit_ssd_a(C, cur, y_pre, g[0], W["ssd_w_in"][slot], W["ssd_conv_w"][slot], W["ssd_conv_b"][slot],
                           W["ssd_dt_bias"][slot], W["ssd_a_log"][slot], W["ssd_d"][slot], ntok, seq=seq)
                emit_out(C, "ssd", cur, hA, y_pre, W["ssd_w_out"][slot], g[1], ntok,
                         g_pre=g[0], w_in=W["ssd_w_in"][slot], norm_w=W["ssd_norm_w"][slot])
            dst = out if i == depth - 1 else hB
            emit_ffn(C, hA, dst, fpart, g[2], g[3], W["ffn_w_up"][i], W["ffn_conv_w"][i], W["ffn_conv_b"][i],
                     W["ffn_w_down"][i], ntok, seq=seq)
            cur = hB
        P.barrier()
    return nc, P


def kernel(**inputs):
    x = np.ascontiguousarray(inputs["x"], dtype=np.float32)
    B, S, D = x.shape
    assert (B, S, D) == (N_CORES * NSEQ_CORE, SEQ, D_MODEL)
    nc, _ = build_program()
    consts = nsa_consts(SEQ)
    shared = {k: np.ascontiguousarray(inputs[k], dtype=np.float32) for k in W_SHAPES}
    shared.update(consts)
    in_maps = []
    for c in range(N_CORES):
        m = dict(shared)
        m["x"] = x[c * NSEQ_CORE:(c + 1) * NSEQ_CORE].reshape(NSEQ_CORE * SEQ, D_MODEL)
        in_maps.append(m)
    res = run_bass_kernel_spmd(nc, in_maps, core_ids=list(range(N_CORES)))
    outs = [r["out"].reshape(NSEQ_CORE, SEQ, D_MODEL) for r in res.results]
    return np.concatenate(outs, axis=0).astype(np.float32)
```
